# Optimizing a Trainium2 kernel written in Bass

```python
import jax, jax.numpy as jnp
from jax import lax
import numpy as np

D_MODEL = 1024
BATCH = 8
SEQ = 4096
DEPTH = 1

D_MIX = D_MODEL
D_LRU = D_MIX // 2
LRU_BLOCKS = 8
LRU_BD = D_LRU // LRU_BLOCKS
CONV_W = 4
LRU_C = 8.0
N_HEADS = 8
QK_NOPE = 64
QK_ROPE = 32
QK_HEAD = QK_NOPE + QK_ROPE
V_HEAD = (D_MIX - D_LRU) // N_HEADS
Q_LORA = 256
KV_LORA = 128
ROPE_THETA = 10000.0
Q_BLOCK = 128
D_IN = 2 * D_LRU + Q_LORA + KV_LORA + QK_ROPE
N_EXPERTS = 32
TOP_K = 4
D_FF = D_MODEL
SWIGLU_LIMIT = 7.0
SWIGLU_ALPHA = 1.702
MOE_BLOCK = 512
EPS = 1e-6

kernel_name = "hymba_lru_mla_moe_adaln"


def rms_norm(x, g):
    x32 = x.astype(jnp.float32)
    y = x32 * lax.rsqrt(jnp.mean(x32 * x32, axis=-1, keepdims=True) + EPS)
    return (y * g.astype(jnp.float32)).astype(x.dtype)


def modulate(h, shift, scale):
    return h * (1.0 + scale[:, None, :]) + shift[:, None, :]


def apply_rope(x, pos):
    half = x.shape[-1] // 2
    freqs = ROPE_THETA ** (-jnp.arange(half, dtype=jnp.float32) / half)
    ang = pos.astype(jnp.float32)[..., None] * freqs
    cos = jnp.cos(ang)[:, :, None, :]
    sin = jnp.sin(ang)[:, :, None, :]
    x32 = x.astype(jnp.float32)
    x1, x2 = x32[..., :half], x32[..., half:]
    return jnp.concatenate([x1 * cos - x2 * sin, x2 * cos + x1 * sin], axis=-1).astype(x.dtype)


def causal_depthwise_conv(x, w, b):
    S = x.shape[1]
    xp = jnp.pad(x, ((0, 0), (CONV_W - 1, 0), (0, 0)))
    y = xp[:, 0:S] * w[0]
    for j in range(1, CONV_W):
        y = y + xp[:, j:j + S] * w[j]
    return y + b


def _lin_comb(left, right):
    a1, b1 = left
    a2, b2 = right
    return a1 * a2, a2 * b1 + b2


def rg_lru(x, w_a, b_a, w_x, b_x, lam):
    B, S, C = x.shape
    xb = x.reshape(B, S, LRU_BLOCKS, LRU_BD)
    r = jax.nn.sigmoid(jnp.einsum('bsnc,ncd->bsnd', xb, w_a).reshape(B, S, C) + b_a)
    i = jax.nn.sigmoid(jnp.einsum('bsnc,ncd->bsnd', xb, w_x).reshape(B, S, C) + b_x)
    log_a = -LRU_C * r.astype(jnp.float32) * jax.nn.softplus(-lam.astype(jnp.float32))
    a = jnp.exp(log_a)
    mult = jnp.sqrt(-jnp.expm1(2.0 * log_a))
    u = mult * (i * x).astype(jnp.float32)
    _, h = lax.associative_scan(_lin_comb, (a, u), axis=1)
    return h.astype(x.dtype)


def causal_block_attention(q, k, v):
    B, S, H, Dk = q.shape
    nb = S // Q_BLOCK
    qb = q.reshape(B, nb, Q_BLOCK, H, Dk).transpose(1, 0, 2, 3, 4)
    kpos = jnp.arange(S, dtype=jnp.int32)
    scale = QK_HEAD ** -0.5

    def one_block(args):
        qblk, bi = args
        s = jnp.einsum('bqhd,bkhd->bhqk', qblk, k, preferred_element_type=jnp.float32) * scale
        qpos = bi * Q_BLOCK + jnp.arange(Q_BLOCK, dtype=jnp.int32)
        mask = kpos[None, :] <= qpos[:, None]
        s = jnp.where(mask[None, None], s, jnp.float32(-1e30))
        p = jax.nn.softmax(s, axis=-1).astype(v.dtype)
        return jnp.einsum('bhqk,bkhd->bqhd', p, v)

    o = lax.map(one_block, (qb, jnp.arange(nb, dtype=jnp.int32)))
    return o.transpose(1, 0, 2, 3, 4).reshape(B, S, H * v.shape[-1])


def moe_ffn(h, w_router, b_router, w1, b1, w2, b2):
    Bn, S, D = h.shape
    T = Bn * S
    A = T * TOP_K
    hf = h.reshape(T, D)
    logits = (hf @ w_router + b_router).astype(jnp.float32)
    top_vals, top_idx = lax.top_k(logits, TOP_K)
    gates = jax.nn.softmax(top_vals, axis=-1).astype(h.dtype)
    flat_e = top_idx.reshape(A).astype(jnp.int32)
    flat_g = gates.reshape(A)
    flat_tok = jnp.arange(A, dtype=jnp.int32) // TOP_K
    order = jnp.argsort(flat_e)
    se, stok, sg = flat_e[order], flat_tok[order], flat_g[order]
    counts = jnp.zeros((N_EXPERTS,), jnp.int32).at[flat_e].add(1)
    group_start = jnp.cumsum(counts) - counts
    padded = (counts + MOE_BLOCK - 1) // MOE_BLOCK * MOE_BLOCK
    pad_end = jnp.cumsum(padded)
    pad_start = pad_end - padded
    dest = pad_start[se] + jnp.arange(A, dtype=jnp.int32) - group_start[se]
    n_blocks = -(-A // MOE_BLOCK) + N_EXPERTS
    P = n_blocks * MOE_BLOCK
    buf_tok = jnp.zeros((P,), jnp.int32).at[dest].set(stok)
    buf_g = jnp.zeros((P,), h.dtype).at[dest].set(sg)
    block_start = jnp.arange(n_blocks, dtype=jnp.int32) * MOE_BLOCK
    block_e = jnp.clip(jnp.searchsorted(pad_end, block_start, side='right'), 0, N_EXPERTS - 1)

    def body(acc, blk):
        tok, g, e = blk
        xb = hf[tok]
        gu = xb @ w1[e] + b1[e]
        glu = jnp.minimum(gu[:, :D_FF], SWIGLU_LIMIT)
        lin = jnp.clip(gu[:, D_FF:], -SWIGLU_LIMIT, SWIGLU_LIMIT)
        y = ((lin + 1.0) * (glu * jax.nn.sigmoid(SWIGLU_ALPHA * glu))) @ w2[e] + b2[e]
        return acc.at[tok].add(y * g[:, None]), None

    out, _ = lax.scan(body, jnp.zeros_like(hf),
                      (buf_tok.reshape(n_blocks, MOE_BLOCK), buf_g.reshape(n_blocks, MOE_BLOCK), block_e))
    return out.reshape(Bn, S, D)


def setup_inputs(seed: int = 0) -> dict:
    key = jax.random.key(seed)
    ks = jax.random.split(key, 32)
    f32 = jnp.float32
    L = DEPTH

    def nrm(k, shape, scale):
        return jax.random.normal(k, shape, f32) * scale

    def gain(k, shape):
        return 1.0 + 0.01 * jax.random.normal(k, shape, f32)

    x = jax.random.normal(ks[0], (BATCH, SEQ, D_MODEL), f32)
    c = jax.random.normal(ks[1], (BATCH, D_MODEL), f32)
    offs = jax.random.randint(ks[2], (BATCH, 1), 0, 1024, dtype=jnp.int32)
    positions = offs + jnp.arange(SEQ, dtype=jnp.int32)[None, :]
    u = jax.random.uniform(ks[3], (L, D_LRU), f32, 0.9, 0.999)
    s = u ** (1.0 / LRU_C)
    lam = jnp.log(s) - jnp.log1p(-s)
    return {
        "x": x, "c": c, "positions": positions,
        "w_ada": nrm(ks[4], (L, D_MODEL, 6 * D_MODEL), D_MODEL ** -0.5),
        "b_ada": nrm(ks[5], (L, 6 * D_MODEL), 0.01),
        "g_mix": gain(ks[6], (L, D_MODEL)),
        "w_in": nrm(ks[7], (L, D_MODEL, D_IN), D_MODEL ** -0.5),
        "conv_w": nrm(ks[8], (L, CONV_W, D_LRU), CONV_W ** -0.5),
        "conv_b": nrm(ks[9], (L, D_LRU), 0.01),
        "w_a": nrm(ks[10], (L, LRU_BLOCKS, LRU_BD, LRU_BD), LRU_BD ** -0.5),
        "b_a": nrm(ks[11], (L, D_LRU), 0.01),
        "w_x": nrm(ks[12], (L, LRU_BLOCKS, LRU_BD, LRU_BD), LRU_BD ** -0.5),
        "b_x": nrm(ks[13], (L, D_LRU), 0.01),
        "lam": lam,
        "g_q_lat": gain(ks[14], (L, Q_LORA)),
        "w_uq": nrm(ks[15], (L, Q_LORA, N_HEADS * QK_HEAD), Q_LORA ** -0.5),
        "g_kv_lat": gain(ks[16], (L, KV_LORA)),
        "w_ukv": nrm(ks[17], (L, KV_LORA, N_HEADS * (QK_NOPE + V_HEAD)), KV_LORA ** -0.5),
        "g_qn": gain(ks[18], (L, QK_HEAD)),
        "g_kn": gain(ks[19], (L, QK_HEAD)),
        "w_out": nrm(ks[20], (L, D_MIX, D_MODEL), D_MIX ** -0.5),
        "g_ffn": gain(ks[21], (L, D_MODEL)),
        "w_router": nrm(ks[22], (L, D_MODEL, N_EXPERTS), D_MODEL ** -0.5),
        "b_router": nrm(ks[23], (L, N_EXPERTS), 0.01),
        "w1": nrm(ks[24], (L, N_EXPERTS, D_MODEL, 2 * D_FF), D_MODEL ** -0.5),
        "b1": nrm(ks[25], (L, N_EXPERTS, 2 * D_FF), 0.01),
        "w2": nrm(ks[26], (L, N_EXPERTS, D_FF, D_MODEL), D_FF ** -0.5),
        "b2": nrm(ks[27], (L, N_EXPERTS, D_MODEL), 0.01),
    }


def reference(x, c, positions, w_ada, b_ada, g_mix, w_in, conv_w, conv_b, w_a, b_a, w_x, b_x, lam,
              g_q_lat, w_uq, g_kv_lat, w_ukv, g_qn, g_kn, w_out, g_ffn, w_router, b_router,
              w1, b1, w2, b2):
    B, S, D = x.shape
    o1 = 2 * D_LRU
    o2 = o1 + Q_LORA
    o3 = o2 + KV_LORA
    for l in range(DEPTH):
        mod = jax.nn.silu(c) @ w_ada[l] + b_ada[l]
        shift1, scale1, gate1, shift2, scale2, gate2 = jnp.split(mod, 6, axis=-1)

        h = modulate(rms_norm(x, g_mix[l]), shift1, scale1)
        z = h @ w_in[l]
        x_lru, y_lru = z[..., :D_LRU], z[..., D_LRU:o1]
        q_lat, kv_lat, k_rope = z[..., o1:o2], z[..., o2:o3], z[..., o3:]

        xc = causal_depthwise_conv(x_lru, conv_w[l], conv_b[l])
        hr = rg_lru(xc, w_a[l], b_a[l], w_x[l], b_x[l], lam[l])
        lru_out = jax.nn.gelu(y_lru) * hr

        q = (rms_norm(q_lat, g_q_lat[l]) @ w_uq[l]).reshape(B, S, N_HEADS, QK_HEAD)
        kv = (rms_norm(kv_lat, g_kv_lat[l]) @ w_ukv[l]).reshape(B, S, N_HEADS, QK_NOPE + V_HEAD)
        k_nope, v = kv[..., :QK_NOPE], kv[..., QK_NOPE:]
        k_r = jnp.broadcast_to(k_rope[:, :, None, :], (B, S, N_HEADS, QK_ROPE))
        k = jnp.concatenate([k_nope, k_r], axis=-1)
        q = rms_norm(q, g_qn[l])
        k = rms_norm(k, g_kn[l])
        q = jnp.concatenate([q[..., :QK_NOPE], apply_rope(q[..., QK_NOPE:], positions)], axis=-1)
        k = jnp.concatenate([k[..., :QK_NOPE], apply_rope(k[..., QK_NOPE:], positions)], axis=-1)
        attn_out = causal_block_attention(q, k, v)

        mix = jnp.concatenate([lru_out, attn_out], axis=-1) @ w_out[l]
        x = x + gate1[:, None, :] * mix

        h2 = modulate(rms_norm(x, g_ffn[l]), shift2, scale2)
        x = x + gate2[:, None, :] * moe_ffn(h2, w_router[l], b_router[l], w1[l], b1[l], w2[l], b2[l])
    return x
```

```python
import math
from contextlib import ExitStack
import numpy as np
import concourse.bass as bass
import concourse.mybir as mybir
from concourse.bass_utils import run_bass_kernel_spmd

F32 = mybir.dt.float32
BF = mybir.dt.bfloat16
I32 = mybir.dt.int32
AF = mybir.ActivationFunctionType
ALU = mybir.AluOpType
IOA = bass.IndirectOffsetOnAxis

S = 4096
D = 1024
NT = S // 128
NE = 32
CAP = 4096
NBMAX = CAP // 128
EPS = 1e-6
TWO_PI = 2.0 * math.pi


class Dep:
    def __init__(self, nc, name):
        self.sem = nc.alloc_semaphore(name)
        self.n = 0
        self.issuer = None


class Buf:
    def __init__(self, name):
        self.name = name
        self.w = None
        self.r = {}
        self.dsem = None
        self.psum = False


class Eng:
    def __init__(self, k, name, ins, has_dep=True):
        self.name = name
        self.ins = ins
        self.dep = Dep(k.nc, "c_" + name) if has_dep else None
        self.seen = {}


class Kern:
    def __init__(self, nc):
        self.nc = nc
        self.pe = Eng(self, "pe", nc.tensor)
        self.act = Eng(self, "act", nc.scalar)
        self.dve = Eng(self, "dve", nc.vector)
        self.pool = Eng(self, "pool", nc.gpsimd)
        self.sp = Eng(self, "sp", nc.sync, has_dep=False)
        self.engs = [self.pe, self.act, self.dve, self.pool, self.sp]
        self.deps = [e.dep for e in self.engs if e.dep is not None]
        self.nbuf = 0

    def buf(self, name=None):
        self.nbuf += 1
        return Buf(name or f"b{self.nbuf}")

    def pbuf(self, name):
        b = Buf(name)
        b.psum = True
        return b

    def _wait(self, eng, dep, val):
        if dep is eng.dep and eng is self.pe:
            return
        if eng.seen.get(dep, 0) < val:
            eng.ins.wait_ge(dep.sem, val)
            eng.seen[dep] = val

    def _deps_for(self, eng, reads, writes):
        for b in reads:
            if b.w is not None:
                self._wait(eng, *b.w)
        for b in writes:
            if b.w is not None:
                self._wait(eng, *b.w)
            for d, v in b.r.items():
                self._wait(eng, d, v)

    def _commit(self, tok, reads, writes):
        for b in writes:
            b.w = tok
            b.r = {}
        for b in reads:
            if b in writes:
                continue
            d, v = tok
            if b.r.get(d, 0) < v:
                b.r[d] = v

    def record(self, thunk):
        self.rec = []
        thunk()
        r, self.rec = self.rec, None
        return r

    def play(self, *streams):
        idx = [0] * len(streams)
        total = max(len(st) for st in streams) if streams else 0
        for step in range(total):
            for k, st in enumerate(streams):
                upto = (step + 1) * len(st) // total
                while idx[k] < upto:
                    kind, args = st[idx[k]]
                    idx[k] += 1
                    (self.op if kind == "op" else self.dma)(*args)

    def op(self, eng, fn, reads=(), writes=()):
        if getattr(self, "rec", None) is not None:
            self.rec.append(("op", (eng, fn, list(reads), list(writes))))
            return
        reads = list(reads)
        writes = list(writes) + [b for b in reads if b.psum and b not in writes]
        self._deps_for(eng, reads, writes)
        ins = fn()
        d = eng.dep
        d.n += 1
        ins.then_inc(d.sem, 1)
        self._commit((d, d.n), reads, writes)

    def dma(self, eng, fn, reads=(), writes=(), on=None):
        if getattr(self, "rec", None) is not None:
            self.rec.append(("dma", (eng, fn, list(reads), list(writes), on)))
            return
        self._deps_for(eng, reads, writes)
        b = on if on is not None else (writes[0] if writes else reads[0])
        if b.dsem is None:
            b.dsem = Dep(self.nc, "d_" + b.name)
            self.deps.append(b.dsem)
        d = b.dsem
        assert d.issuer in (None, eng), (b.name, d.issuer.name, eng.name)
        d.issuer = eng
        ins = fn()
        for i1 in (ins if isinstance(ins, (list, tuple)) else [ins]):
            d.n += 16
            i1.then_inc(d.sem, 16)
        self._commit((d, d.n), reads, writes)

    def wait_buf(self, eng, b):
        self._deps_for(eng, [b], [])

    def barrier(self):
        for e in self.engs:
            for d in self.deps:
                if d.n > 0:
                    self._wait(e, d, d.n)

    def snapshot(self):
        return ({d: d.n for d in self.deps}, {e: dict(e.seen) for e in self.engs})

    def compensate(self, snap):
        before, _ = snap
        for d in self.deps:
            b4 = before.get(d, 0)
            delta = d.n - b4
            if delta <= 0:
                continue
            eng = d.issuer
            if eng is None:
                eng = [e for e in self.engs if e.dep is d][0]
            eng.ins.wait_ge(d.sem, b4)
            eng.ins.sem_inc(d.sem, delta)

    def restore_seen(self, snap):
        _, seen = snap
        for e in self.engs:
            e.seen = dict(seen[e])


def build_nc(stage="full", use_if=True, nbmax=NBMAX):
    nc = bass.Bass("TRN2", target_bir_lowering=False)
    K = Kern(nc)
    PE, ACT, DVE, POOL, SP = K.pe, K.act, K.dve, K.pool, K.sp
    T, A, V, G, Q = nc.tensor, nc.scalar, nc.vector, nc.gpsimd, nc.sync

    def din(name, shape, dt=F32):
        return nc.dram_tensor(name, list(shape), dt, kind="ExternalInput").ap()

    x_d = din("x", [S, D])
    cT_d = din("cT", [128, 8])
    pos_d = din("pos", [64, S], I32)
    wada_d = din("w_ada", [D, 6 * D])
    bada_d = din("b_ada", [1, 6 * D])
    gmix_d = din("g_mix_bc", [128, D])
    gffn_d = din("g_ffn_bc", [128, D])
    win_d = din("w_in_ext", [D, 1472])
    lruv_d = din("lruv", [128, 4, 8])
    wabd_d = din("wa_bd", [4, 128, 128])
    wxbd_d = din("wx_bd", [4, 128, 128])
    gql_d = din("g_q_lat", [128, 2])
    gkvl_d = din("g_kv_lat", [128, 1])
    wuq_d = din("w_uq_h", [8, 256, 128])
    wuk_d = din("w_uk_h", [8, 128, 128])
    wuv_d = din("w_uv", [128, 512])
    gq_d = din("gq", [128, 1])
    gk_d = din("gk", [128, 1])
    rtab_d = din("rtab", [64, 3])
    wout_d = din("w_out", [D, D])
    wr_d = din("w_router", [D, NE])
    br_d = din("b_router", [1, NE])
    w1_d = din("w1", [NE, D, 2 * D])
    b1_d = din("b1", [NE, 2 * D])
    w2_d = din("w2", [NE, D, D])
    b2_d = din("b2", [NE, D])
    out_d = nc.dram_tensor("out", [S, D], F32, kind="ExternalOutput").ap()
    dbg_d = None
    if stage != "full":
        dbg_d = nc.dram_tensor("dbg", [S, D], F32, kind="ExternalOutput").ap()
    X1_d = nc.dram_tensor("X1s", [S, D], F32, kind="Internal").ap()
    H2r_d = nc.dram_tensor("H2r", [S * 4, D], BF, kind="Internal").ap()
    Yb_d = nc.dram_tensor("Ybs", [S * 4, D], F32, kind="Internal").ap()
    Tab_d = nc.dram_tensor("Tabs", [NE * CAP, 1], I32, kind="Internal").ap()
    b_out, b_dbg, b_X1, b_H2r, b_Yb, b_Tab = K.buf("out"), K.buf("dbg"), K.buf("X1"), K.buf("H2r"), K.buf("Yb"), K.buf("Tab")

    top = ExitStack()

    def sb(es, name, shape, dt=F32):
        return es.enter_context(nc.sbuf_tensor("s_" + name, list(shape), dt))

    def ps(es, name, shape, dt=F32):
        return es.enter_context(nc.psum_tensor("p_" + name, list(shape), dt))

    ident_b = sb(top, "ident_b", [128, 128], BF); b_ident_b = K.buf("identb")
    ident_f = sb(top, "ident_f", [128, 128], F32); b_ident_f = K.buf("identf")
    ones_b = sb(top, "ones_b", [128, 128], BF); b_ones_b = K.buf("onesb")
    ones_f = sb(top, "ones_f", [128, 128], F32); b_ones_f = K.buf("onesf")
    sel_b = sb(top, "sel_b", [128, 128], BF); b_sel = K.buf("sel")
    U_b = sb(top, "U_b", [128, 128], BF); b_U = K.buf("U")
    tri_b = sb(top, "tri_b", [128, 128], BF); b_tri = K.buf("tri")
    iof = sb(top, "iof", [128, 128], F32); b_iof = K.buf("iof")
    pidx = sb(top, "pidx", [128, 1], F32); b_pidx = K.buf("pidx")
    ebase = sb(top, "ebase", [128, NE], F32); b_ebase = K.buf("ebase")
    gate2k = sb(top, "gate2k", [128, D], F32); b_gate2k = K.buf("gate2k")
    G_all = sb(top, "G_all", [128, NT, 4], F32); b_G = K.buf("G_all")
    IDX = sb(top, "IDX", [128, NT, 4], I32); b_IDX = K.buf("IDX")
    cnt_i = sb(top, "cnt_i", [1, NE], I32); b_cnt = K.buf("cnt")
    mod_stack = ExitStack()
    mod = sb(mod_stack, "mod", [128, 5 * D], F32)
    b_mod = [K.buf(f"mod{i}") for i in range(6)]
    cat_stack = ExitStack()
    catL = sb(cat_stack, "catL", [128, 4, S], BF); b_cat = [K.buf(f"cat{i}") for i in range(8)]

    K.op(POOL, lambda: G.iota(iof[:], pattern=[[1, 128]], base=0, channel_multiplier=-1,
                              allow_small_or_imprecise_dtypes=True), writes=[b_iof])
    K.op(POOL, lambda: G.iota(pidx[:], pattern=[[0, 1]], base=0, channel_multiplier=1,
                              allow_small_or_imprecise_dtypes=True), writes=[b_pidx])
    K.op(POOL, lambda: G.iota(ebase[:], pattern=[[CAP, NE]], base=0, channel_multiplier=0,
                              allow_small_or_imprecise_dtypes=True), writes=[b_ebase])
    K.op(DVE, lambda: V.tensor_single_scalar(out=ident_b[:], in_=iof[:], scalar=0.0, op=ALU.is_equal), [b_iof], [b_ident_b])
    K.op(DVE, lambda: V.tensor_single_scalar(out=ident_f[:], in_=iof[:], scalar=0.0, op=ALU.is_equal), [b_iof], [b_ident_f])
    K.op(DVE, lambda: V.tensor_single_scalar(out=U_b[:], in_=iof[:], scalar=0.0, op=ALU.is_gt), [b_iof], [b_U])
    K.op(DVE, lambda: V.tensor_single_scalar(out=tri_b[:], in_=iof[:], scalar=0.0, op=ALU.is_ge), [b_iof], [b_tri])
    K.op(DVE, lambda: V.memset(ones_b[:], 1.0), writes=[b_ones_b])
    K.op(DVE, lambda: V.memset(ones_f[:], 1.0), writes=[b_ones_f])
    K.op(DVE, lambda: V.tensor_scalar(out=sel_b[:], in0=ones_f[:], scalar1=pidx[:, 0:1], scalar2=32.0,
                                      op0=ALU.mult, op1=ALU.is_ge), [b_ones_f, b_pidx], [b_sel])

    def dbg_dump(src_ap, rows, cols, b_src, row0=0, col0=0):
        K.dma(POOL, lambda: G.dma_start(out=dbg_d[row0:row0 + rows, col0:col0 + cols], in_=src_ap), [b_src], [b_dbg])

    def finish():
        K.barrier()
        for d in K.deps:
            if d.n > 0:
                Q.wait_ge(d.sem, d.n)
        pass
        return nc

    with ExitStack() as es:
        cT = sb(es, "cT", [128, 8]); b_cT = K.buf("cT")
        sc = sb(es, "sc", [128, 8]); b_sc = K.buf("sc")
        scb = sb(es, "scb", [128, 8, 128]); b_scb = K.buf("scb")
        bada = sb(es, "bada", [1, 6 * D]); b_bada = K.buf("bada")
        wst = [sb(es, f"wst{i}", [128, 8, 512]) for i in range(2)]; b_wst = [K.buf(f"wst{i}") for i in range(2)]
        pA = [ps(es, f"pA{i}", [128, 512]) for i in range(2)]; b_pA = [K.pbuf(f"pA{i}") for i in range(2)]
        gm = sb(es, "gm", [128, D]); b_gm = K.buf("gm")
        gf = sb(es, "gf", [128, D]); b_gf = K.buf("gf")
        K.dma(SP, lambda: Q.dma_start(out=cT[:], in_=cT_d), writes=[b_cT])
        K.dma(SP, lambda: Q.dma_start(out=bada[:], in_=bada_d), writes=[b_bada])
        K.dma(SP, lambda: Q.dma_start(out=gm[:], in_=gmix_d), writes=[b_gm])
        K.dma(SP, lambda: Q.dma_start(out=gf[:], in_=gffn_d), writes=[b_gf])
        K.op(ACT, lambda: A.activation(out=sc[:], in_=cT[:], func=AF.Silu), [b_cT], [b_sc])
        for kc in range(8):
            K.op(DVE, lambda kc=kc: V.tensor_scalar(out=scb[:, kc, :], in0=ones_f[:], scalar1=sc[:, kc:kc + 1],
                                                   scalar2=None, op0=ALU.mult), [b_sc, b_ones_f], [b_scb])
        wada_v = wada_d.rearrange("(kc p) n -> p kc n", p=128)
        for n in range(12):
            s = n % 2
            K.dma(SP, lambda n=n, s=s: Q.dma_start(out=wst[s][:], in_=wada_v[:, :, n * 512:(n + 1) * 512]), writes=[b_wst[s]])

            def mm(n=n, s=s):
                for kc in range(8):
                    T.matmul(pA[s][:], lhsT=scb[:, kc, :], rhs=wst[s][:, kc, :], start=(kc == 0), stop=False)
                return T.matmul(pA[s][:], lhsT=ones_f[0:1, :], rhs=bada[0:1, n * 512:(n + 1) * 512], start=False, stop=True)
            K.op(PE, mm, [b_scb, b_wst[s], b_ones_f, b_bada], [b_pA[s]])
            if n < 10:
                K.op(ACT, lambda n=n, s=s: A.copy(out=mod[:, n * 512:(n + 1) * 512], in_=pA[s][:]), [b_pA[s]], [b_mod[n // 2]])
            else:
                K.op(ACT, lambda n=n, s=s: A.copy(out=gate2k[:, (n - 10) * 512:(n - 9) * 512], in_=pA[s][:]), [b_pA[s]], [b_gate2k])
        K.op(DVE, lambda: V.scalar_tensor_tensor(out=mod[:, D:2 * D], in0=mod[:, D:2 * D], scalar=1.0, in1=gm[:],
                                                 op0=ALU.add, op1=ALU.mult), [b_gm], [b_mod[1]])
        K.op(DVE, lambda: V.scalar_tensor_tensor(out=mod[:, 4 * D:5 * D], in0=mod[:, 4 * D:5 * D], scalar=1.0, in1=gf[:],
                                                 op0=ALU.add, op1=ALU.mult), [b_gf], [b_mod[4]])
        if stage == "A":
            for i in range(5):
                dbg_dump(mod[0:1, i * D:(i + 1) * D], 1, D, b_mod[i], row0=i)
            dbg_dump(gate2k[0:1, :], 1, D, b_gate2k, row0=5)
            return finish()
        K.barrier()
    SH1, A1, GATE1, SH2, A2 = [mod[:, i * D:(i + 1) * D] for i in range(5)]

    def rms_rstd(ss_ap, n, out_ap, b_in, b_tmp, tmp_ap, b_out_):
        K.op(ACT, lambda: A.activation(out=tmp_ap, in_=ss_ap, func=AF.Ln, scale=1.0 / n, bias=EPS), [b_in], [b_tmp])
        K.op(ACT, lambda: A.activation(out=out_ap, in_=tmp_ap, func=AF.Exp, scale=-0.5), [b_tmp], [b_out_])

    es_mix = ExitStack()
    if True:
        qlatn = sb(es_mix, "qlatn", [128, 2, S], BF); b_qlatn = K.buf("qlatn")
        kvlatn = sb(es_mix, "kvlatn", [128, S], BF); b_kvlatn = K.buf("kvlatn")
        zr = sb(es_mix, "zr", [64, S], BF); b_zr = K.buf("zr")
        with ExitStack() as es:
            winb = sb(es, "winb", [128, 8, 1472], BF); b_winb = K.buf("winb")
            win_v = win_d.rearrange("(kc p) n -> p kc n", p=128)
            for kc in range(8):
                K.dma(POOL, lambda kc=kc: G.dma_start(out=winb[:, kc, :], in_=win_v[:, kc, :]), writes=[b_winb])
            lruv = sb(es, "lruv", [128, 4, 8]); b_lruv = K.buf("lruv")
            K.dma(SP, lambda: Q.dma_start(out=lruv[:], in_=lruv_d), writes=[b_lruv])
            nsp = sb(es, "nsp", [128, 4, 2]); b_nsp = K.buf("nsp")
            spt = sb(es, "spt", [128, 4]); b_spt = K.buf("spt")
            K.op(ACT, lambda: A.activation(out=spt[:], in_=lruv[:, :, 7], func=AF.Exp, scale=-1.0), [b_lruv], [b_spt])
            K.op(ACT, lambda: A.activation(out=spt[:], in_=spt[:], func=AF.Ln, bias=1.0), [b_spt], [b_spt])
            K.op(DVE, lambda: V.tensor_scalar(out=nsp[:, :, 0], in0=spt[:], scalar1=-8.0, scalar2=None, op0=ALU.mult), [b_spt], [b_nsp])
            K.op(DVE, lambda: V.tensor_scalar(out=nsp[:, :, 1], in0=spt[:], scalar1=-16.0, scalar2=None, op0=ALU.mult), [b_spt], [b_nsp])
            wab = sb(es, "wab", [128, 4, 128], BF); b_wab = K.buf("wab")
            wxb = sb(es, "wxb", [128, 4, 128], BF); b_wxb = K.buf("wxb")
            K.dma(POOL, lambda: G.dma_start(out=wab[:], in_=wabd_d.rearrange("c p n -> p c n")), writes=[b_wab])
            K.dma(POOL, lambda: G.dma_start(out=wxb[:], in_=wxbd_d.rearrange("c p n -> p c n")), writes=[b_wxb])
            gql = sb(es, "gql", [128, 2]); b_gql = K.buf("gql")
            gkvl = sb(es, "gkvl", [128, 1]); b_gkvl = K.buf("gkvl")
            K.dma(SP, lambda: Q.dma_start(out=gql[:], in_=gql_d), writes=[b_gql])
            K.dma(SP, lambda: Q.dma_start(out=gkvl[:], in_=gkvl_d), writes=[b_gkvl])
            hstate = sb(es, "hstate", [128, 4]); b_hst = K.buf("hstate")
            K.op(DVE, lambda: V.memset(hstate[:], 0.0), writes=[b_hst])
            xt = [sb(es, f"xt{i}", [128, D]) for i in range(2)]; b_xt = [K.buf(f"xt{i}") for i in range(2)]
            ss = sb(es, "ss", [128, 2]); b_ss = K.buf("ss")
            rstd = sb(es, "rstd", [128, 1]); b_rstd = K.buf("rstd")
            h1 = sb(es, "h1", [128, D]); b_h1 = K.buf("h1")
            hb = sb(es, "hb", [128, D], BF); b_hb = K.buf("hb")
            hT = sb(es, "hT", [128, 8, 512], BF); b_hT = K.buf("hT")
            xl = [[sb(es, f"xl{i}{c}", [128, 3 + 512]) for c in range(4)] for i in range(2)]
            b_xl = [[K.buf(f"xl{i}{c}") for c in range(4)] for i in range(2)]
            gy = [[sb(es, f"gy{i}{c}", [128, 512]) for c in range(4)] for i in range(2)]
            b_gy = [[K.buf(f"gy{i}{c}") for c in range(4)] for i in range(2)]
            sq = [sb(es, f"sq{i}", [128, 512], BF) for i in range(2)]; b_sq = [K.buf(f"sq{i}") for i in range(2)]
            qraw = [sb(es, f"qraw{i}", [128, 512]) for i in range(2)]; b_qraw = [K.buf(f"qraw{i}") for i in range(2)]
            rs = sb(es, "rs", [128, 512]); b_rs = K.buf("rs")
            rt2 = sb(es, "rt2", [128, 512]); b_rt2 = K.buf("rt2")
            xc_ = [sb(es, f"xc{i}", [128, 512]) for i in range(2)]; b_xc_ = [K.buf(f"xc{i}") for i in range(2)]
            xcb_ = [sb(es, f"xcb{i}", [128, 512], BF) for i in range(2)]; b_xcb_ = [K.buf(f"xcb{i}") for i in range(2)]
            rr_ = [sb(es, f"rr{i}", [128, 512]) for i in range(2)]; b_rr_ = [K.buf(f"rr{i}") for i in range(2)]
            ii_ = [sb(es, f"ii{i}", [128, 512]) for i in range(2)]; b_ii_ = [K.buf(f"ii{i}") for i in range(2)]
            aa_ = [sb(es, f"aa{i}", [128, 512]) for i in range(2)]; b_aa_ = [K.buf(f"aa{i}") for i in range(2)]
            mm__ = [sb(es, f"mm{i}", [128, 512]) for i in range(2)]; b_mm_ = [K.buf(f"mm{i}") for i in range(2)]
            pT = ps(es, "pT", [128, D], BF); b_pT = K.pbuf("pT")
            NZ = 2
            pz = [ps(es, f"pz{i}", [128, 512]) for i in range(NZ)]; b_pz = [K.pbuf(f"pz{i}") for i in range(NZ)]
            pg_ = [[ps(es, f"pg{i}{j}", [128, 512]) for j in range(2)] for i in range(2)]
            b_pg_ = [[K.pbuf(f"pg{i}{j}") for j in range(2)] for i in range(2)]
            pss = ps(es, "pss", [128, 512]); b_pss = K.pbuf("pss")
            for c in range(4):
                K.op(DVE, lambda c=c: V.memset(xl[0][c][:, 0:3], 0.0), writes=[b_xl[0][c]])
            zi = [0]

            def norm_unit(g, tt):
                t = g * 4 + tt
                s = t % 2
                K.dma(SP, lambda: Q.dma_start(out=xt[s][:], in_=x_d[t * 128:(t + 1) * 128, :]), writes=[b_xt[s]])
                K.op(ACT, lambda: A.activation(out=h1[:], in_=xt[s][:], func=AF.Square, accum_out=ss[:, 0:1]),
                     [b_xt[s]], [b_h1, b_ss])
                rms_rstd(ss[:, 0:1], D, rstd[:], b_ss, b_ss, ss[:, 1:2], b_rstd)
                K.op(DVE, lambda: V.scalar_tensor_tensor(out=h1[:], in0=xt[s][:], scalar=rstd[:, 0:1], in1=A1,
                                                         op0=ALU.mult, op1=ALU.mult), [b_xt[s], b_rstd, b_mod[1]], [b_h1])
                K.op(DVE, lambda: V.tensor_tensor(out=hb[:], in0=h1[:], in1=SH1, op=ALU.add), [b_h1, b_mod[0]], [b_hb])

                def tr():
                    for kc in range(8):
                        last = T.transpose(pT[:, kc * 128:(kc + 1) * 128], hb[:, kc * 128:(kc + 1) * 128], ident_b[:])
                    return last
                K.op(PE, tr, [b_hb, b_ident_b], [b_pT])
                K.op(ACT, lambda: A.copy(out=hT[:, :, tt * 128:(tt + 1) * 128],
                                         in_=pT[:].rearrange("p (k n) -> p k n", k=8)), [b_pT], [b_hT])

            def inproj_unit(g, ch):
                tsl = slice(g * 512, (g + 1) * 512)
                gp = g % 2
                z = zi[0] % NZ
                zi[0] += 1
                M = 128 if ch < 11 else 64

                def mmz():
                    for kc in range(8):
                        last = T.matmul(pz[z][0:M, :], lhsT=winb[:, kc, ch * 128:ch * 128 + M], rhs=hT[:, kc, :],
                                        start=(kc == 0), stop=(kc == 7))
                    return last
                K.op(PE, mmz, [b_winb, b_hT], [b_pz[z]])
                if ch < 4:
                    c = ch
                    if g > 0:
                        K.op(DVE, lambda: V.tensor_copy(out=xl[gp][c][:, 0:3], in_=xl[1 - gp][c][:, 512:515]), [b_xl[1 - gp][c]], [b_xl[gp][c]])
                    K.op(ACT, lambda: A.copy(out=xl[gp][c][:, 3:515], in_=pz[z][:]), [b_pz[z]], [b_xl[gp][c]])
                elif ch < 8:
                    c = ch - 4
                    K.op(ACT, lambda: A.activation(out=gy[gp][c][:], in_=pz[z][:], func=AF.Gelu), [b_pz[z]], [b_gy[gp][c]])
                elif ch < 11:
                    j = ch - 8 if ch < 10 else 0
                    gcol = gql[:, j:j + 1] if ch < 10 else gkvl[:, 0:1]
                    b_g = b_gql if ch < 10 else b_gkvl
                    K.op(ACT, lambda: A.activation(out=sq[j][:], in_=pz[z][:], func=AF.Square), [b_pz[z]], [b_sq[j]])
                    K.op(DVE, lambda: V.tensor_scalar(out=qraw[j][:], in0=pz[z][:], scalar1=gcol, scalar2=None,
                                                      op0=ALU.mult), [b_pz[z], b_g], [b_qraw[j]])
                    if ch == 9 or ch == 10:
                        nch = 2 if ch == 9 else 1

                        def mms():
                            for j2 in range(nch):
                                last = T.matmul(pss[:], lhsT=ones_b[:], rhs=sq[j2][:], start=(j2 == 0), stop=(j2 == nch - 1))
                            return last
                        K.op(PE, mms, [b_ones_b] + b_sq[:nch], [b_pss])
                        rms_rstd(pss[:], 128 * nch, rs[:], b_pss, b_rt2, rt2[:], b_rs)
                        for j2 in range(nch):
                            dst = qlatn[:, j2, tsl] if ch == 9 else kvlatn[:, tsl]
                            bd = b_qlatn if ch == 9 else b_kvlatn
                            K.op(DVE, lambda j2=j2, dst=dst: V.tensor_tensor(out=dst, in0=qraw[j2][:], in1=rs[:], op=ALU.mult),
                                 [b_qraw[j2], b_rs], [bd])
                else:
                    K.op(ACT, lambda: A.copy(out=zr[:, tsl], in_=pz[z][0:64, :]), [b_pz[z]], [b_zr])

            def lru_unit(g, c):
                tsl = slice(g * 512, (g + 1) * 512)
                gp = g % 2
                q_ = c % 2
                xc, xcb, rr, ii, aa, mm_, pg = xc_[q_], xcb_[q_], rr_[q_], ii_[q_], aa_[q_], mm__[q_], pg_[q_]
                b_xc, b_xcb, b_rr, b_ii, b_aa, b_mm, b_pg = b_xc_[q_], b_xcb_[q_], b_rr_[q_], b_ii_[q_], b_aa_[q_], b_mm_[q_], b_pg_[q_]
                lv = lambda f: lruv[:, c, f:f + 1]
                K.op(ACT, lambda: A.activation(out=xc[:], in_=xl[gp][c][:, 3:515], func=AF.Identity, scale=lv(3), bias=lv(4)),
                     [b_xl[gp][c], b_lruv], [b_xc])
                for j in range(3):
                    K.op(DVE, lambda j=j: V.scalar_tensor_tensor(out=xc[:], in0=xl[gp][c][:, j:j + 512], scalar=lv(j), in1=xc[:],
                                                                 op0=ALU.mult, op1=ALU.add), [b_xl[gp][c], b_lruv], [b_xc])
                K.op(ACT, lambda: A.copy(out=xcb[:], in_=xc[:]), [b_xc], [b_xcb])
                K.op(PE, lambda: T.matmul(pg[0][:], lhsT=wab[:, c, :], rhs=xcb[:], start=True, stop=True), [b_wab, b_xcb], [b_pg[0]])
                K.op(PE, lambda: T.matmul(pg[1][:], lhsT=wxb[:, c, :], rhs=xcb[:], start=True, stop=True), [b_wxb, b_xcb], [b_pg[1]])
                K.op(ACT, lambda: A.activation(out=rr[:], in_=pg[0][:], func=AF.Sigmoid, bias=lv(5)), [b_pg[0], b_lruv], [b_rr])
                K.op(ACT, lambda: A.activation(out=ii[:], in_=pg[1][:], func=AF.Sigmoid, bias=lv(6)), [b_pg[1], b_lruv], [b_ii])
                K.op(ACT, lambda: A.activation(out=aa[:], in_=rr[:], func=AF.Exp, scale=nsp[:, c, 0:1]), [b_rr, b_nsp], [b_aa])
                K.op(ACT, lambda: A.activation(out=mm_[:], in_=rr[:], func=AF.Exp, scale=nsp[:, c, 1:2]), [b_rr, b_nsp], [b_mm])
                K.op(ACT, lambda: A.activation(out=mm_[:], in_=mm_[:], func=AF.Sqrt, scale=-1.0, bias=1.0), [b_mm], [b_mm])
                K.op(DVE, lambda: V.tensor_tensor(out=ii[:], in0=ii[:], in1=xc[:], op=ALU.mult), [b_xc], [b_ii])
                K.op(DVE, lambda: V.tensor_tensor(out=ii[:], in0=ii[:], in1=mm_[:], op=ALU.mult), [b_mm], [b_ii])
                K.op(DVE, lambda: V.tensor_tensor_scan(out=rr[:], data0=aa[:], data1=ii[:], initial=hstate[:, c:c + 1],
                                                       op0=ALU.mult, op1=ALU.add), [b_aa, b_ii, b_hst], [b_rr])
                K.op(DVE, lambda: V.tensor_copy(out=hstate[:, c:c + 1], in_=rr[:, 511:512]), [b_rr], [b_hst])
                K.op(DVE, lambda: V.tensor_tensor(out=catL[:, c, tsl], in0=rr[:], in1=gy[gp][c][:], op=ALU.mult),
                     [b_rr, b_gy[gp][c]], [b_cat[c]])

            def x_units(g):
                return [lambda tt=tt: norm_unit(g, tt) for tt in range(4)] + [lambda ch=ch: inproj_unit(g, ch) for ch in range(12)]

            for u in x_units(0):
                u()
            if stage == "B":
                K.barrier()
                dbg_dump(hT[:, 0, :], 128, 512, b_hT)
                return finish()
            if stage == "C":
                K.barrier()
                dbg_dump(xl[0][0][:, 3:515], 128, 512, b_xl[0][0])
                dbg_dump(qlatn[:, 0, 0:512], 128, 512, b_qlatn, row0=128)
                return finish()
            for g in range(8):
                nx = x_units(g + 1) if g + 1 < 8 else []
                for c2 in range(2):
                    ra0 = K.record(lambda: lru_unit(g, 2 * c2))
                    ra1 = K.record(lambda: lru_unit(g, 2 * c2 + 1))
                    rb = K.record(lambda: [u() for u in nx[8 * c2:8 * c2 + 8]])
                    K.play(ra0, ra1, rb) if rb else K.play(ra0, ra1)
            if stage == "D":
                for c in range(4):
                    dbg_dump(catL[:, c, 0:1024], 128, 1024, b_cat[c], row0=c * 128)
                return finish()
            K.barrier()

        catA_stack = ExitStack()
        catA = sb(catA_stack, "catA", [128, 4, S], BF)
        with ExitStack() as es:
            wuqb = sb(es, "wuqb", [128, 2, 8, 128], BF); b_wuqb = K.buf("wuqb")
            wukb = sb(es, "wukb", [128, 8, 128], BF); b_wukb = K.buf("wukb")
            wuvb = sb(es, "wuvb", [128, 512], BF); b_wuvb = K.buf("wuvb")
            for h in range(8):
                K.dma(POOL, lambda h=h: G.dma_start(out=wuqb[:, :, h, :], in_=wuq_d[h].rearrange("(kc p) n -> p kc n", p=128)), writes=[b_wuqb])
            K.dma(POOL, lambda: G.dma_start(out=wukb[:], in_=wuk_d.rearrange("h p n -> p h n")), writes=[b_wukb])
            K.dma(POOL, lambda: G.dma_start(out=wuvb[:], in_=wuv_d), writes=[b_wuvb])
            gq = sb(es, "gq", [128, 1]); b_gq = K.buf("gq")
            gk = sb(es, "gk", [128, 1]); b_gk = K.buf("gk")
            rtab = sb(es, "rtab", [64, 3]); b_rtab = K.buf("rtab")
            K.dma(SP, lambda: Q.dma_start(out=gq[:], in_=gq_d), writes=[b_gq])
            K.dma(SP, lambda: Q.dma_start(out=gk[:], in_=gk_d), writes=[b_gk])
            K.dma(SP, lambda: Q.dma_start(out=rtab[:], in_=rtab_d), writes=[b_rtab])
            esel = ident_b
            Tb = sb(es, "Tb", [64, S], BF); b_Tb = K.buf("Tb")
            with ExitStack() as es2:
                CW = 1024
                posi = sb(es2, "posi", [64, CW], I32); b_posi = K.buf("posi")
                tt_ = sb(es2, "tt_", [64, CW]); b_tt = K.buf("tt")
                kf = sb(es2, "kf", [64, CW]); b_kf = K.buf("kf")
                ki = sb(es2, "ki", [64, CW], I32); b_ki = K.buf("ki")
                for cc in range(S // CW):
                    csl = slice(cc * CW, (cc + 1) * CW)
                    K.dma(SP, lambda csl=csl: Q.dma_start(out=posi[:], in_=pos_d[:, csl]), writes=[b_posi])
                    K.op(DVE, lambda: V.tensor_copy(out=tt_[:], in_=posi[:]), [b_posi], [b_tt])
                    K.op(DVE, lambda: V.tensor_scalar(out=tt_[:], in0=tt_[:], scalar1=rtab[:, 0:1], scalar2=rtab[:, 1:2],
                                                      op0=ALU.mult, op1=ALU.add), [b_rtab], [b_tt])
                    K.op(DVE, lambda: V.tensor_scalar(out=kf[:], in0=tt_[:], scalar1=1.0 / TWO_PI, scalar2=None, op0=ALU.mult), [b_tt], [b_kf])
                    K.op(DVE, lambda: V.tensor_copy(out=ki[:], in_=kf[:]), [b_kf], [b_ki])
                    K.op(DVE, lambda: V.tensor_copy(out=kf[:], in_=ki[:]), [b_ki], [b_kf])
                    K.op(DVE, lambda: V.scalar_tensor_tensor(out=tt_[:], in0=kf[:], scalar=-TWO_PI, in1=tt_[:], op0=ALU.mult, op1=ALU.add),
                         [b_kf], [b_tt])
                    K.op(DVE, lambda: V.tensor_scalar(out=kf[:], in0=tt_[:], scalar1=math.pi, scalar2=-TWO_PI, op0=ALU.is_gt, op1=ALU.mult),
                         [b_tt], [b_kf])
                    K.op(DVE, lambda: V.tensor_tensor(out=tt_[:], in0=tt_[:], in1=kf[:], op=ALU.add), [b_kf], [b_tt])
                    K.op(DVE, lambda: V.tensor_scalar(out=tt_[:], in0=tt_[:], scalar1=-math.pi, scalar2=math.pi, op0=ALU.max, op1=ALU.min),
                         [], [b_tt])
                    K.op(ACT, lambda csl=csl: A.activation(out=Tb[:, csl], in_=tt_[:], func=AF.Sin), [b_tt], [b_Tb])
                    K.op(DVE, lambda csl=csl: V.tensor_scalar(out=Tb[:, csl], in0=Tb[:, csl], scalar1=rtab[:, 2:3], scalar2=None, op0=ALU.mult),
                         [b_rtab], [b_Tb])
                K.barrier()
            qT = [sb(es, f"qT{i}", [128, S], BF) for i in range(2)]; b_qT = [K.buf(f"qT{i}") for i in range(2)]
            kT = [sb(es, f"kT{i}", [128, S], BF) for i in range(2)]; b_kT = [K.buf(f"kT{i}") for i in range(2)]
            Va = [sb(es, f"Va{i}", [128, NT, 128], BF) for i in range(2)]; b_Va = [K.buf(f"Va{i}") for i in range(2)]
            for i in range(2):
                K.op(POOL, lambda i=i: G.memset(Va[i][:, :, 64:128], 1.0), writes=[b_Va[i]])
                K.op(DVE, lambda i=i: V.memset(qT[i][0:32, :], 0.0), writes=[b_qT[i]])
                K.op(DVE, lambda i=i: V.memset(kT[i][0:32, :], 0.0), writes=[b_kT[i]])
            sqa = sb(es, "sqa", [128, 512], BF); b_sqa = K.buf("sqa")
            rsa = sb(es, "rsa", [128, 512]); b_rsa = K.buf("rsa")
            qn = sb(es, "qn", [128, 512]); b_qn = K.buf("qn")
            tmp2 = sb(es, "tmp2", [128, 512]); b_tmp2 = K.buf("tmp2")
            PT = [sb(es, f"PT{i}", [128, 2, 512], BF) for i in range(2)]; b_PT = [K.buf(f"PT{i}") for i in range(2)]
            rd = sb(es, "rd", [64, 512]); b_rd = K.buf("rd")
            pq = ps(es, "pq", [128, 512]); b_pq = K.pbuf("pq")
            pssa = ps(es, "pssa", [128, 512]); b_pssa = K.pbuf("pssa")
            pv = pssa[:].rearrange("p (a b) -> p a b", a=8); b_pv = b_pssa
            pS = [ps(es, f"pS{i}", [128, 2, 512]) for i in range(2)]; b_pS = [K.pbuf(f"pS{i}") for i in range(2)]
            pO = [ps(es, f"pO{i}", [128, 512]) for i in range(2)]; b_pO = [K.pbuf(f"pO{i}") for i in range(2)]
            SCALE = math.sqrt(96.0)
            import os as _os2
            S_REP = int(_os2.environ.get("S_REP", "1"))
            def prep_units(h):
                hb_ = h % 2
                phases = []
                for tg in range(4):
                    def uv_a(tg=tg):
                        def mmv():
                            for j in range(8):
                                t = tg * 8 + j
                                last = T.matmul(pv[:, j, :], lhsT=kvlatn[:, t * 128:(t + 1) * 128], rhs=wuvb[:, h * 64:(h + 1) * 64],
                                                start=True, stop=True)
                            return last
                        K.op(PE, mmv, [b_kvlatn, b_wuvb], [b_pv])

                    def uv_b(tg=tg):
                        K.op(ACT, lambda: A.copy(out=Va[hb_][:, tg * 8:(tg + 1) * 8, 0:64], in_=pv), [b_pv], [b_Va[hb_]])
                    phases += [uv_a, uv_b]
                for g in range(8):
                    for which in range(2):
                        tsl = slice(g * 512, (g + 1) * 512)
                        if which == 0:
                            gcol, b_gc, dstT, b_dst = gq, b_gq, qT[hb_], b_qT[hb_]
                        else:
                            gcol, b_gc, dstT, b_dst = gk, b_gk, kT[hb_], b_kT[hb_]

                        def ua(tsl=tsl, which=which):
                            if which == 0:
                                def mmq():
                                    T.matmul(pq[:], lhsT=wuqb[:, 0, h, :], rhs=qlatn[:, 0, tsl], start=True, stop=False)
                                    return T.matmul(pq[:], lhsT=wuqb[:, 1, h, :], rhs=qlatn[:, 1, tsl], start=False, stop=True)
                                K.op(PE, mmq, [b_wuqb, b_qlatn], [b_pq])
                            else:
                                def mmk():
                                    T.matmul(pq[:], lhsT=wukb[:, h, :], rhs=kvlatn[:, tsl], start=True, stop=False)
                                    return T.matmul(pq[:], lhsT=esel[0:64, :], rhs=zr[:, tsl], start=False, stop=True)
                                K.op(PE, mmk, [b_wukb, b_kvlatn, b_ident_b, b_zr], [b_pq])

                        def ub():
                            K.op(ACT, lambda: A.activation(out=sqa[:], in_=pq[:], func=AF.Square), [b_pq], [b_sqa])
                            K.op(PE, lambda: T.matmul(pssa[:], lhsT=sel_b[:], rhs=sqa[:], start=True, stop=True), [b_sel, b_sqa], [b_pssa])

                        def uc(tsl=tsl, gcol=gcol, b_gc=b_gc, dstT=dstT, b_dst=b_dst):
                            K.op(ACT, lambda: A.activation(out=tmp2[:], in_=pssa[:], func=AF.Ln, bias=96.0 * EPS), [b_pssa], [b_tmp2])
                            K.op(ACT, lambda: A.activation(out=rsa[:], in_=tmp2[:], func=AF.Exp, scale=-0.5), [b_tmp2], [b_rsa])
                            K.op(DVE, lambda: V.scalar_tensor_tensor(out=qn[:], in0=pq[:], scalar=gcol[:, 0:1], in1=rsa[:],
                                                                     op0=ALU.mult, op1=ALU.mult), [b_pq, b_gc, b_rsa], [b_qn])
                            K.op(DVE, lambda: V.tensor_tensor(out=qn[0:64, :], in0=qn[0:64, :], in1=Tb[:, tsl], op=ALU.mult), [b_Tb], [b_qn])
                            K.op(DVE, lambda: V.tensor_copy(out=tmp2[32:64, :], in_=qn[0:32, :]), [b_qn], [b_tmp2])
                            K.op(DVE, lambda: V.tensor_tensor(out=dstT[32:64, tsl], in0=qn[32:64, :], in1=tmp2[32:64, :], op=ALU.add),
                                 [b_qn, b_tmp2], [b_dst])
                            K.op(POOL, lambda: G.tensor_copy(out=dstT[64:128, tsl], in_=qn[64:128, :]), [b_qn], [b_dst])
                        phases += [ua, ub, uc]
                return phases

            for u in prep_units(0):
                u()
            if stage == "E0":
                K.barrier()
                dbg_dump(qT[0][:, 0:1024], 128, 1024, b_qT[0], row0=0)
                dbg_dump(kT[0][:, 0:1024], 128, 1024, b_kT[0], row0=128)
                dbg_dump(Va[0][:, 0:8, :].rearrange("p a b -> p (a b)"), 128, 1024, b_Va[0], row0=256)
                return finish()
            oi = 0
            for h in range(8):
                hb_ = h % 2
                nxt = prep_units(h + 1) if h + 1 < 8 else []
                steps = [(qg, kp) for qg in range(8) for kp in range(2 * qg + 2)]
                every = 1 if nxt else 0

                def geom(qg, kt):
                    n0 = 0 if kt < 4 * qg else 128 * (kt - 4 * qg)
                    return n0, 512 - n0

                def emit_S(i):
                    qg, kp = steps[i]
                    s = i % 2

                    def mmS():
                        for j in range(2):
                            kt = 2 * kp + j
                            n0, w = geom(qg, kt)
                            last = T.matmul(pS[s][:, j, 0:w], lhsT=kT[hb_][:, kt * 128:(kt + 1) * 128],
                                            rhs=qT[hb_][:, qg * 512 + n0:(qg + 1) * 512], start=True, stop=True)
                        return last
                    K.op(PE, mmS, [b_kT[hb_], b_qT[hb_]], [b_pS[s]])
                emit_S(0)
                o = oi % 2
                for i, (qg, kp) in enumerate(steps):
                    s = i % 2
                    nkt = 4 * qg + 4
                    diag = kp >= 2 * qg
                    if kp == 0:
                        o = oi % 2
                        oi += 1
                    if i + 1 < len(steps):
                        emit_S(i + 1)
                    if not diag:
                        K.op(ACT, lambda: A.activation(out=PT[s][:].rearrange("p a b -> p (a b)"), in_=pS[s][:].rearrange("p a b -> p (a b)"),
                                                       func=AF.Exp, scale=SCALE), [b_pS[s]], [b_PT[s]])
                    else:
                        for j in range(2):
                            n0, w = geom(qg, 2 * kp + j)
                            K.op(ACT, lambda j=j, w=w: A.activation(out=PT[s][:, j, 0:w], in_=pS[s][:, j, 0:w], func=AF.Exp, scale=SCALE),
                                 [b_pS[s]], [b_PT[s]])
                        for j in range(2):
                            K.op(DVE, lambda j=j: V.tensor_tensor(out=PT[s][:, j, 0:128], in0=PT[s][:, j, 0:128], in1=tri_b[:], op=ALU.mult),
                                 [b_tri], [b_PT[s]])

                    def mmPV():
                        for j in range(2):
                            kt = 2 * kp + j
                            n0, w = geom(qg, kt)
                            last = T.matmul(pO[o][:, n0:512], lhsT=Va[hb_][:, kt, :], rhs=PT[s][:, j, 0:w],
                                            start=(kt == 0), stop=(kt == nkt - 1))
                        return last
                    K.op(PE, mmPV, [b_Va[hb_], b_PT[s]], [b_pO[o]])
                    if kp == 2 * qg + 1:
                        K.op(DVE, lambda: V.reciprocal(out=rd[:], in_=pO[o][64:128, :]), [b_pO[o]], [b_rd])
                        ch = 4 + h // 2
                        p0 = (h % 2) * 64
                        K.op(DVE, lambda: V.tensor_tensor(out=catA[p0:p0 + 64, ch - 4, qg * 512:(qg + 1) * 512],
                                                          in0=pO[o][0:64, :], in1=rd[:], op=ALU.mult),
                             [b_pO[o], b_rd], [b_cat[ch]])
                    if nxt and every and i % every == every - 1 and (i // every) < len(nxt):
                        nxt[i // every]()
                for j in range((len(steps) // every) if every else 0, len(nxt)):
                    nxt[j]()
            if stage == "E":
                K.barrier()
                for c in range(4):
                    dbg_dump(catA[:, c, 0:1024], 128, 1024, b_cat[4 + c], row0=c * 128)
                return finish()
            K.barrier()

    with ExitStack() as es:
        wob = sb(es, "wob", [128, 8, D], BF); b_wob = K.buf("wob")
        wst2 = [sb(es, f"wst2{i}", [128, D]) for i in range(2)]; b_wst2 = [K.buf(f"wst2{i}") for i in range(2)]
        wout_v = wout_d.rearrange("(kc p) n -> p kc n", p=128)
        for kc in range(8):
            s = kc % 2
            K.dma(SP, lambda kc=kc, s=s: Q.dma_start(out=wst2[s][:], in_=wout_v[:, kc, :]), writes=[b_wst2[s]])
            K.op(DVE, lambda kc=kc, s=s: V.tensor_tensor(out=wob[:, kc, :], in0=wst2[s][:], in1=GATE1, op=ALU.mult),
                 [b_wst2[s], b_mod[2]], [b_wob])
        wrt = sb(es, "wrt", [128, 8, NE]); b_wrt = K.buf("wrt")
        brt = sb(es, "brt", [1, NE]); b_brt = K.buf("brt")
        K.dma(SP, lambda: Q.dma_start(out=wrt[:], in_=wr_d.rearrange("(kc p) n -> p kc n", p=128)), writes=[b_wrt])
        K.dma(SP, lambda: Q.dma_start(out=brt[:], in_=br_d), writes=[b_brt])
        mask_all = sb(es, "mask_all", [128, NT, NE], BF); b_mask = [K.buf(f"mask{t}") for t in range(NT)]
        VAL = sb(es, "VAL", [128, NT, 4], I32); b_VAL = K.buf("VAL")
        K.op(POOL, lambda: G.iota(VAL[:], pattern=[[512, NT], [1, 4]], base=0, channel_multiplier=4), writes=[b_VAL])
        tinit = sb(es, "tinit", [128, 1024], I32); b_tinit = K.buf("tinit")
        K.op(POOL, lambda: G.iota(tinit[:], pattern=[[0, 1024]], base=1 << 30, channel_multiplier=0), writes=[b_tinit])
        K.dma(POOL, lambda: G.dma_start(out=Tab_d.rearrange("(p n) o -> p (n o)", p=128), in_=tinit[:]), [b_tinit], [b_Tab])
        H2r_v = H2r_d.rearrange("(n r) d -> n r d", r=4)
        TC = 8
        xt = [sb(es, f"xtf{i}", [128, D]) for i in range(2)]; b_xt = [K.buf(f"xtf{i}") for i in range(2)]
        x1 = [sb(es, f"x1{i}", [128, D]) for i in range(2)]; b_x1 = [K.buf(f"x1{i}") for i in range(2)]
        junk = sb(es, "junkf", [128, D], BF); b_junk = K.buf("junkf")
        ss = sb(es, "ssf", [128, 2]); b_ss = K.buf("ssf")
        rstd = sb(es, "rstdf", [128, 1]); b_rstd = K.buf("rstdf")
        h2f = [sb(es, f"h2f{i}", [128, D]) for i in range(2)]; b_h2f = [K.buf(f"h2f{i}") for i in range(2)]
        h2b = [sb(es, f"h2b{i}", [128, D], BF) for i in range(2)]; b_h2b = [K.buf(f"h2b{i}") for i in range(2)]
        h2T = sb(es, "h2T", [128, 8, 128]); b_h2T = K.buf("h2T")
        lg_all = sb(es, "lg_all", [128, NT, NE]); b_lg = [K.buf(f"lg{c}") for c in range(NT // TC)]
        top8_all = sb(es, "top8_all", [128, NT, 8]); b_top8 = [K.buf(f"top8{c}") for c in range(NT // TC)]
        d4 = sb(es, "d4", [128, TC, 4]); b_d4 = K.buf("d4")
        den = sb(es, "den", [128, TC]); b_den = K.buf("den")
        oh = sb(es, "oh", [128, 4, TC, NE]); b_oh = K.buf("oh")
        mk = sb(es, "mk", [128, TC, NE]); b_mk = K.buf("mk")
        idxfull = sb(es, "idxfull", [128, TC, NE]); b_idxf = K.buf("idxf")
        junk2 = sb(es, "junk2", [128, TC, NE]); b_junk2 = K.buf("junk2")
        IDXf = sb(es, "IDXf", [128, NT, 4]); b_idx4 = K.buf("idx4")
        cntf = sb(es, "cntf", [1, NE]); b_cntf = K.buf("cntf")
        pm = [ps(es, f"pm{i}", [128, D]) for i in range(2)]; b_pm = [K.pbuf(f"pm{i}") for i in range(2)]
        pT32 = ps(es, "pT32", [128, D]); b_pT32 = K.pbuf("pT32")
        pl = ps(es, "pl", [128, NE]); b_pl = K.pbuf("pl")
        ppos = ps(es, "ppos", [128, TC, NE]); b_ppos = K.pbuf("ppos")
        pcnt = pl[0:1, :]; b_pcnt = b_pl
        AXX = mybir.AxisListType.X

        def load_x(t):
            K.dma(SP, lambda: Q.dma_start(out=xt[t % 2][:], in_=x_d[t * 128:(t + 1) * 128, :]), writes=[b_xt[t % 2]])

        def route_chunk(c):
            t0 = c * TC
            sl = slice(t0, t0 + TC)
            bl, bt = b_lg[c], b_top8[c]
            K.op(DVE, lambda: V.tensor_tensor(out=d4[:], in0=top8_all[:, sl, 0:4], in1=top8_all[:, sl, 0:1].to_broadcast([128, TC, 4]),
                                              op=ALU.subtract), [bt], [b_d4])
            K.op(ACT, lambda: A.activation(out=d4[:], in_=d4[:], func=AF.Exp), [], [b_d4])
            K.op(DVE, lambda: V.tensor_reduce(out=den[:], in_=d4[:], axis=AXX, op=ALU.add), [b_d4], [b_den])
            K.op(DVE, lambda: V.reciprocal(out=den[:], in_=den[:]), [], [b_den])
            K.op(DVE, lambda: V.tensor_tensor(out=G_all[:, sl, :], in0=d4[:], in1=den[:].to_broadcast([128, TC, 4]) if False else
                                              den[:].rearrange("p (t o) -> p t o", o=1).to_broadcast([128, TC, 4]), op=ALU.mult),
                 [b_d4, b_den], [b_G])
            for r in range(4):
                K.op(DVE, lambda r=r: V.tensor_tensor(out=oh[:, r, :, :], in0=lg_all[:, sl, :],
                                                      in1=top8_all[:, sl, r:r + 1].to_broadcast([128, TC, NE]), op=ALU.is_equal),
                     [bl, bt], [b_oh])
            K.op(DVE, lambda: V.tensor_tensor(out=mk[:], in0=oh[:, 0, :, :], in1=oh[:, 1, :, :], op=ALU.add), [b_oh], [b_mk])
            K.op(DVE, lambda: V.tensor_tensor(out=mk[:], in0=mk[:], in1=oh[:, 2, :, :], op=ALU.add), [b_oh], [b_mk])
            K.op(DVE, lambda: V.tensor_tensor(out=mask_all[:, sl, :], in0=mk[:], in1=oh[:, 3, :, :], op=ALU.add), [b_oh, b_mk], b_mask[t0:t0 + TC])

            def mmp():
                for j in range(TC):
                    t = t0 + j
                    last = T.matmul(ppos[:, j, :], lhsT=U_b[:], rhs=mask_all[:, t, :], start=True, stop=(t == 0))
                    for i in range(t):
                        last = T.matmul(ppos[:, j, :], lhsT=ones_b[:], rhs=mask_all[:, i, :], start=False, stop=(i == t - 1))
                return last
            K.op(PE, mmp, [b_U, b_ones_b] + b_mask[:t0 + TC], [b_ppos])
            K.op(DVE, lambda: V.tensor_tensor(out=idxfull[:], in0=ppos[:], in1=ebase[:].rearrange("p (o e) -> p o e", o=1).to_broadcast([128, TC, NE]),
                                              op=ALU.add), [b_ppos, b_ebase], [b_idxf])
            for r in range(4):
                K.op(DVE, lambda r=r: V.tensor_tensor(out=junk2[:], in0=oh[:, r, :, :], in1=idxfull[:], op=ALU.mult), [b_oh, b_idxf], [b_junk2])
                K.op(DVE, lambda r=r: V.tensor_reduce(out=IDXf[:, sl, r], in_=junk2[:], axis=AXX, op=ALU.add), [b_junk2], [b_idx4])
            K.op(DVE, lambda: V.tensor_copy(out=IDX[:, sl, :], in_=IDXf[:, sl, :]), [b_idx4], [b_IDX])
            K.dma(POOL, lambda: [G.indirect_dma_start(out=Tab_d, out_offset=IOA(ap=IDX[:, t, r:r + 1], axis=0),
                                                      in_=VAL[:, t, r:r + 1], in_offset=None)
                                 for t in range(t0, t0 + TC) for r in range(4)],
                  [b_VAL, b_IDX], [b_Tab])

        def stage_a(t):
            s = t % 2
            rows = slice(t * 128, (t + 1) * 128)
            if t + 1 < NT:
                load_x(t + 1)

            def mmo():
                for half in range(2):
                    for kc in range(8):
                        last = T.matmul(pm[s][:, half * 512:(half + 1) * 512], lhsT=(catL[:, kc, rows] if kc < 4 else catA[:, kc - 4, rows]),
                                        rhs=wob[:, kc, half * 512:(half + 1) * 512], start=(kc == 0), stop=(kc == 7))
                return last
            K.op(PE, mmo, b_cat + [b_wob], [b_pm[s]])
            K.op(DVE, lambda: V.tensor_tensor(out=x1[s][:], in0=pm[s][:], in1=xt[s][:], op=ALU.add), [b_pm[s], b_xt[s]], [b_x1[s]])
            K.dma(ACT, lambda: A.dma_start(out=X1_d[rows, :], in_=x1[s][:]), [b_x1[s]], [b_X1])
            if stage == "F1":
                dbg_dump(x1[s][:], 128, D, b_x1[s], row0=t * 128)
                return
            K.op(ACT, lambda: A.activation(out=junk[:], in_=x1[s][:], func=AF.Square, accum_out=ss[:, 0:1]), [b_x1[s]], [b_junk, b_ss])
            rms_rstd(ss[:, 0:1], D, rstd[:], b_ss, b_ss, ss[:, 1:2], b_rstd)
            K.op(DVE, lambda: V.scalar_tensor_tensor(out=h2f[s][:], in0=x1[s][:], scalar=rstd[:, 0:1], in1=A2, op0=ALU.mult, op1=ALU.mult),
                 [b_x1[s], b_rstd, b_mod[4]], [b_h2f[s]])
            K.op(DVE, lambda: V.tensor_tensor(out=h2f[s][:], in0=h2f[s][:], in1=SH2, op=ALU.add), [b_mod[3]], [b_h2f[s]])
            K.op(POOL, lambda: G.tensor_copy(out=h2b[s][:], in_=h2f[s][:]), [b_h2f[s]], [b_h2b[s]])
            K.dma(POOL, lambda: [G.dma_start(out=H2r_v[rows, r, :], in_=h2b[s][:]) for r in range(4)], [b_h2b[s]], [b_H2r])

        def stage_b(t):
            s = t % 2
            c = t // TC

            def tr32():
                for kc in range(8):
                    last = T.transpose(pT32[:, kc * 128:(kc + 1) * 128], h2f[s][:, kc * 128:(kc + 1) * 128], ident_f[:])
                return last
            K.op(PE, tr32, [b_h2f[s], b_ident_f], [b_pT32])
            K.op(ACT, lambda: A.copy(out=h2T[:].rearrange("p k n -> p (k n)"), in_=pT32[:]), [b_pT32], [b_h2T])

            def mmr():
                for kc in range(8):
                    T.matmul(pl[:], lhsT=h2T[:, kc, :], rhs=wrt[:, kc, :], start=(kc == 0), stop=False)
                return T.matmul(pl[:], lhsT=ones_f[0:1, :], rhs=brt[0:1, :], start=False, stop=True)
            K.op(PE, mmr, [b_h2T, b_wrt, b_ones_f, b_brt], [b_pl])
            K.op(DVE, lambda: V.tensor_copy(out=lg_all[:, t, :], in_=pl[:]), [b_pl], [b_lg[c]])
            K.op(DVE, lambda: V.max(out=top8_all[:, t, :], in_=lg_all[:, t, :]), [b_lg[c]], [b_top8[c]])

        load_x(0)
        stage_a(0)
        pending = None
        for t in range(NT):
            if stage == "F1":
                if t + 1 < NT:
                    stage_a(t + 1)
                continue
            streams = []
            if t + 1 < NT:
                streams.append(K.record(lambda: stage_a(t + 1)))
            streams.append(K.record(lambda: stage_b(t)))
            if pending is not None:
                streams.append(pending)
                pending = None
            K.play(*streams)
            if t % TC == TC - 1:
                pending = K.record(lambda: route_chunk(t // TC))
        if pending is not None:
            K.play(pending)
        if stage == "F1":
            return finish()

        def mmc():
            for t in range(NT):
                last = T.matmul(pcnt, lhsT=ones_b[:, 0:1], rhs=mask_all[:, t, :], start=(t == 0), stop=(t == NT - 1))
            return last
        K.op(PE, mmc, [b_ones_b] + b_mask, [b_pcnt])
        K.op(DVE, lambda: V.tensor_copy(out=cntf[:], in_=pcnt), [b_pcnt], [b_cntf])
        K.op(DVE, lambda: V.tensor_copy(out=cnt_i[:], in_=cntf[:]), [b_cntf], [b_cnt])
        if stage == "F":
            K.barrier()
            K.op(DVE, lambda: V.tensor_copy(out=h2f[0][:, 0:4 * NT], in_=IDX[:].rearrange("p a b -> p (a b)")), [b_IDX], [b_h2f[0]])
            dbg_dump(h2f[0][:, 0:4 * NT], 128, 4 * NT, b_h2f[0], row0=0)
            dbg_dump(G_all[:].rearrange("p a b -> p (a b)"), 128, 4 * NT, b_G, row0=128)
            dbg_dump(cntf[:], 1, NE, b_cntf, row0=256)
            return finish()
        K.barrier()

    catA_stack.close()
    es_mix.close()
    cat_stack.close()
    K.barrier()
    mod_stack.close()
    with ExitStack() as es:
        NW = 3
        w1b = [sb(es, f"w1b{i}", [128, 8, 2 * D], BF) for i in range(NW)]
        b_w1b = [[K.buf(f"w1b{i}")] for i in range(NW)]
        w2b = [sb(es, f"w2b{i}", [128, 8, D], BF) for i in range(NW)]
        b_w2b = [[K.buf(f"w2b{i}")] for i in range(NW)]
        b1all = sb(es, "b1all", [96, 2 * D], BF); b_b1b = [K.buf(f"b1b{i}") for i in range(NW)]
        b2all = sb(es, "b2all", [96, D], BF); b_b2b = [K.buf(f"b2b{i}") for i in range(NW)]
        NS = 4
        xe = [[sb(es, f"xe{i}{b}", [128, D], BF) for b in range(2)] for i in range(NS)]
        b_xe = [[K.buf(f"xe{i}{b}") for b in range(2)] for i in range(NS)]
        tb = [[sb(es, f"tb{i}{b}", [128, 1], I32) for b in range(2)] for i in range(NS)]
        b_tb = [[K.buf(f"tb{i}{b}") for b in range(2)] for i in range(NS)]
        for i in range(NS):
            for b in range(2):
                K.op(DVE, lambda i=i, b=b: V.memset(xe[i][b][:], 0.0), writes=[b_xe[i][b]])
        xT2 = [[sb(es, f"xT{i}{b}", [128, 8, 128], BF) for b in range(2)] for i in range(2)]
        b_xT2 = [[K.buf(f"xT{i}{b}") for b in range(2)] for i in range(2)]
        xT = [xT2[i % 2] for i in range(NS)]
        b_xT = [b_xT2[i % 2] for i in range(NS)]
        glu1 = sb(es, "glu", [128, 512]); glu = [glu1, glu1]; b_glu1 = K.buf("glu"); b_glu = [b_glu1, b_glu1]
        sg1 = sb(es, "sg", [128, 512]); sg = [sg1, sg1]; b_sg1 = K.buf("sg"); b_sg = [b_sg1, b_sg1]
        lin1 = sb(es, "lin", [128, 512]); lin = [lin1, lin1]; b_lin1 = K.buf("lin"); b_lin = [b_lin1, b_lin1]
        actb = [sb(es, f"actb{b}", [128, D], BF) for b in range(2)]; b_actb = [K.buf(f"actb{b}") for b in range(2)]
        aT1 = sb(es, "aT", [128, 8, 128], BF); aT = [aT1, aT1]; b_aT1 = K.buf("aT"); b_aT = [b_aT1, b_aT1]
        yo = [sb(es, f"yo{i}", [128, D]) for i in range(2)]; b_yo = [K.buf(f"yo{i}") for i in range(2)]
        pTx = ps(es, "pTx", [128, D], BF); b_pTx = K.pbuf("pTx")
        pgu = [ps(es, f"pgu{i}", [128, D]) for i in range(2)]; b_pgu = [K.pbuf(f"pgu{i}") for i in range(2)]
        pTa = ps(es, "pTa", [128, D], BF); b_pTa = K.pbuf("pTa")
        py = ps(es, "py", [128, D]); b_py = K.pbuf("py")
        NPAIR = nbmax // 2
        GRP = 4

        def load_w(e):
            s = e % NW
            v1 = w1_d[e].rearrange("(kc p) n -> p kc n", p=128)
            v2 = w2_d[e].rearrange("(kc p) n -> p kc n", p=128)
            K.dma(POOL, lambda: [G.dma_start(out=w1b[s][:, kc, :], in_=v1[:, kc, :]) for kc in range(8)], writes=[b_w1b[s][0]])
            K.dma(POOL, lambda: [G.dma_start(out=w2b[s][:, kc, :], in_=v2[:, kc, :]) for kc in range(8)], writes=[b_w2b[s][0]])
            K.dma(POOL, lambda: G.dma_start(out=b1all[32 * s:32 * s + 1, :], in_=b1_d[e:e + 1, :]), writes=[b_b1b[s]])
            K.dma(POOL, lambda: G.dma_start(out=b2all[32 * s:32 * s + 1, :], in_=b2_d[e:e + 1, :]), writes=[b_b2b[s]])

        bcreg = G.alloc_register("bcreg")
        G.reg_mov(bcreg, S * 4 - 1)

        def load_pair(e, pr, s):
            r0 = e * CAP + pr * 256
            for b in range(2):
                K.dma(SP, lambda b=b: Q.dma_start(out=tb[s][b][:], in_=Tab_d[r0 + b * 128:r0 + (b + 1) * 128, :]), [b_Tab], [b_tb[s][b]])
            for b in range(2):
                K.dma(POOL, lambda b=b: G.indirect_dma_start(out=xe[s][b][:], out_offset=None, in_=H2r_d,
                                                             in_offset=IOA(ap=tb[s][b][:, 0:1], axis=0),
                                                             bounds_check=bcreg, oob_is_err=False),
                      [b_H2r, b_tb[s][b]], [b_xe[s][b]])

        ei = [0]

        def slot_of(e, pr):
            return 2 * (e % 2) + (pr % 2)

        def p1(e, pr):
            s = slot_of(e, pr)
            for b in range(2):
                def trx(b=b):
                    for kc in range(8):
                        last = T.transpose(pTx[:, kc * 128:(kc + 1) * 128], xe[s][b][:, kc * 128:(kc + 1) * 128], ident_b[:])
                    return last
                K.op(PE, trx, [b_xe[s][b], b_ident_b], [b_pTx])
                K.op(ACT, lambda b=b: A.copy(out=xT[s][b][:].rearrange("p k n -> p (k n)"), in_=pTx[:]), [b_pTx], [b_xT[s][b]])

        def pair_body(e, pr, s, ws):
            if pr + 1 < NPAIR:
                load_pair(e, pr + 1, slot_of(e, pr + 1))
            def stage2(b, h):
                def mm1():
                    for n, col in ((0, h * 512), (1, D + h * 512)):
                        for kc in range(8):
                            T.matmul(pgu[h][:, n * 512:(n + 1) * 512], lhsT=xT[s][b][:, kc, :], rhs=w1b[ws][:, kc, col:col + 512],
                                     start=(kc == 0), stop=False)
                        last = T.matmul(pgu[h][:, n * 512:(n + 1) * 512], lhsT=ones_b[32 * ws:32 * ws + 1, :], rhs=b1all[32 * ws:32 * ws + 1, col:col + 512],
                                        start=False, stop=True)
                    return last
                K.op(PE, mm1, [b_xT[s][b], b_b1b[ws], b_ones_b] + b_w1b[ws], [b_pgu[h]])
                r = ei[0] % 2
                ei[0] += 1
                K.op(DVE, lambda: V.tensor_scalar(out=glu[r][:], in0=pgu[h][:, 0:512], scalar1=7.0, scalar2=None, op0=ALU.min),
                     [b_pgu[h]], [b_glu[r]])
                K.op(ACT, lambda: A.activation(out=sg[r][:], in_=glu[r][:], func=AF.Sigmoid, scale=1.702), [b_glu[r]], [b_sg[r]])
                K.op(DVE, lambda: V.tensor_scalar(out=lin[r][:], in0=pgu[h][:, 512:1024], scalar1=-7.0, scalar2=7.0,
                                                  op0=ALU.max, op1=ALU.min), [b_pgu[h]], [b_lin[r]])
                K.op(DVE, lambda: V.scalar_tensor_tensor(out=lin[r][:], in0=lin[r][:], scalar=1.0, in1=glu[r][:],
                                                         op0=ALU.add, op1=ALU.mult), [b_glu[r]], [b_lin[r]])
                K.op(DVE, lambda: V.tensor_tensor(out=actb[b][:, h * 512:(h + 1) * 512], in0=lin[r][:], in1=sg[r][:], op=ALU.mult),
                     [b_lin[r], b_sg[r]], [b_actb[b]])

            def stage3(b):
                def tra():
                    for kc in range(8):
                        last = T.transpose(pTa[:, kc * 128:(kc + 1) * 128], actb[b][:, kc * 128:(kc + 1) * 128], ident_b[:])
                    return last
                K.op(PE, tra, [b_actb[b], b_ident_b], [b_pTa])
                K.op(ACT, lambda: A.copy(out=aT[b][:].rearrange("p k n -> p (k n)"), in_=pTa[:]), [b_pTa], [b_aT[b]])

            def stage4(b):
                def mm2():
                    for n in range(2):
                        for kc in range(8):
                            T.matmul(py[:, n * 512:(n + 1) * 512], lhsT=aT[b][:, kc, :], rhs=w2b[ws][:, kc, n * 512:(n + 1) * 512],
                                     start=(kc == 0), stop=False)
                        last = T.matmul(py[:, n * 512:(n + 1) * 512], lhsT=ones_b[32 * ws:32 * ws + 1, :], rhs=b2all[32 * ws:32 * ws + 1, n * 512:(n + 1) * 512],
                                        start=False, stop=True)
                    return last
                K.op(PE, mm2, [b_aT[b], b_b2b[ws], b_ones_b] + b_w2b[ws], [b_py])
                K.op(ACT, lambda: A.copy(out=yo[b][:], in_=py[:]), [b_py], [b_yo[b]])
                K.dma(POOL, lambda: G.indirect_dma_start(out=Yb_d, out_offset=IOA(ap=tb[s][b][:, 0:1], axis=0),
                                                         in_=yo[b][:], in_offset=None,
                                                         bounds_check=bcreg, oob_is_err=False),
                      [b_yo[b], b_tb[s][b]], [b_Yb])

            stage2(0, 0)
            stage2(0, 1)
            stage2(1, 0)
            stage3(0)
            stage2(1, 1)
            stage4(0)
            stage3(1)
            stage4(1)
            if pr + 1 < NPAIR:
                p1(e, pr + 1)

        if use_if:
            regs = nc.alloc_registers("cntreg")
        load_w(0)
        load_w(1)
        load_pair(0, 0, slot_of(0, 0))
        p1(0, 0)
        for e in range(NE):
            ws = e % NW
            if e + 1 < NE:
                load_pair(e + 1, 0, slot_of(e + 1, 0))
            if e + 2 < NE:
                load_w(e + 2)
            if use_if:
                for eng in K.engs:
                    K.wait_buf(eng, b_cnt)
                for reg in regs:
                    nc.reg_load(reg, cnt_i[0:1, e:e + 1])
            pair_body(e, 0, slot_of(e, 0), ws)

            def chain(pr):
                if pr >= NPAIR:
                    return
                snap = K.snapshot()
                ctx = nc.If_cmp(regs, 256 * pr, "IS_GT") if use_if else ExitStack()
                with ctx:
                    pair_body(e, pr, slot_of(e, pr), ws)
                    chain(pr + 1)
                if use_if:
                    with nc.Else():
                        K.compensate(snap)
                    K.restore_seen(snap)
            chain(1)
            if e + 1 < NE:
                p1(e + 1, 0)
        K.barrier()

    with ExitStack() as es:
        NR = 3
        x1t = [sb(es, f"x1t{i}", [128, D]) for i in range(NR)]; b_x1t = [K.buf(f"x1t{i}") for i in range(NR)]
        yg = [sb(es, f"yg{i}", [128, 4, D]) for i in range(NR)]; b_yg = [K.buf(f"yg{i}") for i in range(NR)]
        Yb_v = Yb_d.rearrange("(n r) d -> n (r d)", r=4)
        acc = [sb(es, f"acc{i}", [128, D]) for i in range(2)]; b_acc = [K.buf(f"acc{i}") for i in range(2)]

        def loads(t):
            s = t % NR
            rows = slice(t * 128, (t + 1) * 128)
            K.dma(SP, lambda: Q.dma_start(out=yg[s][:].rearrange("p r d -> p (r d)"), in_=Yb_v[rows, :]), [b_Yb], [b_yg[s]])
            K.dma(SP, lambda: Q.dma_start(out=x1t[s][:], in_=X1_d[rows, :]), [b_X1], [b_x1t[s]])
        loads(0)
        loads(1)
        for t in range(NT):
            s = t % NR
            a = t % 2
            rows = slice(t * 128, (t + 1) * 128)
            if t + 2 < NT:
                loads(t + 2)
            K.op(DVE, lambda: V.tensor_scalar(out=acc[a][:], in0=yg[s][:, 0, :], scalar1=G_all[:, t, 0:1], scalar2=None, op0=ALU.mult),
                 [b_yg[s], b_G], [b_acc[a]])
            for r in range(1, 4):
                K.op(DVE, lambda r=r: V.scalar_tensor_tensor(out=acc[a][:], in0=yg[s][:, r, :], scalar=G_all[:, t, r:r + 1],
                                                             in1=acc[a][:], op0=ALU.mult, op1=ALU.add),
                     [b_yg[s], b_G], [b_acc[a]])
            K.op(DVE, lambda: V.tensor_tensor(out=acc[a][:], in0=acc[a][:], in1=gate2k[:], op=ALU.mult), [b_gate2k], [b_acc[a]])
            K.op(DVE, lambda: V.tensor_tensor(out=acc[a][:], in0=acc[a][:], in1=x1t[s][:], op=ALU.add), [b_x1t[s]], [b_acc[a]])
            K.dma(ACT, lambda: A.dma_start(out=out_d[rows, :], in_=acc[a][:]), [b_acc[a]], [b_out])
    return finish()


def prep_inputs(inp):
    f = np.float32
    L = 0
    g = lambda n: np.asarray(inp[n][L])
    w_in = g("w_in")
    kro = w_in[:, 1408:1440]
    perm = np.concatenate([np.arange(16, 32), np.arange(0, 16)])
    w_in_ext = np.ascontiguousarray(np.concatenate([w_in[:, :1408], kro[:, perm], kro], axis=1), dtype=f)
    fm = lambda v: np.ascontiguousarray(np.asarray(v, dtype=f).reshape(-1, 128).T)
    cw = g("conv_w")
    fields = [cw[0], cw[1], cw[2], cw[3], g("conv_b"), g("b_a"), g("b_x"), g("lam")]
    lruv = np.ascontiguousarray(np.stack([fm(v) for v in fields], axis=-1), dtype=f)

    def bd(w):
        o = np.zeros((4, 128, 128), f)
        for c in range(4):
            o[c, 0:64, 0:64] = w[2 * c]
            o[c, 64:128, 64:128] = w[2 * c + 1]
        return o
    w_uq = g("w_uq"); w_ukv = g("w_ukv")
    w_uq_h = np.zeros((8, 256, 128), f)
    w_uk_h = np.zeros((8, 128, 128), f)
    w_uv = np.zeros((128, 512), f)
    for h in range(8):
        nope = w_uq[:, h * 96:h * 96 + 64]; rope = w_uq[:, h * 96 + 64:h * 96 + 96]
        w_uq_h[h] = np.concatenate([rope[:, perm], rope, nope], axis=1)
        w_uk_h[h, :, 64:128] = w_ukv[:, h * 128:h * 128 + 64]
        w_uv[:, h * 64:(h + 1) * 64] = w_ukv[:, h * 128 + 64:h * 128 + 128]

    def grow(gv):
        return np.ascontiguousarray(np.concatenate([gv[64:96][perm], gv[64:96], gv[0:64]]).reshape(128, 1), dtype=f)
    half = 16
    freqs = (10000.0 ** (-np.arange(half, dtype=np.float64) / half)).astype(f)
    fr32 = np.concatenate([freqs, freqs])
    rtab = np.zeros((64, 3), f)
    rtab[0:32, 0] = fr32; rtab[32:64, 0] = fr32
    rtab[0:32, 1] = 0.0; rtab[32:64, 1] = math.pi / 2
    rtab[0:16, 2] = -1.0; rtab[16:32, 2] = 1.0; rtab[32:64, 2] = 1.0
    shared = {
        "w_ada": g("w_ada"), "b_ada": g("b_ada").reshape(1, -1),
        "g_mix_bc": np.ascontiguousarray(np.broadcast_to(g("g_mix")[None, :], (128, D))),
        "g_ffn_bc": np.ascontiguousarray(np.broadcast_to(g("g_ffn")[None, :], (128, D))),
        "w_in_ext": w_in_ext, "lruv": lruv, "wa_bd": bd(g("w_a")), "wx_bd": bd(g("w_x")),
        "g_q_lat": fm(g("g_q_lat")), "g_kv_lat": fm(g("g_kv_lat")),
        "w_uq_h": w_uq_h, "w_uk_h": w_uk_h, "w_uv": w_uv, "gq": grow(g("g_qn")), "gk": grow(g("g_kn")),
        "rtab": rtab, "w_out": g("w_out"), "w_router": g("w_router"), "b_router": g("b_router").reshape(1, -1),
        "w1": g("w1"), "b1": g("b1"), "w2": g("w2"), "b2": g("b2"),
    }
    shared = {k: np.ascontiguousarray(v, dtype=f) for k, v in shared.items()}
    maps = []
    for b in range(8):
        m = dict(shared)
        m["x"] = np.ascontiguousarray(inp["x"][b], dtype=f)
        m["cT"] = fm(np.asarray(inp["c"][b]))
        m["pos"] = np.ascontiguousarray(np.broadcast_to(np.asarray(inp["positions"][b], dtype=np.int32)[None, :], (64, S)))
        maps.append(m)
    return maps


def kernel(**inputs):
    maps = prep_inputs(inputs)
    nc = build_nc("full")
    res = run_bass_kernel_spmd(nc, maps, core_ids=list(range(8)))
    return np.stack([np.asarray(r["out"], dtype=np.float32) for r in res.results], axis=0)
```

```python
import math
from contextlib import ExitStack
import numpy as np
import concourse.bass as bass
import concourse.mybir as mybir
from concourse.bass_utils import run_bass_kernel_spmd

F32 = mybir.dt.float32
BF = mybir.dt.bfloat16
I32 = mybir.dt.int32
AF = mybir.ActivationFunctionType
ALU = mybir.AluOpType
IOA = bass.IndirectOffsetOnAxis

S = 4096
D = 1024
NT = S // 128
NE = 32
CAP = 4096
NBMAX = CAP // 128
EPS = 1e-6
TWO_PI = 2.0 * math.pi


class Dep:
    def __init__(self, nc, name):
        self.sem = nc.alloc_semaphore(name)
        self.n = 0
        self.issuer = None


class Buf:
    def __init__(self, name):
        self.name = name
        self.w = None
        self.r = {}
        self.dsem = None
        self.psum = False


class Eng:
    def __init__(self, k, name, ins, has_dep=True):
        self.name = name
        self.ins = ins
        self.dep = Dep(k.nc, "c_" + name) if has_dep else None
        self.seen = {}


class Kern:
    def __init__(self, nc):
        self.nc = nc
        self.pe = Eng(self, "pe", nc.tensor)
        self.act = Eng(self, "act", nc.scalar)
        self.dve = Eng(self, "dve", nc.vector)
        self.pool = Eng(self, "pool", nc.gpsimd)
        self.sp = Eng(self, "sp", nc.sync, has_dep=False)
        self.engs = [self.pe, self.act, self.dve, self.pool, self.sp]
        self.deps = [e.dep for e in self.engs if e.dep is not None]
        self.nbuf = 0

    def buf(self, name=None):
        self.nbuf += 1
        return Buf(name or f"b{self.nbuf}")

    def pbuf(self, name):
        b = Buf(name)
        b.psum = True
        return b

    def _wait(self, eng, dep, val):
        if dep is eng.dep and eng is self.pe:
            return
        if eng.seen.get(dep, 0) < val:
            eng.ins.wait_ge(dep.sem, val)
            eng.seen[dep] = val

    def _deps_for(self, eng, reads, writes):
        for b in reads:
            if b.w is not None:
                self._wait(eng, *b.w)
        for b in writes:
            if b.w is not None:
                self._wait(eng, *b.w)
            for d, v in b.r.items():
                self._wait(eng, d, v)

    def _commit(self, tok, reads, writes):
        for b in writes:
            b.w = tok
            b.r = {}
        for b in reads:
            if b in writes:
                continue
            d, v = tok
            if b.r.get(d, 0) < v:
                b.r[d] = v

    def record(self, thunk):
        self.rec = []
        thunk()
        r, self.rec = self.rec, None
        return r

    def play(self, *streams):
        idx = [0] * len(streams)
        total = max(len(st) for st in streams) if streams else 0
        for step in range(total):
            for k, st in enumerate(streams):
                upto = (step + 1) * len(st) // total
                while idx[k] < upto:
                    kind, args = st[idx[k]]
                    idx[k] += 1
                    (self.op if kind == "op" else self.dma)(*args)

    def op(self, eng, fn, reads=(), writes=()):
        if getattr(self, "rec", None) is not None:
            self.rec.append(("op", (eng, fn, list(reads), list(writes))))
            return
        reads = list(reads)
        writes = list(writes) + [b for b in reads if b.psum and b not in writes]
        self._deps_for(eng, reads, writes)
        ins = fn()
        d = eng.dep
        d.n += 1
        ins.then_inc(d.sem, 1)
        self._commit((d, d.n), reads, writes)

    def dma(self, eng, fn, reads=(), writes=(), on=None):
        if getattr(self, "rec", None) is not None:
            self.rec.append(("dma", (eng, fn, list(reads), list(writes), on)))
            return
        self._deps_for(eng, reads, writes)
        b = on if on is not None else (writes[0] if writes else reads[0])
        if b.dsem is None:
            b.dsem = Dep(self.nc, "d_" + b.name)
            self.deps.append(b.dsem)
        d = b.dsem
        assert d.issuer in (None, eng), (b.name, d.issuer.name, eng.name)
        d.issuer = eng
        ins = fn()
        for i1 in (ins if isinstance(ins, (list, tuple)) else [ins]):
            d.n += 16
            i1.then_inc(d.sem, 16)
        self._commit((d, d.n), reads, writes)

    def wait_buf(self, eng, b):
        self._deps_for(eng, [b], [])

    def barrier(self):
        for e in self.engs:
            for d in self.deps:
                if d.n > 0:
                    self._wait(e, d, d.n)

    def snapshot(self):
        return ({d: d.n for d in self.deps}, {e: dict(e.seen) for e in self.engs})

    def compensate(self, snap):
        before, _ = snap
        for d in self.deps:
            b4 = before.get(d, 0)
            delta = d.n - b4
            if delta <= 0:
                continue
            eng = d.issuer
            if eng is None:
                eng = [e for e in self.engs if e.dep is d][0]
            eng.ins.wait_ge(d.sem, b4)
            eng.ins.sem_inc(d.sem, delta)

    def restore_seen(self, snap):
        _, seen = snap
        for e in self.engs:
            e.seen = dict(seen[e])


def build_nc(stage="full", use_if=True, nbmax=NBMAX):
    nc = bass.Bass("TRN2", target_bir_lowering=False)
    K = Kern(nc)
    PE, ACT, DVE, POOL, SP = K.pe, K.act, K.dve, K.pool, K.sp
    T, A, V, G, Q = nc.tensor, nc.scalar, nc.vector, nc.gpsimd, nc.sync

    def din(name, shape, dt=F32):
        return nc.dram_tensor(name, list(shape), dt, kind="ExternalInput").ap()

    x_d = din("x", [S, D])
    cT_d = din("cT", [128, 8])
    pos_d = din("pos", [64, S], I32)
    wada_d = din("w_ada", [D, 6 * D])
    bada_d = din("b_ada", [1, 6 * D])
    gmix_d = din("g_mix_bc", [128, D])
    gffn_d = din("g_ffn_bc", [128, D])
    win_d = din("w_in_ext", [D, 1472])
    lruv_d = din("lruv", [128, 4, 8])
    wabd_d = din("wa_bd", [4, 128, 128])
    wxbd_d = din("wx_bd", [4, 128, 128])
    gql_d = din("g_q_lat", [128, 2])
    gkvl_d = din("g_kv_lat", [128, 1])
    wuq_d = din("w_uq_h", [8, 256, 128])
    wuk_d = din("w_uk_h", [8, 128, 128])
    wuv_d = din("w_uv", [128, 512])
    gq_d = din("gq", [128, 1])
    gk_d = din("gk", [128, 1])
    rtab_d = din("rtab", [64, 3])
    wout_d = din("w_out", [D, D])
    wr_d = din("w_router", [D, NE])
    br_d = din("b_router", [1, NE])
    w1_d = din("w1", [NE, D, 2 * D])
    b1_d = din("b1", [NE, 2 * D])
    w2_d = din("w2", [NE, D, D])
    b2_d = din("b2", [NE, D])
    out_d = nc.dram_tensor("out", [S, D], F32, kind="ExternalOutput").ap()
    dbg_d = None
    if stage != "full":
        dbg_d = nc.dram_tensor("dbg", [S, D], F32, kind="ExternalOutput").ap()
    X1_d = nc.dram_tensor("X1s", [S, D], F32, kind="Internal").ap()
    H2r_d = nc.dram_tensor("H2r", [S * 4, D], BF, kind="Internal").ap()
    Yb_d = nc.dram_tensor("Ybs", [S * 4, D], F32, kind="Internal").ap()
    Tab_d = nc.dram_tensor("Tabs", [NE * CAP, 1], I32, kind="Internal").ap()
    b_out, b_dbg, b_X1, b_H2r, b_Yb, b_Tab = K.buf("out"), K.buf("dbg"), K.buf("X1"), K.buf("H2r"), K.buf("Yb"), K.buf("Tab")

    top = ExitStack()

    def sb(es, name, shape, dt=F32):
        return es.enter_context(nc.sbuf_tensor("s_" + name, list(shape), dt))

    def ps(es, name, shape, dt=F32):
        return es.enter_context(nc.psum_tensor("p_" + name, list(shape), dt))

    ident_b = sb(top, "ident_b", [128, 128], BF); b_ident_b = K.buf("identb")
    ident_f = sb(top, "ident_f", [128, 128], F32); b_ident_f = K.buf("identf")
    ones_b = sb(top, "ones_b", [128, 128], BF); b_ones_b = K.buf("onesb")
    ones_f = sb(top, "ones_f", [128, 128], F32); b_ones_f = K.buf("onesf")
    sel_b = sb(top, "sel_b", [128, 128], BF); b_sel = K.buf("sel")
    U_b = sb(top, "U_b", [128, 128], BF); b_U = K.buf("U")
    tri_b = sb(top, "tri_b", [128, 128], BF); b_tri = K.buf("tri")
    iof = sb(top, "iof", [128, 128], F32); b_iof = K.buf("iof")
    pidx = sb(top, "pidx", [128, 1], F32); b_pidx = K.buf("pidx")
    ebase = sb(top, "ebase", [128, NE], F32); b_ebase = K.buf("ebase")
    gate2k = sb(top, "gate2k", [128, D], F32); b_gate2k = K.buf("gate2k")
    G_all = sb(top, "G_all", [128, NT, 4], F32); b_G = K.buf("G_all")
    IDX = sb(top, "IDX", [128, NT, 4], I32); b_IDX = K.buf("IDX")
    cnt_i = sb(top, "cnt_i", [1, NE], I32); b_cnt = K.buf("cnt")
    mod_stack = ExitStack()
    mod = sb(mod_stack, "mod", [128, 5 * D], F32)
    b_mod = [K.buf(f"mod{i}") for i in range(6)]
    cat_stack = ExitStack()
    catL = sb(cat_stack, "catL", [128, 4, S], BF); b_cat = [K.buf(f"cat{i}") for i in range(8)]

    K.op(POOL, lambda: G.iota(iof[:], pattern=[[1, 128]], base=0, channel_multiplier=-1,
                              allow_small_or_imprecise_dtypes=True), writes=[b_iof])
    K.op(POOL, lambda: G.iota(pidx[:], pattern=[[0, 1]], base=0, channel_multiplier=1,
                              allow_small_or_imprecise_dtypes=True), writes=[b_pidx])
    K.op(POOL, lambda: G.iota(ebase[:], pattern=[[CAP, NE]], base=0, channel_multiplier=0,
                              allow_small_or_imprecise_dtypes=True), writes=[b_ebase])
    K.op(DVE, lambda: V.tensor_single_scalar(out=ident_b[:], in_=iof[:], scalar=0.0, op=ALU.is_equal), [b_iof], [b_ident_b])
    K.op(DVE, lambda: V.tensor_single_scalar(out=ident_f[:], in_=iof[:], scalar=0.0, op=ALU.is_equal), [b_iof], [b_ident_f])
    K.op(DVE, lambda: V.tensor_single_scalar(out=U_b[:], in_=iof[:], scalar=0.0, op=ALU.is_gt), [b_iof], [b_U])
    K.op(DVE, lambda: V.tensor_single_scalar(out=tri_b[:], in_=iof[:], scalar=0.0, op=ALU.is_ge), [b_iof], [b_tri])
    K.op(DVE, lambda: V.memset(ones_b[:], 1.0), writes=[b_ones_b])
    K.op(DVE, lambda: V.memset(ones_f[:], 1.0), writes=[b_ones_f])
    K.op(DVE, lambda: V.tensor_scalar(out=sel_b[:], in0=ones_f[:], scalar1=pidx[:, 0:1], scalar2=32.0,
                                      op0=ALU.mult, op1=ALU.is_ge), [b_ones_f, b_pidx], [b_sel])

    def dbg_dump(src_ap, rows, cols, b_src, row0=0, col0=0):
        K.dma(POOL, lambda: G.dma_start(out=dbg_d[row0:row0 + rows, col0:col0 + cols], in_=src_ap), [b_src], [b_dbg])

    def finish():
        K.barrier()
        for d in K.deps:
            if d.n > 0:
                Q.wait_ge(d.sem, d.n)
        pass
        return nc

    with ExitStack() as es:
        cT = sb(es, "cT", [128, 8]); b_cT = K.buf("cT")
        sc = sb(es, "sc", [128, 8]); b_sc = K.buf("sc")
        scb = sb(es, "scb", [128, 8, 128]); b_scb = K.buf("scb")
        bada = sb(es, "bada", [1, 6 * D]); b_bada = K.buf("bada")
        wst = [sb(es, f"wst{i}", [128, 8, 512]) for i in range(2)]; b_wst = [K.buf(f"wst{i}") for i in range(2)]
        pA = [ps(es, f"pA{i}", [128, 512]) for i in range(2)]; b_pA = [K.pbuf(f"pA{i}") for i in range(2)]
        gm = sb(es, "gm", [128, D]); b_gm = K.buf("gm")
        gf = sb(es, "gf", [128, D]); b_gf = K.buf("gf")
        K.dma(SP, lambda: Q.dma_start(out=cT[:], in_=cT_d), writes=[b_cT])
        K.dma(SP, lambda: Q.dma_start(out=bada[:], in_=bada_d), writes=[b_bada])
        K.dma(SP, lambda: Q.dma_start(out=gm[:], in_=gmix_d), writes=[b_gm])
        K.dma(SP, lambda: Q.dma_start(out=gf[:], in_=gffn_d), writes=[b_gf])
        K.op(ACT, lambda: A.activation(out=sc[:], in_=cT[:], func=AF.Silu), [b_cT], [b_sc])
        for kc in range(8):
            K.op(DVE, lambda kc=kc: V.tensor_scalar(out=scb[:, kc, :], in0=ones_f[:], scalar1=sc[:, kc:kc + 1],
                                                   scalar2=None, op0=ALU.mult), [b_sc, b_ones_f], [b_scb])
        wada_v = wada_d.rearrange("(kc p) n -> p kc n", p=128)
        for n in range(12):
            s = n % 2
            K.dma(SP, lambda n=n, s=s: Q.dma_start(out=wst[s][:], in_=wada_v[:, :, n * 512:(n + 1) * 512]), writes=[b_wst[s]])

            def mm(n=n, s=s):
                for kc in range(8):
                    T.matmul(pA[s][:], lhsT=scb[:, kc, :], rhs=wst[s][:, kc, :], start=(kc == 0), stop=False)
                return T.matmul(pA[s][:], lhsT=ones_f[0:1, :], rhs=bada[0:1, n * 512:(n + 1) * 512], start=False, stop=True)
            K.op(PE, mm, [b_scb, b_wst[s], b_ones_f, b_bada], [b_pA[s]])
            if n < 10:
                K.op(ACT, lambda n=n, s=s: A.copy(out=mod[:, n * 512:(n + 1) * 512], in_=pA[s][:]), [b_pA[s]], [b_mod[n // 2]])
            else:
                K.op(ACT, lambda n=n, s=s: A.copy(out=gate2k[:, (n - 10) * 512:(n - 9) * 512], in_=pA[s][:]), [b_pA[s]], [b_gate2k])
        K.op(DVE, lambda: V.scalar_tensor_tensor(out=mod[:, D:2 * D], in0=mod[:, D:2 * D], scalar=1.0, in1=gm[:],
                                                 op0=ALU.add, op1=ALU.mult), [b_gm], [b_mod[1]])
        K.op(DVE, lambda: V.scalar_tensor_tensor(out=mod[:, 4 * D:5 * D], in0=mod[:, 4 * D:5 * D], scalar=1.0, in1=gf[:],
                                                 op0=ALU.add, op1=ALU.mult), [b_gf], [b_mod[4]])
        if stage == "A":
            for i in range(5):
                dbg_dump(mod[0:1, i * D:(i + 1) * D], 1, D, b_mod[i], row0=i)
            dbg_dump(gate2k[0:1, :], 1, D, b_gate2k, row0=5)
            return finish()
        K.barrier()
    SH1, A1, GATE1, SH2, A2 = [mod[:, i * D:(i + 1) * D] for i in range(5)]

    def rms_rstd(ss_ap, n, out_ap, b_in, b_tmp, tmp_ap, b_out_):
        K.op(ACT, lambda: A.activation(out=tmp_ap, in_=ss_ap, func=AF.Ln, scale=1.0 / n, bias=EPS), [b_in], [b_tmp])
        K.op(ACT, lambda: A.activation(out=out_ap, in_=tmp_ap, func=AF.Exp, scale=-0.5), [b_tmp], [b_out_])

    es_mix = ExitStack()
    if True:
        qlatn = sb(es_mix, "qlatn", [128, 2, S], BF); b_qlatn = K.buf("qlatn")
        kvlatn = sb(es_mix, "kvlatn", [128, S], BF); b_kvlatn = K.buf("kvlatn")
        zr = sb(es_mix, "zr", [64, S], BF); b_zr = K.buf("zr")
        with ExitStack() as es:
            winb = sb(es, "winb", [128, 8, 1472], BF); b_winb = K.buf("winb")
            win_v = win_d.rearrange("(kc p) n -> p kc n", p=128)
            for kc in range(8):
                K.dma(POOL, lambda kc=kc: G.dma_start(out=winb[:, kc, :], in_=win_v[:, kc, :]), writes=[b_winb])
            lruv = sb(es, "lruv", [128, 4, 8]); b_lruv = K.buf("lruv")
            K.dma(SP, lambda: Q.dma_start(out=lruv[:], in_=lruv_d), writes=[b_lruv])
            nsp = sb(es, "nsp", [128, 4, 2]); b_nsp = K.buf("nsp")
            spt = sb(es, "spt", [128, 4]); b_spt = K.buf("spt")
            K.op(ACT, lambda: A.activation(out=spt[:], in_=lruv[:, :, 7], func=AF.Exp, scale=-1.0), [b_lruv], [b_spt])
            K.op(ACT, lambda: A.activation(out=spt[:], in_=spt[:], func=AF.Ln, bias=1.0), [b_spt], [b_spt])
            K.op(DVE, lambda: V.tensor_scalar(out=nsp[:, :, 0], in0=spt[:], scalar1=-8.0, scalar2=None, op0=ALU.mult), [b_spt], [b_nsp])
            K.op(DVE, lambda: V.tensor_scalar(out=nsp[:, :, 1], in0=spt[:], scalar1=-16.0, scalar2=None, op0=ALU.mult), [b_spt], [b_nsp])
            wab = sb(es, "wab", [128, 4, 128], BF); b_wab = K.buf("wab")
            wxb = sb(es, "wxb", [128, 4, 128], BF); b_wxb = K.buf("wxb")
            K.dma(POOL, lambda: G.dma_start(out=wab[:], in_=wabd_d.rearrange("c p n -> p c n")), writes=[b_wab])
            K.dma(POOL, lambda: G.dma_start(out=wxb[:], in_=wxbd_d.rearrange("c p n -> p c n")), writes=[b_wxb])
            gql = sb(es, "gql", [128, 2]); b_gql = K.buf("gql")
            gkvl = sb(es, "gkvl", [128, 1]); b_gkvl = K.buf("gkvl")
            K.dma(SP, lambda: Q.dma_start(out=gql[:], in_=gql_d), writes=[b_gql])
            K.dma(SP, lambda: Q.dma_start(out=gkvl[:], in_=gkvl_d), writes=[b_gkvl])
            hstate = sb(es, "hstate", [128, 4]); b_hst = K.buf("hstate")
            K.op(DVE, lambda: V.memset(hstate[:], 0.0), writes=[b_hst])
            xt = [sb(es, f"xt{i}", [128, D]) for i in range(2)]; b_xt = [K.buf(f"xt{i}") for i in range(2)]
            ss = sb(es, "ss", [128, 2]); b_ss = K.buf("ss")
            rstd = sb(es, "rstd", [128, 1]); b_rstd = K.buf("rstd")
            h1 = sb(es, "h1", [128, D]); b_h1 = K.buf("h1")
            hb = sb(es, "hb", [128, D], BF); b_hb = K.buf("hb")
            hT = sb(es, "hT", [128, 8, 512], BF); b_hT = K.buf("hT")
            xl = [[sb(es, f"xl{i}{c}", [128, 3 + 512]) for c in range(4)] for i in range(2)]
            b_xl = [[K.buf(f"xl{i}{c}") for c in range(4)] for i in range(2)]
            gy = [[sb(es, f"gy{i}{c}", [128, 512]) for c in range(4)] for i in range(2)]
            b_gy = [[K.buf(f"gy{i}{c}") for c in range(4)] for i in range(2)]
            sq = [sb(es, f"sq{i}", [128, 512], BF) for i in range(2)]; b_sq = [K.buf(f"sq{i}") for i in range(2)]
            qraw = [sb(es, f"qraw{i}", [128, 512]) for i in range(2)]; b_qraw = [K.buf(f"qraw{i}") for i in range(2)]
            rs = sb(es, "rs", [128, 512]); b_rs = K.buf("rs")
            rt2 = sb(es, "rt2", [128, 512]); b_rt2 = K.buf("rt2")
            xc_ = [sb(es, f"xc{i}", [128, 512]) for i in range(2)]; b_xc_ = [K.buf(f"xc{i}") for i in range(2)]
            xcb_ = [sb(es, f"xcb{i}", [128, 512], BF) for i in range(2)]; b_xcb_ = [K.buf(f"xcb{i}") for i in range(2)]
            rr_ = [sb(es, f"rr{i}", [128, 512]) for i in range(2)]; b_rr_ = [K.buf(f"rr{i}") for i in range(2)]
            ii_ = [sb(es, f"ii{i}", [128, 512]) for i in range(2)]; b_ii_ = [K.buf(f"ii{i}") for i in range(2)]
            aa_ = [sb(es, f"aa{i}", [128, 512]) for i in range(2)]; b_aa_ = [K.buf(f"aa{i}") for i in range(2)]
            mm__ = [sb(es, f"mm{i}", [128, 512]) for i in range(2)]; b_mm_ = [K.buf(f"mm{i}") for i in range(2)]
            pT = ps(es, "pT", [128, D], BF); b_pT = K.pbuf("pT")
            NZ = 2
            pz = [ps(es, f"pz{i}", [128, 512]) for i in range(NZ)]; b_pz = [K.pbuf(f"pz{i}") for i in range(NZ)]
            pg_ = [[ps(es, f"pg{i}{j}", [128, 512]) for j in range(2)] for i in range(2)]
            b_pg_ = [[K.pbuf(f"pg{i}{j}") for j in range(2)] for i in range(2)]
            pss = ps(es, "pss", [128, 512]); b_pss = K.pbuf("pss")
            for c in range(4):
                K.op(DVE, lambda c=c: V.memset(xl[0][c][:, 0:3], 0.0), writes=[b_xl[0][c]])
            zi = [0]

            def norm_unit(g, tt):
                t = g * 4 + tt
                s = t % 2
                K.dma(SP, lambda: Q.dma_start(out=xt[s][:], in_=x_d[t * 128:(t + 1) * 128, :]), writes=[b_xt[s]])
                K.op(ACT, lambda: A.activation(out=h1[:], in_=xt[s][:], func=AF.Square, accum_out=ss[:, 0:1]),
                     [b_xt[s]], [b_h1, b_ss])
                rms_rstd(ss[:, 0:1], D, rstd[:], b_ss, b_ss, ss[:, 1:2], b_rstd)
                K.op(DVE, lambda: V.scalar_tensor_tensor(out=h1[:], in0=xt[s][:], scalar=rstd[:, 0:1], in1=A1,
                                                         op0=ALU.mult, op1=ALU.mult), [b_xt[s], b_rstd, b_mod[1]], [b_h1])
                K.op(DVE, lambda: V.tensor_tensor(out=hb[:], in0=h1[:], in1=SH1, op=ALU.add), [b_h1, b_mod[0]], [b_hb])

                def tr():
                    for kc in range(8):
                        last = T.transpose(pT[:, kc * 128:(kc + 1) * 128], hb[:, kc * 128:(kc + 1) * 128], ident_b[:])
                    return last
                K.op(PE, tr, [b_hb, b_ident_b], [b_pT])
                K.op(ACT, lambda: A.copy(out=hT[:, :, tt * 128:(tt + 1) * 128],
                                         in_=pT[:].rearrange("p (k n) -> p k n", k=8)), [b_pT], [b_hT])

            def inproj_unit(g, ch):
                tsl = slice(g * 512, (g + 1) * 512)
                gp = g % 2
                z = zi[0] % NZ
                zi[0] += 1
                M = 128 if ch < 11 else 64

                def mmz():
                    for kc in range(8):
                        last = T.matmul(pz[z][0:M, :], lhsT=winb[:, kc, ch * 128:ch * 128 + M], rhs=hT[:, kc, :],
                                        start=(kc == 0), stop=(kc == 7))
                    return last
                K.op(PE, mmz, [b_winb, b_hT], [b_pz[z]])
                if ch < 4:
                    c = ch
                    if g > 0:
                        K.op(DVE, lambda: V.tensor_copy(out=xl[gp][c][:, 0:3], in_=xl[1 - gp][c][:, 512:515]), [b_xl[1 - gp][c]], [b_xl[gp][c]])
                    K.op(ACT, lambda: A.copy(out=xl[gp][c][:, 3:515], in_=pz[z][:]), [b_pz[z]], [b_xl[gp][c]])
                elif ch < 8:
                    c = ch - 4
                    K.op(ACT, lambda: A.activation(out=gy[gp][c][:], in_=pz[z][:], func=AF.Gelu), [b_pz[z]], [b_gy[gp][c]])
                elif ch < 11:
                    j = ch - 8 if ch < 10 else 0
                    gcol = gql[:, j:j + 1] if ch < 10 else gkvl[:, 0:1]
                    b_g = b_gql if ch < 10 else b_gkvl
                    K.op(ACT, lambda: A.activation(out=sq[j][:], in_=pz[z][:], func=AF.Square), [b_pz[z]], [b_sq[j]])
                    K.op(DVE, lambda: V.tensor_scalar(out=qraw[j][:], in0=pz[z][:], scalar1=gcol, scalar2=None,
                                                      op0=ALU.mult), [b_pz[z], b_g], [b_qraw[j]])
                    if ch == 9 or ch == 10:
                        nch = 2 if ch == 9 else 1

                        def mms():
                            for j2 in range(nch):
                                last = T.matmul(pss[:], lhsT=ones_b[:], rhs=sq[j2][:], start=(j2 == 0), stop=(j2 == nch - 1))
                            return last
                        K.op(PE, mms, [b_ones_b] + b_sq[:nch], [b_pss])
                        rms_rstd(pss[:], 128 * nch, rs[:], b_pss, b_rt2, rt2[:], b_rs)
                        for j2 in range(nch):
                            dst = qlatn[:, j2, tsl] if ch == 9 else kvlatn[:, tsl]
                            bd = b_qlatn if ch == 9 else b_kvlatn
                            K.op(DVE, lambda j2=j2, dst=dst: V.tensor_tensor(out=dst, in0=qraw[j2][:], in1=rs[:], op=ALU.mult),
                                 [b_qraw[j2], b_rs], [bd])
                else:
                    K.op(ACT, lambda: A.copy(out=zr[:, tsl], in_=pz[z][0:64, :]), [b_pz[z]], [b_zr])

            def lru_unit(g, c):
                tsl = slice(g * 512, (g + 1) * 512)
                gp = g % 2
                q_ = c % 2
                xc, xcb, rr, ii, aa, mm_, pg = xc_[q_], xcb_[q_], rr_[q_], ii_[q_], aa_[q_], mm__[q_], pg_[q_]
                b_xc, b_xcb, b_rr, b_ii, b_aa, b_mm, b_pg = b_xc_[q_], b_xcb_[q_], b_rr_[q_], b_ii_[q_], b_aa_[q_], b_mm_[q_], b_pg_[q_]
                lv = lambda f: lruv[:, c, f:f + 1]
                K.op(ACT, lambda: A.activation(out=xc[:], in_=xl[gp][c][:, 3:515], func=AF.Identity, scale=lv(3), bias=lv(4)),
                     [b_xl[gp][c], b_lruv], [b_xc])
                for j in range(3):
                    K.op(DVE, lambda j=j: V.scalar_tensor_tensor(out=xc[:], in0=xl[gp][c][:, j:j + 512], scalar=lv(j), in1=xc[:],
                                                                 op0=ALU.mult, op1=ALU.add), [b_xl[gp][c], b_lruv], [b_xc])
                K.op(ACT, lambda: A.copy(out=xcb[:], in_=xc[:]), [b_xc], [b_xcb])
                K.op(PE, lambda: T.matmul(pg[0][:], lhsT=wab[:, c, :], rhs=xcb[:], start=True, stop=True), [b_wab, b_xcb], [b_pg[0]])
                K.op(PE, lambda: T.matmul(pg[1][:], lhsT=wxb[:, c, :], rhs=xcb[:], start=True, stop=True), [b_wxb, b_xcb], [b_pg[1]])
                K.op(ACT, lambda: A.activation(out=rr[:], in_=pg[0][:], func=AF.Sigmoid, bias=lv(5)), [b_pg[0], b_lruv], [b_rr])
                K.op(ACT, lambda: A.activation(out=ii[:], in_=pg[1][:], func=AF.Sigmoid, bias=lv(6)), [b_pg[1], b_lruv], [b_ii])
                K.op(ACT, lambda: A.activation(out=aa[:], in_=rr[:], func=AF.Exp, scale=nsp[:, c, 0:1]), [b_rr, b_nsp], [b_aa])
                K.op(ACT, lambda: A.activation(out=mm_[:], in_=rr[:], func=AF.Exp, scale=nsp[:, c, 1:2]), [b_rr, b_nsp], [b_mm])
                K.op(ACT, lambda: A.activation(out=mm_[:], in_=mm_[:], func=AF.Sqrt, scale=-1.0, bias=1.0), [b_mm], [b_mm])
                K.op(DVE, lambda: V.tensor_tensor(out=ii[:], in0=ii[:], in1=xc[:], op=ALU.mult), [b_xc], [b_ii])
                K.op(DVE, lambda: V.tensor_tensor(out=ii[:], in0=ii[:], in1=mm_[:], op=ALU.mult), [b_mm], [b_ii])
                K.op(DVE, lambda: V.tensor_tensor_scan(out=rr[:], data0=aa[:], data1=ii[:], initial=hstate[:, c:c + 1],
                                                       op0=ALU.mult, op1=ALU.add), [b_aa, b_ii, b_hst], [b_rr])
                K.op(DVE, lambda: V.tensor_copy(out=hstate[:, c:c + 1], in_=rr[:, 511:512]), [b_rr], [b_hst])
                K.op(DVE, lambda: V.tensor_tensor(out=catL[:, c, tsl], in0=rr[:], in1=gy[gp][c][:], op=ALU.mult),
                     [b_rr, b_gy[gp][c]], [b_cat[c]])

            def x_units(g):
                return [lambda tt=tt: norm_unit(g, tt) for tt in range(4)] + [lambda ch=ch: inproj_unit(g, ch) for ch in range(12)]

            for u in x_units(0):
                u()
            if stage == "B":
                K.barrier()
                dbg_dump(hT[:, 0, :], 128, 512, b_hT)
                return finish()
            if stage == "C":
                K.barrier()
                dbg_dump(xl[0][0][:, 3:515], 128, 512, b_xl[0][0])
                dbg_dump(qlatn[:, 0, 0:512], 128, 512, b_qlatn, row0=128)
                return finish()
            for g in range(8):
                nx = x_units(g + 1) if g + 1 < 8 else []
                for c2 in range(2):
                    ra0 = K.record(lambda: lru_unit(g, 2 * c2))
                    ra1 = K.record(lambda: lru_unit(g, 2 * c2 + 1))
                    rb = K.record(lambda: [u() for u in nx[8 * c2:8 * c2 + 8]])
                    K.play(ra0, ra1, rb) if rb else K.play(ra0, ra1)
            if stage == "D":
                for c in range(4):
                    dbg_dump(catL[:, c, 0:1024], 128, 1024, b_cat[c], row0=c * 128)
                return finish()
            K.barrier()

        catA_stack = ExitStack()
        catA = sb(catA_stack, "catA", [128, 4, S], BF)
        with ExitStack() as es:
            wuqb = sb(es, "wuqb", [128, 2, 8, 128], BF); b_wuqb = K.buf("wuqb")
            wukb = sb(es, "wukb", [128, 8, 128], BF); b_wukb = K.buf("wukb")
            wuvb = sb(es, "wuvb", [128, 512], BF); b_wuvb = K.buf("wuvb")
            for h in range(8):
                K.dma(POOL, lambda h=h: G.dma_start(out=wuqb[:, :, h, :], in_=wuq_d[h].rearrange("(kc p) n -> p kc n", p=128)), writes=[b_wuqb])
            K.dma(POOL, lambda: G.dma_start(out=wukb[:], in_=wuk_d.rearrange("h p n -> p h n")), writes=[b_wukb])
            K.dma(POOL, lambda: G.dma_start(out=wuvb[:], in_=wuv_d), writes=[b_wuvb])
            gq = sb(es, "gq", [128, 1]); b_gq = K.buf("gq")
            gk = sb(es, "gk", [128, 1]); b_gk = K.buf("gk")
            rtab = sb(es, "rtab", [64, 3]); b_rtab = K.buf("rtab")
            K.dma(SP, lambda: Q.dma_start(out=gq[:], in_=gq_d), writes=[b_gq])
            K.dma(SP, lambda: Q.dma_start(out=gk[:], in_=gk_d), writes=[b_gk])
            K.dma(SP, lambda: Q.dma_start(out=rtab[:], in_=rtab_d), writes=[b_rtab])
            esel = ident_b
            Tb = sb(es, "Tb", [64, S], BF); b_Tb = K.buf("Tb")
            with ExitStack() as es2:
                CW = 1024
                posi = sb(es2, "posi", [64, CW], I32); b_posi = K.buf("posi")
                tt_ = sb(es2, "tt_", [64, CW]); b_tt = K.buf("tt")
                kf = sb(es2, "kf", [64, CW]); b_kf = K.buf("kf")
                ki = sb(es2, "ki", [64, CW], I32); b_ki = K.buf("ki")
                for cc in range(S // CW):
                    csl = slice(cc * CW, (cc + 1) * CW)
                    K.dma(SP, lambda csl=csl: Q.dma_start(out=posi[:], in_=pos_d[:, csl]), writes=[b_posi])
                    K.op(DVE, lambda: V.tensor_copy(out=tt_[:], in_=posi[:]), [b_posi], [b_tt])
                    K.op(DVE, lambda: V.tensor_scalar(out=tt_[:], in0=tt_[:], scalar1=rtab[:, 0:1], scalar2=rtab[:, 1:2],
                                                      op0=ALU.mult, op1=ALU.add), [b_rtab], [b_tt])
                    K.op(DVE, lambda: V.tensor_scalar(out=kf[:], in0=tt_[:], scalar1=1.0 / TWO_PI, scalar2=None, op0=ALU.mult), [b_tt], [b_kf])
                    K.op(DVE, lambda: V.tensor_copy(out=ki[:], in_=kf[:]), [b_kf], [b_ki])
                    K.op(DVE, lambda: V.tensor_copy(out=kf[:], in_=ki[:]), [b_ki], [b_kf])
                    K.op(DVE, lambda: V.scalar_tensor_tensor(out=tt_[:], in0=kf[:], scalar=-TWO_PI, in1=tt_[:], op0=ALU.mult, op1=ALU.add),
                         [b_kf], [b_tt])
                    K.op(DVE, lambda: V.tensor_scalar(out=kf[:], in0=tt_[:], scalar1=math.pi, scalar2=-TWO_PI, op0=ALU.is_gt, op1=ALU.mult),
                         [b_tt], [b_kf])
                    K.op(DVE, lambda: V.tensor_tensor(out=tt_[:], in0=tt_[:], in1=kf[:], op=ALU.add), [b_kf], [b_tt])
                    K.op(DVE, lambda: V.tensor_scalar(out=tt_[:], in0=tt_[:], scalar1=-math.pi, scalar2=math.pi, op0=ALU.max, op1=ALU.min),
                         [], [b_tt])
                    K.op(ACT, lambda csl=csl: A.activation(out=Tb[:, csl], in_=tt_[:], func=AF.Sin), [b_tt], [b_Tb])
                    K.op(DVE, lambda csl=csl: V.tensor_scalar(out=Tb[:, csl], in0=Tb[:, csl], scalar1=rtab[:, 2:3], scalar2=None, op0=ALU.mult),
                         [b_rtab], [b_Tb])
                K.barrier()
            qT = [sb(es, f"qT{i}", [128, S], BF) for i in range(2)]; b_qT = [K.buf(f"qT{i}") for i in range(2)]
            kT = [sb(es, f"kT{i}", [128, S], BF) for i in range(2)]; b_kT = [K.buf(f"kT{i}") for i in range(2)]
            Va = [sb(es, f"Va{i}", [128, NT, 128], BF) for i in range(2)]; b_Va = [K.buf(f"Va{i}") for i in range(2)]
            for i in range(2):
                K.op(POOL, lambda i=i: G.memset(Va[i][:, :, 64:128], 1.0), writes=[b_Va[i]])
                K.op(DVE, lambda i=i: V.memset(qT[i][0:32, :], 0.0), writes=[b_qT[i]])
                K.op(DVE, lambda i=i: V.memset(kT[i][0:32, :], 0.0), writes=[b_kT[i]])
            sqa = sb(es, "sqa", [128, 512], BF); b_sqa = K.buf("sqa")
            rsa = sb(es, "rsa", [128, 512]); b_rsa = K.buf("rsa")
            qn = sb(es, "qn", [128, 512]); b_qn = K.buf("qn")
            tmp2 = sb(es, "tmp2", [128, 512]); b_tmp2 = K.buf("tmp2")
            PT = [sb(es, f"PT{i}", [128, 2, 512], BF) for i in range(2)]; b_PT = [K.buf(f"PT{i}") for i in range(2)]
            rd = sb(es, "rd", [64, 512]); b_rd = K.buf("rd")
            pq = ps(es, "pq", [128, 512]); b_pq = K.pbuf("pq")
            pssa = ps(es, "pssa", [128, 512]); b_pssa = K.pbuf("pssa")
            pv = pssa[:].rearrange("p (a b) -> p a b", a=8); b_pv = b_pssa
            pS = [ps(es, f"pS{i}", [128, 2, 512]) for i in range(2)]; b_pS = [K.pbuf(f"pS{i}") for i in range(2)]
            pO = [ps(es, f"pO{i}", [128, 512]) for i in range(2)]; b_pO = [K.pbuf(f"pO{i}") for i in range(2)]
            SCALE = math.sqrt(96.0)
            S_REP = 1
            def prep_units(h):
                hb_ = h % 2
                phases = []
                for tg in range(4):
                    def uv_a(tg=tg):
                        def mmv():
                            for j in range(8):
                                t = tg * 8 + j
                                last = T.matmul(pv[:, j, :], lhsT=kvlatn[:, t * 128:(t + 1) * 128], rhs=wuvb[:, h * 64:(h + 1) * 64],
                                                start=True, stop=True)
                            return last
                        K.op(PE, mmv, [b_kvlatn, b_wuvb], [b_pv])

                    def uv_b(tg=tg):
                        K.op(ACT, lambda: A.copy(out=Va[hb_][:, tg * 8:(tg + 1) * 8, 0:64], in_=pv), [b_pv], [b_Va[hb_]])
                    phases += [uv_a, uv_b]
                for g in range(8):
                    for which in range(2):
                        tsl = slice(g * 512, (g + 1) * 512)
                        if which == 0:
                            gcol, b_gc, dstT, b_dst = gq, b_gq, qT[hb_], b_qT[hb_]
                        else:
                            gcol, b_gc, dstT, b_dst = gk, b_gk, kT[hb_], b_kT[hb_]

                        def ua(tsl=tsl, which=which):
                            if which == 0:
                                def mmq():
                                    T.matmul(pq[:], lhsT=wuqb[:, 0, h, :], rhs=qlatn[:, 0, tsl], start=True, stop=False)
                                    return T.matmul(pq[:], lhsT=wuqb[:, 1, h, :], rhs=qlatn[:, 1, tsl], start=False, stop=True)
                                K.op(PE, mmq, [b_wuqb, b_qlatn], [b_pq])
                            else:
                                def mmk():
                                    T.matmul(pq[:], lhsT=wukb[:, h, :], rhs=kvlatn[:, tsl], start=True, stop=False)
                                    return T.matmul(pq[:], lhsT=esel[0:64, :], rhs=zr[:, tsl], start=False, stop=True)
                                K.op(PE, mmk, [b_wukb, b_kvlatn, b_ident_b, b_zr], [b_pq])

                        def ub():
                            K.op(ACT, lambda: A.activation(out=sqa[:], in_=pq[:], func=AF.Square), [b_pq], [b_sqa])
                            K.op(PE, lambda: T.matmul(pssa[:], lhsT=sel_b[:], rhs=sqa[:], start=True, stop=True), [b_sel, b_sqa], [b_pssa])

                        def uc(tsl=tsl, gcol=gcol, b_gc=b_gc, dstT=dstT, b_dst=b_dst):
                            K.op(ACT, lambda: A.activation(out=tmp2[:], in_=pssa[:], func=AF.Ln, bias=96.0 * EPS), [b_pssa], [b_tmp2])
                            K.op(ACT, lambda: A.activation(out=rsa[:], in_=tmp2[:], func=AF.Exp, scale=-0.5), [b_tmp2], [b_rsa])
                            K.op(DVE, lambda: V.scalar_tensor_tensor(out=qn[:], in0=pq[:], scalar=gcol[:, 0:1], in1=rsa[:],
                                                                     op0=ALU.mult, op1=ALU.mult), [b_pq, b_gc, b_rsa], [b_qn])
                            K.op(DVE, lambda: V.tensor_tensor(out=qn[0:64, :], in0=qn[0:64, :], in1=Tb[:, tsl], op=ALU.mult), [b_Tb], [b_qn])
                            K.op(DVE, lambda: V.tensor_copy(out=tmp2[32:64, :], in_=qn[0:32, :]), [b_qn], [b_tmp2])
                            K.op(DVE, lambda: V.tensor_tensor(out=dstT[32:64, tsl], in0=qn[32:64, :], in1=tmp2[32:64, :], op=ALU.add),
                                 [b_qn, b_tmp2], [b_dst])
                            K.op(POOL, lambda: G.tensor_copy(out=dstT[64:128, tsl], in_=qn[64:128, :]), [b_qn], [b_dst])
                        phases += [ua, ub, uc]
                return phases

            for u in prep_units(0):
                u()
            if stage == "E0":
                K.barrier()
                dbg_dump(qT[0][:, 0:1024], 128, 1024, b_qT[0], row0=0)
                dbg_dump(kT[0][:, 0:1024], 128, 1024, b_kT[0], row0=128)
                dbg_dump(Va[0][:, 0:8, :].rearrange("p a b -> p (a b)"), 128, 1024, b_Va[0], row0=256)
                return finish()
            oi = 0
            for h in range(8):
                hb_ = h % 2
                nxt = prep_units(h + 1) if h + 1 < 8 else []
                steps = [(qg, kp) for qg in range(8) for kp in range(2 * qg + 2)]
                every = 1 if nxt else 0

                def geom(qg, kt):
                    n0 = 0 if kt < 4 * qg else 128 * (kt - 4 * qg)
                    return n0, 512 - n0

                def emit_S(i):
                    qg, kp = steps[i]
                    s = i % 2

                    def mmS():
                        for j in range(2):
                            kt = 2 * kp + j
                            n0, w = geom(qg, kt)
                            last = T.matmul(pS[s][:, j, 0:w], lhsT=kT[hb_][:, kt * 128:(kt + 1) * 128],
                                            rhs=qT[hb_][:, qg * 512 + n0:(qg + 1) * 512], start=True, stop=True)
                        return last
                    K.op(PE, mmS, [b_kT[hb_], b_qT[hb_]], [b_pS[s]])
                emit_S(0)
                o = oi % 2
                for i, (qg, kp) in enumerate(steps):
                    s = i % 2
                    nkt = 4 * qg + 4
                    diag = kp >= 2 * qg
                    if kp == 0:
                        o = oi % 2
                        oi += 1
                    if i + 1 < len(steps):
                        emit_S(i + 1)
                    if not diag:
                        K.op(ACT, lambda: A.activation(out=PT[s][:].rearrange("p a b -> p (a b)"), in_=pS[s][:].rearrange("p a b -> p (a b)"),
                                                       func=AF.Exp, scale=SCALE), [b_pS[s]], [b_PT[s]])
                    else:
                        for j in range(2):
                            n0, w = geom(qg, 2 * kp + j)
                            K.op(ACT, lambda j=j, w=w: A.activation(out=PT[s][:, j, 0:w], in_=pS[s][:, j, 0:w], func=AF.Exp, scale=SCALE),
                                 [b_pS[s]], [b_PT[s]])
                        for j in range(2):
                            K.op(DVE, lambda j=j: V.tensor_tensor(out=PT[s][:, j, 0:128], in0=PT[s][:, j, 0:128], in1=tri_b[:], op=ALU.mult),
                                 [b_tri], [b_PT[s]])

                    def mmPV():
                        for j in range(2):
                            kt = 2 * kp + j
                            n0, w = geom(qg, kt)
                            last = T.matmul(pO[o][:, n0:512], lhsT=Va[hb_][:, kt, :], rhs=PT[s][:, j, 0:w],
                                            start=(kt == 0), stop=(kt == nkt - 1))
                        return last
                    K.op(PE, mmPV, [b_Va[hb_], b_PT[s]], [b_pO[o]])
                    if kp == 2 * qg + 1:
                        K.op(DVE, lambda: V.reciprocal(out=rd[:], in_=pO[o][64:128, :]), [b_pO[o]], [b_rd])
                        ch = 4 + h // 2
                        p0 = (h % 2) * 64
                        K.op(DVE, lambda: V.tensor_tensor(out=catA[p0:p0 + 64, ch - 4, qg * 512:(qg + 1) * 512],
                                                          in0=pO[o][0:64, :], in1=rd[:], op=ALU.mult),
                             [b_pO[o], b_rd], [b_cat[ch]])
                    if nxt and every and i % every == every - 1 and (i // every) < len(nxt):
                        nxt[i // every]()
                for j in range((len(steps) // every) if every else 0, len(nxt)):
                    nxt[j]()
            if stage == "E":
                K.barrier()
                for c in range(4):
                    dbg_dump(catA[:, c, 0:1024], 128, 1024, b_cat[4 + c], row0=c * 128)
                return finish()
            K.barrier()

    with ExitStack() as es:
        wob = sb(es, "wob", [128, 8, D], BF); b_wob = K.buf("wob")
        wst2 = [sb(es, f"wst2{i}", [128, D]) for i in range(2)]; b_wst2 = [K.buf(f"wst2{i}") for i in range(2)]
        wout_v = wout_d.rearrange("(kc p) n -> p kc n", p=128)
        for kc in range(8):
            s = kc % 2
            K.dma(SP, lambda kc=kc, s=s: Q.dma_start(out=wst2[s][:], in_=wout_v[:, kc, :]), writes=[b_wst2[s]])
            K.op(DVE, lambda kc=kc, s=s: V.tensor_tensor(out=wob[:, kc, :], in0=wst2[s][:], in1=GATE1, op=ALU.mult),
                 [b_wst2[s], b_mod[2]], [b_wob])
        wrt = sb(es, "wrt", [128, 8, NE]); b_wrt = K.buf("wrt")
        brt = sb(es, "brt", [1, NE]); b_brt = K.buf("brt")
        K.dma(SP, lambda: Q.dma_start(out=wrt[:], in_=wr_d.rearrange("(kc p) n -> p kc n", p=128)), writes=[b_wrt])
        K.dma(SP, lambda: Q.dma_start(out=brt[:], in_=br_d), writes=[b_brt])
        mask_all = sb(es, "mask_all", [128, NT, NE], BF); b_mask = [K.buf(f"mask{t}") for t in range(NT)]
        VAL = sb(es, "VAL", [128, NT, 4], I32); b_VAL = K.buf("VAL")
        K.op(POOL, lambda: G.iota(VAL[:], pattern=[[512, NT], [1, 4]], base=0, channel_multiplier=4), writes=[b_VAL])
        tinit = sb(es, "tinit", [128, 1024], I32); b_tinit = K.buf("tinit")
        K.op(POOL, lambda: G.iota(tinit[:], pattern=[[0, 1024]], base=1 << 30, channel_multiplier=0), writes=[b_tinit])
        K.dma(POOL, lambda: G.dma_start(out=Tab_d.rearrange("(p n) o -> p (n o)", p=128), in_=tinit[:]), [b_tinit], [b_Tab])
        H2r_v = H2r_d.rearrange("(n r) d -> n r d", r=4)
        TC = 8
        xt = [sb(es, f"xtf{i}", [128, D]) for i in range(2)]; b_xt = [K.buf(f"xtf{i}") for i in range(2)]
        x1 = [sb(es, f"x1{i}", [128, D]) for i in range(2)]; b_x1 = [K.buf(f"x1{i}") for i in range(2)]
        junk = sb(es, "junkf", [128, D], BF); b_junk = K.buf("junkf")
        ss = sb(es, "ssf", [128, 2]); b_ss = K.buf("ssf")
        rstd = sb(es, "rstdf", [128, 1]); b_rstd = K.buf("rstdf")
        h2f = [sb(es, f"h2f{i}", [128, D]) for i in range(2)]; b_h2f = [K.buf(f"h2f{i}") for i in range(2)]
        h2b = [sb(es, f"h2b{i}", [128, D], BF) for i in range(2)]; b_h2b = [K.buf(f"h2b{i}") for i in range(2)]
        h2T = sb(es, "h2T", [128, 8, 128]); b_h2T = K.buf("h2T")
        lg_all = sb(es, "lg_all", [128, NT, NE]); b_lg = [K.buf(f"lg{c}") for c in range(NT // TC)]
        top8_all = sb(es, "top8_all", [128, NT, 8]); b_top8 = [K.buf(f"top8{c}") for c in range(NT // TC)]
        d4 = sb(es, "d4", [128, TC, 4]); b_d4 = K.buf("d4")
        den = sb(es, "den", [128, TC]); b_den = K.buf("den")
        oh = sb(es, "oh", [128, 4, TC, NE]); b_oh = K.buf("oh")
        mk = sb(es, "mk", [128, TC, NE]); b_mk = K.buf("mk")
        idxfull = sb(es, "idxfull", [128, TC, NE]); b_idxf = K.buf("idxf")
        junk2 = sb(es, "junk2", [128, TC, NE]); b_junk2 = K.buf("junk2")
        IDXf = sb(es, "IDXf", [128, NT, 4]); b_idx4 = K.buf("idx4")
        cntf = sb(es, "cntf", [1, NE]); b_cntf = K.buf("cntf")
        pm = [ps(es, f"pm{i}", [128, D]) for i in range(2)]; b_pm = [K.pbuf(f"pm{i}") for i in range(2)]
        pT32 = ps(es, "pT32", [128, D]); b_pT32 = K.pbuf("pT32")
        pl = ps(es, "pl", [128, NE]); b_pl = K.pbuf("pl")
        ppos = ps(es, "ppos", [128, TC, NE]); b_ppos = K.pbuf("ppos")
        pcnt = pl[0:1, :]; b_pcnt = b_pl
        AXX = mybir.AxisListType.X

        def load_x(t):
            K.dma(SP, lambda: Q.dma_start(out=xt[t % 2][:], in_=x_d[t * 128:(t + 1) * 128, :]), writes=[b_xt[t % 2]])

        def route_chunk(c):
            t0 = c * TC
            sl = slice(t0, t0 + TC)
            bl, bt = b_lg[c], b_top8[c]
            K.op(DVE, lambda: V.tensor_tensor(out=d4[:], in0=top8_all[:, sl, 0:4], in1=top8_all[:, sl, 0:1].to_broadcast([128, TC, 4]),
                                              op=ALU.subtract), [bt], [b_d4])
            K.op(ACT, lambda: A.activation(out=d4[:], in_=d4[:], func=AF.Exp), [], [b_d4])
            K.op(DVE, lambda: V.tensor_reduce(out=den[:], in_=d4[:], axis=AXX, op=ALU.add), [b_d4], [b_den])
            K.op(DVE, lambda: V.reciprocal(out=den[:], in_=den[:]), [], [b_den])
            K.op(DVE, lambda: V.tensor_tensor(out=G_all[:, sl, :], in0=d4[:], in1=den[:].to_broadcast([128, TC, 4]) if False else
                                              den[:].rearrange("p (t o) -> p t o", o=1).to_broadcast([128, TC, 4]), op=ALU.mult),
                 [b_d4, b_den], [b_G])
            for r in range(4):
                K.op(DVE, lambda r=r: V.tensor_tensor(out=oh[:, r, :, :], in0=lg_all[:, sl, :],
                                                      in1=top8_all[:, sl, r:r + 1].to_broadcast([128, TC, NE]), op=ALU.is_equal),
                     [bl, bt], [b_oh])
            K.op(DVE, lambda: V.tensor_tensor(out=mk[:], in0=oh[:, 0, :, :], in1=oh[:, 1, :, :], op=ALU.add), [b_oh], [b_mk])
            K.op(DVE, lambda: V.tensor_tensor(out=mk[:], in0=mk[:], in1=oh[:, 2, :, :], op=ALU.add), [b_oh], [b_mk])
            K.op(DVE, lambda: V.tensor_tensor(out=mask_all[:, sl, :], in0=mk[:], in1=oh[:, 3, :, :], op=ALU.add), [b_oh, b_mk], b_mask[t0:t0 + TC])

            def mmp():
                for j in range(TC):
                    t = t0 + j
                    last = T.matmul(ppos[:, j, :], lhsT=U_b[:], rhs=mask_all[:, t, :], start=True, stop=(t == 0))
                    for i in range(t):
                        last = T.matmul(ppos[:, j, :], lhsT=ones_b[:], rhs=mask_all[:, i, :], start=False, stop=(i == t - 1))
                return last
            K.op(PE, mmp, [b_U, b_ones_b] + b_mask[:t0 + TC], [b_ppos])
            K.op(DVE, lambda: V.tensor_tensor(out=idxfull[:], in0=ppos[:], in1=ebase[:].rearrange("p (o e) -> p o e", o=1).to_broadcast([128, TC, NE]),
                                              op=ALU.add), [b_ppos, b_ebase], [b_idxf])
            for r in range(4):
                K.op(DVE, lambda r=r: V.tensor_tensor(out=junk2[:], in0=oh[:, r, :, :], in1=idxfull[:], op=ALU.mult), [b_oh, b_idxf], [b_junk2])
                K.op(DVE, lambda r=r: V.tensor_reduce(out=IDXf[:, sl, r], in_=junk2[:], axis=AXX, op=ALU.add), [b_junk2], [b_idx4])
            K.op(DVE, lambda: V.tensor_copy(out=IDX[:, sl, :], in_=IDXf[:, sl, :]), [b_idx4], [b_IDX])
            K.dma(POOL, lambda: [G.indirect_dma_start(out=Tab_d, out_offset=IOA(ap=IDX[:, t, r:r + 1], axis=0),
                                                      in_=VAL[:, t, r:r + 1], in_offset=None)
                                 for t in range(t0, t0 + TC) for r in range(4)],
                  [b_VAL, b_IDX], [b_Tab])

        def stage_a(t):
            s = t % 2
            rows = slice(t * 128, (t + 1) * 128)
            if t + 1 < NT:
                load_x(t + 1)

            def mmo():
                for half in range(2):
                    for kc in range(8):
                        last = T.matmul(pm[s][:, half * 512:(half + 1) * 512], lhsT=(catL[:, kc, rows] if kc < 4 else catA[:, kc - 4, rows]),
                                        rhs=wob[:, kc, half * 512:(half + 1) * 512], start=(kc == 0), stop=(kc == 7))
                return last
            K.op(PE, mmo, b_cat + [b_wob], [b_pm[s]])
            K.op(DVE, lambda: V.tensor_tensor(out=x1[s][:], in0=pm[s][:], in1=xt[s][:], op=ALU.add), [b_pm[s], b_xt[s]], [b_x1[s]])
            K.dma(ACT, lambda: A.dma_start(out=X1_d[rows, :], in_=x1[s][:]), [b_x1[s]], [b_X1])
            if stage == "F1":
                dbg_dump(x1[s][:], 128, D, b_x1[s], row0=t * 128)
                return
            K.op(ACT, lambda: A.activation(out=junk[:], in_=x1[s][:], func=AF.Square, accum_out=ss[:, 0:1]), [b_x1[s]], [b_junk, b_ss])
            rms_rstd(ss[:, 0:1], D, rstd[:], b_ss, b_ss, ss[:, 1:2], b_rstd)
            K.op(DVE, lambda: V.scalar_tensor_tensor(out=h2f[s][:], in0=x1[s][:], scalar=rstd[:, 0:1], in1=A2, op0=ALU.mult, op1=ALU.mult),
                 [b_x1[s], b_rstd, b_mod[4]], [b_h2f[s]])
            K.op(DVE, lambda: V.tensor_tensor(out=h2f[s][:], in0=h2f[s][:], in1=SH2, op=ALU.add), [b_mod[3]], [b_h2f[s]])
            K.op(POOL, lambda: G.tensor_copy(out=h2b[s][:], in_=h2f[s][:]), [b_h2f[s]], [b_h2b[s]])
            K.dma(POOL, lambda: [G.dma_start(out=H2r_v[rows, r, :], in_=h2b[s][:]) for r in range(4)], [b_h2b[s]], [b_H2r])

        def stage_b(t):
            s = t % 2
            c = t // TC

            def tr32():
                for kc in range(8):
                    last = T.transpose(pT32[:, kc * 128:(kc + 1) * 128], h2f[s][:, kc * 128:(kc + 1) * 128], ident_f[:])
                return last
            K.op(PE, tr32, [b_h2f[s], b_ident_f], [b_pT32])
            K.op(ACT, lambda: A.copy(out=h2T[:].rearrange("p k n -> p (k n)"), in_=pT32[:]), [b_pT32], [b_h2T])

            def mmr():
                for kc in range(8):
                    T.matmul(pl[:], lhsT=h2T[:, kc, :], rhs=wrt[:, kc, :], start=(kc == 0), stop=False)
                return T.matmul(pl[:], lhsT=ones_f[0:1, :], rhs=brt[0:1, :], start=False, stop=True)
            K.op(PE, mmr, [b_h2T, b_wrt, b_ones_f, b_brt], [b_pl])
            K.op(DVE, lambda: V.tensor_copy(out=lg_all[:, t, :], in_=pl[:]), [b_pl], [b_lg[c]])
            K.op(DVE, lambda: V.max(out=top8_all[:, t, :], in_=lg_all[:, t, :]), [b_lg[c]], [b_top8[c]])

        load_x(0)
        stage_a(0)
        pending = None
        for t in range(NT):
            if stage == "F1":
                if t + 1 < NT:
                    stage_a(t + 1)
                continue
            streams = []
            if t + 1 < NT:
                streams.append(K.record(lambda: stage_a(t + 1)))
            streams.append(K.record(lambda: stage_b(t)))
            if pending is not None:
                streams.append(pending)
                pending = None
            K.play(*streams)
            if t % TC == TC - 1:
                pending = K.record(lambda: route_chunk(t // TC))
        if pending is not None:
            K.play(pending)
        if stage == "F1":
            return finish()

        def mmc():
            for t in range(NT):
                last = T.matmul(pcnt, lhsT=ones_b[:, 0:1], rhs=mask_all[:, t, :], start=(t == 0), stop=(t == NT - 1))
            return last
        K.op(PE, mmc, [b_ones_b] + b_mask, [b_pcnt])
        K.op(DVE, lambda: V.tensor_copy(out=cntf[:], in_=pcnt), [b_pcnt], [b_cntf])
        K.op(DVE, lambda: V.tensor_copy(out=cnt_i[:], in_=cntf[:]), [b_cntf], [b_cnt])
        if stage == "F":
            K.barrier()
            K.op(DVE, lambda: V.tensor_copy(out=h2f[0][:, 0:4 * NT], in_=IDX[:].rearrange("p a b -> p (a b)")), [b_IDX], [b_h2f[0]])
            dbg_dump(h2f[0][:, 0:4 * NT], 128, 4 * NT, b_h2f[0], row0=0)
            dbg_dump(G_all[:].rearrange("p a b -> p (a b)"), 128, 4 * NT, b_G, row0=128)
            dbg_dump(cntf[:], 1, NE, b_cntf, row0=256)
            return finish()
        K.barrier()

    catA_stack.close()
    es_mix.close()
    cat_stack.close()
    K.barrier()
    mod_stack.close()
    with ExitStack() as es:
        NW = 3
        w1b = [sb(es, f"w1b{i}", [128, 8, 2 * D], BF) for i in range(NW)]
        b_w1b = [[K.buf(f"w1b{i}")] for i in range(NW)]
        w2b = [sb(es, f"w2b{i}", [128, 8, D], BF) for i in range(NW)]
        b_w2b = [[K.buf(f"w2b{i}")] for i in range(NW)]
        b1all = sb(es, "b1all", [96, 2 * D], BF); b_b1b = [K.buf(f"b1b{i}") for i in range(NW)]
        b2all = sb(es, "b2all", [96, D], BF); b_b2b = [K.buf(f"b2b{i}") for i in range(NW)]
        NS = 4
        xe = [[sb(es, f"xe{i}{b}", [128, D], BF) for b in range(2)] for i in range(NS)]
        b_xe = [[K.buf(f"xe{i}{b}") for b in range(2)] for i in range(NS)]
        tb = [[sb(es, f"tb{i}{b}", [128, 1], I32) for b in range(2)] for i in range(NS)]
        b_tb = [[K.buf(f"tb{i}{b}") for b in range(2)] for i in range(NS)]
        for i in range(NS):
            for b in range(2):
                K.op(DVE, lambda i=i, b=b: V.memset(xe[i][b][:], 0.0), writes=[b_xe[i][b]])
        xT2 = [[sb(es, f"xT{i}{b}", [128, 8, 128], BF) for b in range(2)] for i in range(2)]
        b_xT2 = [[K.buf(f"xT{i}{b}") for b in range(2)] for i in range(2)]
        xT = [xT2[i % 2] for i in range(NS)]
        b_xT = [b_xT2[i % 2] for i in range(NS)]
        glu1 = sb(es, "glu", [128, 512]); glu = [glu1, glu1]; b_glu1 = K.buf("glu"); b_glu = [b_glu1, b_glu1]
        sg1 = sb(es, "sg", [128, 512]); sg = [sg1, sg1]; b_sg1 = K.buf("sg"); b_sg = [b_sg1, b_sg1]
        lin1 = sb(es, "lin", [128, 512]); lin = [lin1, lin1]; b_lin1 = K.buf("lin"); b_lin = [b_lin1, b_lin1]
        actb = [sb(es, f"actb{b}", [128, D], BF) for b in range(2)]; b_actb = [K.buf(f"actb{b}") for b in range(2)]
        aT1 = sb(es, "aT", [128, 8, 128], BF); aT = [aT1, aT1]; b_aT1 = K.buf("aT"); b_aT = [b_aT1, b_aT1]
        yo = [sb(es, f"yo{i}", [128, D]) for i in range(2)]; b_yo = [K.buf(f"yo{i}") for i in range(2)]
        pTx = ps(es, "pTx", [128, D], BF); b_pTx = K.pbuf("pTx")
        pgu = [ps(es, f"pgu{i}", [128, D]) for i in range(2)]; b_pgu = [K.pbuf(f"pgu{i}") for i in range(2)]
        pTa = ps(es, "pTa", [128, D], BF); b_pTa = K.pbuf("pTa")
        py = ps(es, "py", [128, D]); b_py = K.pbuf("py")
        NPAIR = nbmax // 2
        GRP = 4

        def load_w(e):
            s = e % NW
            v1 = w1_d[e].rearrange("(kc p) n -> p kc n", p=128)
            v2 = w2_d[e].rearrange("(kc p) n -> p kc n", p=128)
            K.dma(POOL, lambda: [G.dma_start(out=w1b[s][:, kc, :], in_=v1[:, kc, :]) for kc in range(8)], writes=[b_w1b[s][0]])
            K.dma(POOL, lambda: [G.dma_start(out=w2b[s][:, kc, :], in_=v2[:, kc, :]) for kc in range(8)], writes=[b_w2b[s][0]])
            K.dma(POOL, lambda: G.dma_start(out=b1all[32 * s:32 * s + 1, :], in_=b1_d[e:e + 1, :]), writes=[b_b1b[s]])
            K.dma(POOL, lambda: G.dma_start(out=b2all[32 * s:32 * s + 1, :], in_=b2_d[e:e + 1, :]), writes=[b_b2b[s]])

        bcreg = G.alloc_register("bcreg")
        G.reg_mov(bcreg, S * 4 - 1)

        def load_pair(e, pr, s):
            r0 = e * CAP + pr * 256
            for b in range(2):
                K.dma(SP, lambda b=b: Q.dma_start(out=tb[s][b][:], in_=Tab_d[r0 + b * 128:r0 + (b + 1) * 128, :]), [b_Tab], [b_tb[s][b]])
            for b in range(2):
                K.dma(POOL, lambda b=b: G.indirect_dma_start(out=xe[s][b][:], out_offset=None, in_=H2r_d,
                                                             in_offset=IOA(ap=tb[s][b][:, 0:1], axis=0),
                                                             bounds_check=bcreg, oob_is_err=False),
                      [b_H2r, b_tb[s][b]], [b_xe[s][b]])

        ei = [0]

        def slot_of(e, pr):
            return 2 * (e % 2) + (pr % 2)

        def p1(e, pr):
            s = slot_of(e, pr)
            for b in range(2):
                def trx(b=b):
                    for kc in range(8):
                        last = T.transpose(pTx[:, kc * 128:(kc + 1) * 128], xe[s][b][:, kc * 128:(kc + 1) * 128], ident_b[:])
                    return last
                K.op(PE, trx, [b_xe[s][b], b_ident_b], [b_pTx])
                K.op(ACT, lambda b=b: A.copy(out=xT[s][b][:].rearrange("p k n -> p (k n)"), in_=pTx[:]), [b_pTx], [b_xT[s][b]])

        def pair_body(e, pr, s, ws, after_loads=None):
            if pr + 1 < NPAIR:
                load_pair(e, pr + 1, slot_of(e, pr + 1))
            if after_loads is not None:
                after_loads()
            def stage2(b, h):
                def mm1():
                    for n, col in ((0, h * 512), (1, D + h * 512)):
                        for kc in range(8):
                            T.matmul(pgu[h][:, n * 512:(n + 1) * 512], lhsT=xT[s][b][:, kc, :], rhs=w1b[ws][:, kc, col:col + 512],
                                     start=(kc == 0), stop=False)
                        last = T.matmul(pgu[h][:, n * 512:(n + 1) * 512], lhsT=ones_b[32 * ws:32 * ws + 1, :], rhs=b1all[32 * ws:32 * ws + 1, col:col + 512],
                                        start=False, stop=True)
                    return last
                K.op(PE, mm1, [b_xT[s][b], b_b1b[ws], b_ones_b] + b_w1b[ws], [b_pgu[h]])
                r = ei[0] % 2
                ei[0] += 1
                K.op(DVE, lambda: V.tensor_scalar(out=glu[r][:], in0=pgu[h][:, 0:512], scalar1=7.0, scalar2=None, op0=ALU.min),
                     [b_pgu[h]], [b_glu[r]])
                K.op(ACT, lambda: A.activation(out=sg[r][:], in_=glu[r][:], func=AF.Sigmoid, scale=1.702), [b_glu[r]], [b_sg[r]])
                K.op(DVE, lambda: V.tensor_scalar(out=lin[r][:], in0=pgu[h][:, 512:1024], scalar1=-7.0, scalar2=7.0,
                                                  op0=ALU.max, op1=ALU.min), [b_pgu[h]], [b_lin[r]])
                K.op(DVE, lambda: V.scalar_tensor_tensor(out=lin[r][:], in0=lin[r][:], scalar=1.0, in1=glu[r][:],
                                                         op0=ALU.add, op1=ALU.mult), [b_glu[r]], [b_lin[r]])
                K.op(DVE, lambda: V.tensor_tensor(out=actb[b][:, h * 512:(h + 1) * 512], in0=lin[r][:], in1=sg[r][:], op=ALU.mult),
                     [b_lin[r], b_sg[r]], [b_actb[b]])

            def stage3(b):
                def tra():
                    for kc in range(8):
                        last = T.transpose(pTa[:, kc * 128:(kc + 1) * 128], actb[b][:, kc * 128:(kc + 1) * 128], ident_b[:])
                    return last
                K.op(PE, tra, [b_actb[b], b_ident_b], [b_pTa])
                K.op(ACT, lambda: A.copy(out=aT[b][:].rearrange("p k n -> p (k n)"), in_=pTa[:]), [b_pTa], [b_aT[b]])

            def stage4(b):
                def mm2():
                    for n in range(2):
                        for kc in range(8):
                            T.matmul(py[:, n * 512:(n + 1) * 512], lhsT=aT[b][:, kc, :], rhs=w2b[ws][:, kc, n * 512:(n + 1) * 512],
                                     start=(kc == 0), stop=False)
                        last = T.matmul(py[:, n * 512:(n + 1) * 512], lhsT=ones_b[32 * ws:32 * ws + 1, :], rhs=b2all[32 * ws:32 * ws + 1, n * 512:(n + 1) * 512],
                                        start=False, stop=True)
                    return last
                K.op(PE, mm2, [b_aT[b], b_b2b[ws], b_ones_b] + b_w2b[ws], [b_py])
                K.op(ACT, lambda: A.copy(out=yo[b][:], in_=py[:]), [b_py], [b_yo[b]])
                K.dma(POOL, lambda: G.indirect_dma_start(out=Yb_d, out_offset=IOA(ap=tb[s][b][:, 0:1], axis=0),
                                                         in_=yo[b][:], in_offset=None,
                                                         bounds_check=bcreg, oob_is_err=False),
                      [b_yo[b], b_tb[s][b]], [b_Yb])

            stage2(0, 0)
            stage2(0, 1)
            stage2(1, 0)
            stage3(0)
            stage2(1, 1)
            stage4(0)
            stage3(1)
            stage4(1)
            if pr + 1 < NPAIR:
                p1(e, pr + 1)

        if use_if:
            regs = nc.alloc_registers("cntreg")
        load_w(0)
        load_w(1)
        load_pair(0, 0, slot_of(0, 0))
        p1(0, 0)
        for e in range(NE):
            ws = e % NW
            if e + 1 < NE:
                load_pair(e + 1, 0, slot_of(e + 1, 0))
            if use_if:
                for eng in K.engs:
                    K.wait_buf(eng, b_cnt)
                for reg in regs:
                    nc.reg_load(reg, cnt_i[0:1, e:e + 1])
            pair_body(e, 0, slot_of(e, 0), ws, after_loads=(lambda: load_w(e + 2)) if e + 2 < NE else None)

            def chain(pr):
                if pr >= NPAIR:
                    return
                snap = K.snapshot()
                ctx = nc.If_cmp(regs, 256 * pr, "IS_GT") if use_if else ExitStack()
                with ctx:
                    pair_body(e, pr, slot_of(e, pr), ws)
                    chain(pr + 1)
                if use_if:
                    with nc.Else():
                        K.compensate(snap)
                    K.restore_seen(snap)
            chain(1)
            if e + 1 < NE:
                p1(e + 1, 0)
        K.barrier()

    with ExitStack() as es:
        NR = 3
        x1t = [sb(es, f"x1t{i}", [128, D]) for i in range(NR)]; b_x1t = [K.buf(f"x1t{i}") for i in range(NR)]
        yg = [sb(es, f"yg{i}", [128, 4, D]) for i in range(NR)]; b_yg = [K.buf(f"yg{i}") for i in range(NR)]
        Yb_v = Yb_d.rearrange("(n r) d -> n (r d)", r=4)
        acc = [sb(es, f"acc{i}", [128, D]) for i in range(2)]; b_acc = [K.buf(f"acc{i}") for i in range(2)]

        def loads(t):
            s = t % NR
            rows = slice(t * 128, (t + 1) * 128)
            K.dma(SP, lambda: Q.dma_start(out=yg[s][:].rearrange("p r d -> p (r d)"), in_=Yb_v[rows, :]), [b_Yb], [b_yg[s]])
            K.dma(SP, lambda: Q.dma_start(out=x1t[s][:], in_=X1_d[rows, :]), [b_X1], [b_x1t[s]])
        loads(0)
        loads(1)
        for t in range(NT):
            s = t % NR
            a = t % 2
            rows = slice(t * 128, (t + 1) * 128)
            if t + 2 < NT:
                loads(t + 2)
            K.op(DVE, lambda: V.tensor_scalar(out=acc[a][:], in0=yg[s][:, 0, :], scalar1=G_all[:, t, 0:1], scalar2=None, op0=ALU.mult),
                 [b_yg[s], b_G], [b_acc[a]])
            for r in range(1, 4):
                K.op(DVE, lambda r=r: V.scalar_tensor_tensor(out=acc[a][:], in0=yg[s][:, r, :], scalar=G_all[:, t, r:r + 1],
                                                             in1=acc[a][:], op0=ALU.mult, op1=ALU.add),
                     [b_yg[s], b_G], [b_acc[a]])
            K.op(DVE, lambda: V.tensor_tensor(out=acc[a][:], in0=acc[a][:], in1=gate2k[:], op=ALU.mult), [b_gate2k], [b_acc[a]])
            K.op(DVE, lambda: V.tensor_tensor(out=acc[a][:], in0=acc[a][:], in1=x1t[s][:], op=ALU.add), [b_x1t[s]], [b_acc[a]])
            K.dma(ACT, lambda: A.dma_start(out=out_d[rows, :], in_=acc[a][:]), [b_acc[a]], [b_out])
    return finish()


def prep_inputs(inp):
    f = np.float32
    L = 0
    g = lambda n: np.asarray(inp[n][L])
    w_in = g("w_in")
    kro = w_in[:, 1408:1440]
    perm = np.concatenate([np.arange(16, 32), np.arange(0, 16)])
    w_in_ext = np.ascontiguousarray(np.concatenate([w_in[:, :1408], kro[:, perm], kro], axis=1), dtype=f)
    fm = lambda v: np.ascontiguousarray(np.asarray(v, dtype=f).reshape(-1, 128).T)
    cw = g("conv_w")
    fields = [cw[0], cw[1], cw[2], cw[3], g("conv_b"), g("b_a"), g("b_x"), g("lam")]
    lruv = np.ascontiguousarray(np.stack([fm(v) for v in fields], axis=-1), dtype=f)

    def bd(w):
        o = np.zeros((4, 128, 128), f)
        for c in range(4):
            o[c, 0:64, 0:64] = w[2 * c]
            o[c, 64:128, 64:128] = w[2 * c + 1]
        return o
    w_uq = g("w_uq"); w_ukv = g("w_ukv")
    w_uq_h = np.zeros((8, 256, 128), f)
    w_uk_h = np.zeros((8, 128, 128), f)
    w_uv = np.zeros((128, 512), f)
    for h in range(8):
        nope = w_uq[:, h * 96:h * 96 + 64]; rope = w_uq[:, h * 96 + 64:h * 96 + 96]
        w_uq_h[h] = np.concatenate([rope[:, perm], rope, nope], axis=1)
        w_uk_h[h, :, 64:128] = w_ukv[:, h * 128:h * 128 + 64]
        w_uv[:, h * 64:(h + 1) * 64] = w_ukv[:, h * 128 + 64:h * 128 + 128]

    def grow(gv):
        return np.ascontiguousarray(np.concatenate([gv[64:96][perm], gv[64:96], gv[0:64]]).reshape(128, 1), dtype=f)
    half = 16
    freqs = (10000.0 ** (-np.arange(half, dtype=np.float64) / half)).astype(f)
    fr32 = np.concatenate([freqs, freqs])
    rtab = np.zeros((64, 3), f)
    rtab[0:32, 0] = fr32; rtab[32:64, 0] = fr32
    rtab[0:32, 1] = 0.0; rtab[32:64, 1] = math.pi / 2
    rtab[0:16, 2] = -1.0; rtab[16:32, 2] = 1.0; rtab[32:64, 2] = 1.0
    shared = {
        "w_ada": g("w_ada"), "b_ada": g("b_ada").reshape(1, -1),
        "g_mix_bc": np.ascontiguousarray(np.broadcast_to(g("g_mix")[None, :], (128, D))),
        "g_ffn_bc": np.ascontiguousarray(np.broadcast_to(g("g_ffn")[None, :], (128, D))),
        "w_in_ext": w_in_ext, "lruv": lruv, "wa_bd": bd(g("w_a")), "wx_bd": bd(g("w_x")),
        "g_q_lat": fm(g("g_q_lat")), "g_kv_lat": fm(g("g_kv_lat")),
        "w_uq_h": w_uq_h, "w_uk_h": w_uk_h, "w_uv": w_uv, "gq": grow(g("g_qn")), "gk": grow(g("g_kn")),
        "rtab": rtab, "w_out": g("w_out"), "w_router": g("w_router"), "b_router": g("b_router").reshape(1, -1),
        "w1": g("w1"), "b1": g("b1"), "w2": g("w2"), "b2": g("b2"),
    }
    shared = {k: np.ascontiguousarray(v, dtype=f) for k, v in shared.items()}
    maps = []
    for b in range(8):
        m = dict(shared)
        m["x"] = np.ascontiguousarray(inp["x"][b], dtype=f)
        m["cT"] = fm(np.asarray(inp["c"][b]))
        m["pos"] = np.ascontiguousarray(np.broadcast_to(np.asarray(inp["positions"][b], dtype=np.int32)[None, :], (64, S)))
        maps.append(m)
    return maps


def kernel(**inputs):
    maps = prep_inputs(inputs)
    nc = build_nc("full")
    res = run_bass_kernel_spmd(nc, maps, core_ids=list(range(8)))
    return np.stack([np.asarray(r["out"], dtype=np.float32) for r in res.results], axis=0)
```

```python
import math
from contextlib import ExitStack
import numpy as np
import concourse.bass as bass
import concourse.mybir as mybir
from concourse.bass_utils import run_bass_kernel_spmd

F32 = mybir.dt.float32
BF = mybir.dt.bfloat16
I32 = mybir.dt.int32
AF = mybir.ActivationFunctionType
ALU = mybir.AluOpType
IOA = bass.IndirectOffsetOnAxis

S = 4096
D = 1024
NT = S // 128
NE = 32
CAP = 4096
NBMAX = CAP // 128
EPS = 1e-6
TWO_PI = 2.0 * math.pi


class Dep:
    def __init__(self, nc, name):
        self.sem = nc.alloc_semaphore(name)
        self.n = 0
        self.issuer = None


class Buf:
    def __init__(self, name):
        self.name = name
        self.w = None
        self.r = {}
        self.dsem = None
        self.psum = False


class Eng:
    def __init__(self, k, name, ins, has_dep=True):
        self.name = name
        self.ins = ins
        self.dep = Dep(k.nc, "c_" + name) if has_dep else None
        self.seen = {}


class Kern:
    def __init__(self, nc):
        self.nc = nc
        self.pe = Eng(self, "pe", nc.tensor)
        self.act = Eng(self, "act", nc.scalar)
        self.dve = Eng(self, "dve", nc.vector)
        self.pool = Eng(self, "pool", nc.gpsimd)
        self.sp = Eng(self, "sp", nc.sync, has_dep=False)
        self.engs = [self.pe, self.act, self.dve, self.pool, self.sp]
        self.deps = [e.dep for e in self.engs if e.dep is not None]
        self.nbuf = 0

    def buf(self, name=None):
        self.nbuf += 1
        return Buf(name or f"b{self.nbuf}")

    def pbuf(self, name):
        b = Buf(name)
        b.psum = True
        return b

    def _wait(self, eng, dep, val):
        if dep is eng.dep and eng is self.pe:
            return
        if eng.seen.get(dep, 0) < val:
            eng.ins.wait_ge(dep.sem, val)
            eng.seen[dep] = val

    def _deps_for(self, eng, reads, writes):
        for b in reads:
            if b.w is not None:
                self._wait(eng, *b.w)
        for b in writes:
            if b.w is not None:
                self._wait(eng, *b.w)
            for d, v in b.r.items():
                self._wait(eng, d, v)

    def _commit(self, tok, reads, writes):
        for b in writes:
            b.w = tok
            b.r = {}
        for b in reads:
            if b in writes:
                continue
            d, v = tok
            if b.r.get(d, 0) < v:
                b.r[d] = v

    def record(self, thunk):
        self.rec = []
        thunk()
        r, self.rec = self.rec, None
        return r

    def play(self, *streams):
        idx = [0] * len(streams)
        total = max(len(st) for st in streams) if streams else 0
        for step in range(total):
            for k, st in enumerate(streams):
                upto = (step + 1) * len(st) // total
                while idx[k] < upto:
                    kind, args = st[idx[k]]
                    idx[k] += 1
                    (self.op if kind == "op" else self.dma)(*args)

    def op(self, eng, fn, reads=(), writes=()):
        if getattr(self, "rec", None) is not None:
            self.rec.append(("op", (eng, fn, list(reads), list(writes))))
            return
        reads = list(reads)
        writes = list(writes) + [b for b in reads if b.psum and b not in writes]
        self._deps_for(eng, reads, writes)
        ins = fn()
        d = eng.dep
        d.n += 1
        ins.then_inc(d.sem, 1)
        self._commit((d, d.n), reads, writes)

    def dma(self, eng, fn, reads=(), writes=(), on=None):
        if getattr(self, "rec", None) is not None:
            self.rec.append(("dma", (eng, fn, list(reads), list(writes), on)))
            return
        self._deps_for(eng, reads, writes)
        b = on if on is not None else (writes[0] if writes else reads[0])
        if b.dsem is None:
            b.dsem = Dep(self.nc, "d_" + b.name)
            self.deps.append(b.dsem)
        d = b.dsem
        assert d.issuer in (None, eng), (b.name, d.issuer.name, eng.name)
        d.issuer = eng
        ins = fn()
        for i1 in (ins if isinstance(ins, (list, tuple)) else [ins]):
            d.n += 16
            i1.then_inc(d.sem, 16)
        self._commit((d, d.n), reads, writes)

    def wait_buf(self, eng, b):
        self._deps_for(eng, [b], [])

    def barrier(self):
        for e in self.engs:
            for d in self.deps:
                if d.n > 0:
                    self._wait(e, d, d.n)

    def snapshot(self):
        return ({d: d.n for d in self.deps}, {e: dict(e.seen) for e in self.engs})

    def compensate(self, snap):
        before, _ = snap
        for d in self.deps:
            b4 = before.get(d, 0)
            delta = d.n - b4
            if delta <= 0:
                continue
            eng = d.issuer
            if eng is None:
                eng = [e for e in self.engs if e.dep is d][0]
            eng.ins.wait_ge(d.sem, b4)
            eng.ins.sem_inc(d.sem, delta)

    def restore_seen(self, snap):
        _, seen = snap
        for e in self.engs:
            e.seen = dict(seen[e])


def build_nc(stage="full", use_if=True, nbmax=NBMAX):
    nc = bass.Bass("TRN2", target_bir_lowering=False)
    K = Kern(nc)
    PE, ACT, DVE, POOL, SP = K.pe, K.act, K.dve, K.pool, K.sp
    T, A, V, G, Q = nc.tensor, nc.scalar, nc.vector, nc.gpsimd, nc.sync

    def din(name, shape, dt=F32):
        return nc.dram_tensor(name, list(shape), dt, kind="ExternalInput").ap()

    x_d = din("x", [S, D])
    cT_d = din("cT", [128, 8])
    pos_d = din("pos", [64, S], I32)
    wada_d = din("w_ada", [D, 6 * D])
    bada_d = din("b_ada", [1, 6 * D])
    gmix_d = din("g_mix_bc", [128, D])
    gffn_d = din("g_ffn_bc", [128, D])
    win_d = din("w_in_ext", [D, 1472])
    lruv_d = din("lruv", [128, 4, 8])
    wabd_d = din("wa_bd", [4, 128, 128])
    wxbd_d = din("wx_bd", [4, 128, 128])
    gql_d = din("g_q_lat", [128, 2])
    gkvl_d = din("g_kv_lat", [128, 1])
    wuq_d = din("w_uq_h", [8, 256, 128])
    wuk_d = din("w_uk_h", [8, 128, 128])
    wuv_d = din("w_uv", [128, 512])
    gq_d = din("gq", [128, 1])
    gk_d = din("gk", [128, 1])
    rtab_d = din("rtab", [64, 3])
    wout_d = din("w_out", [D, D])
    wr_d = din("w_router", [D, NE])
    br_d = din("b_router", [1, NE])
    w1_d = din("w1", [NE, D, 2 * D])
    b1_d = din("b1", [NE, 2 * D])
    w2_d = din("w2", [NE, D, D])
    b2_d = din("b2", [NE, D])
    out_d = nc.dram_tensor("out", [S, D], F32, kind="ExternalOutput").ap()
    dbg_d = None
    if stage != "full":
        dbg_d = nc.dram_tensor("dbg", [S, D], F32, kind="ExternalOutput").ap()
    X1_d = nc.dram_tensor("X1s", [S, D], F32, kind="Internal").ap()
    H2r_d = nc.dram_tensor("H2r", [S * 4, D], BF, kind="Internal").ap()
    Yb_d = nc.dram_tensor("Ybs", [S * 4, D], F32, kind="Internal").ap()
    Tab_d = nc.dram_tensor("Tabs", [NE * CAP, 1], I32, kind="Internal").ap()
    b_out, b_dbg, b_X1, b_H2r, b_Yb, b_Tab = K.buf("out"), K.buf("dbg"), K.buf("X1"), K.buf("H2r"), K.buf("Yb"), K.buf("Tab")

    top = ExitStack()

    def sb(es, name, shape, dt=F32):
        return es.enter_context(nc.sbuf_tensor("s_" + name, list(shape), dt))

    def ps(es, name, shape, dt=F32):
        return es.enter_context(nc.psum_tensor("p_" + name, list(shape), dt))

    ident_b = sb(top, "ident_b", [128, 128], BF); b_ident_b = K.buf("identb")
    ident_f = sb(top, "ident_f", [128, 128], F32); b_ident_f = K.buf("identf")
    ones_b = sb(top, "ones_b", [128, 128], BF); b_ones_b = K.buf("onesb")
    ones_f = sb(top, "ones_f", [128, 128], F32); b_ones_f = K.buf("onesf")
    sel_b = sb(top, "sel_b", [128, 128], BF); b_sel = K.buf("sel")
    U_b = sb(top, "U_b", [128, 128], BF); b_U = K.buf("U")
    tri_b = sb(top, "tri_b", [128, 128], BF); b_tri = K.buf("tri")
    iof = sb(top, "iof", [128, 128], F32); b_iof = K.buf("iof")
    pidx = sb(top, "pidx", [128, 1], F32); b_pidx = K.buf("pidx")
    ebase = sb(top, "ebase", [128, NE], F32); b_ebase = K.buf("ebase")
    gate2k = sb(top, "gate2k", [128, D], F32); b_gate2k = K.buf("gate2k")
    G_all = sb(top, "G_all", [128, NT, 4], F32); b_G = K.buf("G_all")
    IDX = sb(top, "IDX", [128, NT, 4], I32); b_IDX = K.buf("IDX")
    cnt_i = sb(top, "cnt_i", [1, NE], I32); b_cnt = K.buf("cnt")
    mod_stack = ExitStack()
    mod = sb(mod_stack, "mod", [128, 5 * D], F32)
    b_mod = [K.buf(f"mod{i}") for i in range(6)]
    cat_stack = ExitStack()
    catL = sb(cat_stack, "catL", [128, 4, S], BF); b_cat = [K.buf(f"cat{i}") for i in range(8)]

    K.op(POOL, lambda: G.iota(iof[:], pattern=[[1, 128]], base=0, channel_multiplier=-1,
                              allow_small_or_imprecise_dtypes=True), writes=[b_iof])
    K.op(POOL, lambda: G.iota(pidx[:], pattern=[[0, 1]], base=0, channel_multiplier=1,
                              allow_small_or_imprecise_dtypes=True), writes=[b_pidx])
    K.op(POOL, lambda: G.iota(ebase[:], pattern=[[CAP, NE]], base=0, channel_multiplier=0,
                              allow_small_or_imprecise_dtypes=True), writes=[b_ebase])
    K.op(DVE, lambda: V.tensor_single_scalar(out=ident_b[:], in_=iof[:], scalar=0.0, op=ALU.is_equal), [b_iof], [b_ident_b])
    K.op(DVE, lambda: V.tensor_single_scalar(out=ident_f[:], in_=iof[:], scalar=0.0, op=ALU.is_equal), [b_iof], [b_ident_f])
    K.op(DVE, lambda: V.tensor_single_scalar(out=U_b[:], in_=iof[:], scalar=0.0, op=ALU.is_gt), [b_iof], [b_U])
    K.op(DVE, lambda: V.tensor_single_scalar(out=tri_b[:], in_=iof[:], scalar=0.0, op=ALU.is_ge), [b_iof], [b_tri])
    K.op(DVE, lambda: V.memset(ones_b[:], 1.0), writes=[b_ones_b])
    K.op(DVE, lambda: V.memset(ones_f[:], 1.0), writes=[b_ones_f])
    K.op(DVE, lambda: V.tensor_scalar(out=sel_b[:], in0=ones_f[:], scalar1=pidx[:, 0:1], scalar2=32.0,
                                      op0=ALU.mult, op1=ALU.is_ge), [b_ones_f, b_pidx], [b_sel])

    def dbg_dump(src_ap, rows, cols, b_src, row0=0, col0=0):
        K.dma(POOL, lambda: G.dma_start(out=dbg_d[row0:row0 + rows, col0:col0 + cols], in_=src_ap), [b_src], [b_dbg])

    def finish():
        K.barrier()
        for d in K.deps:
            if d.n > 0:
                Q.wait_ge(d.sem, d.n)
        pass
        return nc

    with ExitStack() as es:
        cT = sb(es, "cT", [128, 8]); b_cT = K.buf("cT")
        sc = sb(es, "sc", [128, 8]); b_sc = K.buf("sc")
        scb = sb(es, "scb", [128, 8, 128]); b_scb = K.buf("scb")
        bada = sb(es, "bada", [1, 6 * D]); b_bada = K.buf("bada")
        wst = [sb(es, f"wst{i}", [128, 8, 512]) for i in range(2)]; b_wst = [K.buf(f"wst{i}") for i in range(2)]
        pA = [ps(es, f"pA{i}", [128, 512]) for i in range(2)]; b_pA = [K.pbuf(f"pA{i}") for i in range(2)]
        gm = sb(es, "gm", [128, D]); b_gm = K.buf("gm")
        gf = sb(es, "gf", [128, D]); b_gf = K.buf("gf")
        K.dma(SP, lambda: Q.dma_start(out=cT[:], in_=cT_d), writes=[b_cT])
        K.dma(SP, lambda: Q.dma_start(out=bada[:], in_=bada_d), writes=[b_bada])
        K.dma(SP, lambda: Q.dma_start(out=gm[:], in_=gmix_d), writes=[b_gm])
        K.dma(SP, lambda: Q.dma_start(out=gf[:], in_=gffn_d), writes=[b_gf])
        K.op(ACT, lambda: A.activation(out=sc[:], in_=cT[:], func=AF.Silu), [b_cT], [b_sc])
        for kc in range(8):
            K.op(DVE, lambda kc=kc: V.tensor_scalar(out=scb[:, kc, :], in0=ones_f[:], scalar1=sc[:, kc:kc + 1],
                                                   scalar2=None, op0=ALU.mult), [b_sc, b_ones_f], [b_scb])
        wada_v = wada_d.rearrange("(kc p) n -> p kc n", p=128)
        for n in range(12):
            s = n % 2
            K.dma(SP, lambda n=n, s=s: Q.dma_start(out=wst[s][:], in_=wada_v[:, :, n * 512:(n + 1) * 512]), writes=[b_wst[s]])

            def mm(n=n, s=s):
                for kc in range(8):
                    T.matmul(pA[s][:], lhsT=scb[:, kc, :], rhs=wst[s][:, kc, :], start=(kc == 0), stop=False)
                return T.matmul(pA[s][:], lhsT=ones_f[0:1, :], rhs=bada[0:1, n * 512:(n + 1) * 512], start=False, stop=True)
            K.op(PE, mm, [b_scb, b_wst[s], b_ones_f, b_bada], [b_pA[s]])
            if n < 10:
                K.op(ACT, lambda n=n, s=s: A.copy(out=mod[:, n * 512:(n + 1) * 512], in_=pA[s][:]), [b_pA[s]], [b_mod[n // 2]])
            else:
                K.op(ACT, lambda n=n, s=s: A.copy(out=gate2k[:, (n - 10) * 512:(n - 9) * 512], in_=pA[s][:]), [b_pA[s]], [b_gate2k])
        K.op(DVE, lambda: V.scalar_tensor_tensor(out=mod[:, D:2 * D], in0=mod[:, D:2 * D], scalar=1.0, in1=gm[:],
                                                 op0=ALU.add, op1=ALU.mult), [b_gm], [b_mod[1]])
        K.op(DVE, lambda: V.scalar_tensor_tensor(out=mod[:, 4 * D:5 * D], in0=mod[:, 4 * D:5 * D], scalar=1.0, in1=gf[:],
                                                 op0=ALU.add, op1=ALU.mult), [b_gf], [b_mod[4]])
        if stage == "A":
            for i in range(5):
                dbg_dump(mod[0:1, i * D:(i + 1) * D], 1, D, b_mod[i], row0=i)
            dbg_dump(gate2k[0:1, :], 1, D, b_gate2k, row0=5)
            return finish()
        K.barrier()
    SH1, A1, GATE1, SH2, A2 = [mod[:, i * D:(i + 1) * D] for i in range(5)]

    def rms_rstd(ss_ap, n, out_ap, b_in, b_tmp, tmp_ap, b_out_):
        K.op(ACT, lambda: A.activation(out=tmp_ap, in_=ss_ap, func=AF.Ln, scale=1.0 / n, bias=EPS), [b_in], [b_tmp])
        K.op(ACT, lambda: A.activation(out=out_ap, in_=tmp_ap, func=AF.Exp, scale=-0.5), [b_tmp], [b_out_])

    es_mix = ExitStack()
    if True:
        qlatn = sb(es_mix, "qlatn", [128, 2, S], BF); b_qlatn = K.buf("qlatn")
        kvlatn = sb(es_mix, "kvlatn", [128, S], BF); b_kvlatn = K.buf("kvlatn")
        zr = sb(es_mix, "zr", [64, S], BF); b_zr = K.buf("zr")
        with ExitStack() as es:
            winb = sb(es, "winb", [128, 8, 1472], BF); b_winb = K.buf("winb")
            win_v = win_d.rearrange("(kc p) n -> p kc n", p=128)
            for kc in range(8):
                K.dma(POOL, lambda kc=kc: G.dma_start(out=winb[:, kc, :], in_=win_v[:, kc, :]), writes=[b_winb])
            lruv = sb(es, "lruv", [128, 4, 8]); b_lruv = K.buf("lruv")
            K.dma(SP, lambda: Q.dma_start(out=lruv[:], in_=lruv_d), writes=[b_lruv])
            nsp = sb(es, "nsp", [128, 4, 2]); b_nsp = K.buf("nsp")
            spt = sb(es, "spt", [128, 4]); b_spt = K.buf("spt")
            K.op(ACT, lambda: A.activation(out=spt[:], in_=lruv[:, :, 7], func=AF.Exp, scale=-1.0), [b_lruv], [b_spt])
            K.op(ACT, lambda: A.activation(out=spt[:], in_=spt[:], func=AF.Ln, bias=1.0), [b_spt], [b_spt])
            K.op(DVE, lambda: V.tensor_scalar(out=nsp[:, :, 0], in0=spt[:], scalar1=-8.0, scalar2=None, op0=ALU.mult), [b_spt], [b_nsp])
            K.op(DVE, lambda: V.tensor_scalar(out=nsp[:, :, 1], in0=spt[:], scalar1=-16.0, scalar2=None, op0=ALU.mult), [b_spt], [b_nsp])
            wab = sb(es, "wab", [128, 4, 128], BF); b_wab = K.buf("wab")
            wxb = sb(es, "wxb", [128, 4, 128], BF); b_wxb = K.buf("wxb")
            K.dma(POOL, lambda: G.dma_start(out=wab[:], in_=wabd_d.rearrange("c p n -> p c n")), writes=[b_wab])
            K.dma(POOL, lambda: G.dma_start(out=wxb[:], in_=wxbd_d.rearrange("c p n -> p c n")), writes=[b_wxb])
            gql = sb(es, "gql", [128, 2]); b_gql = K.buf("gql")
            gkvl = sb(es, "gkvl", [128, 1]); b_gkvl = K.buf("gkvl")
            K.dma(SP, lambda: Q.dma_start(out=gql[:], in_=gql_d), writes=[b_gql])
            K.dma(SP, lambda: Q.dma_start(out=gkvl[:], in_=gkvl_d), writes=[b_gkvl])
            hstate = sb(es, "hstate", [128, 4]); b_hst = K.buf("hstate")
            K.op(DVE, lambda: V.memset(hstate[:], 0.0), writes=[b_hst])
            xt = [sb(es, f"xt{i}", [128, D]) for i in range(2)]; b_xt = [K.buf(f"xt{i}") for i in range(2)]
            ss = sb(es, "ss", [128, 2]); b_ss = K.buf("ss")
            rstd = sb(es, "rstd", [128, 1]); b_rstd = K.buf("rstd")
            h1 = sb(es, "h1", [128, D]); b_h1 = K.buf("h1")
            hb = sb(es, "hb", [128, D], BF); b_hb = K.buf("hb")
            hT = sb(es, "hT", [128, 8, 512], BF); b_hT = K.buf("hT")
            xl = [[sb(es, f"xl{i}{c}", [128, 3 + 512]) for c in range(4)] for i in range(2)]
            b_xl = [[K.buf(f"xl{i}{c}") for c in range(4)] for i in range(2)]
            gy = [[sb(es, f"gy{i}{c}", [128, 512]) for c in range(4)] for i in range(2)]
            b_gy = [[K.buf(f"gy{i}{c}") for c in range(4)] for i in range(2)]
            sq = [sb(es, f"sq{i}", [128, 512], BF) for i in range(2)]; b_sq = [K.buf(f"sq{i}") for i in range(2)]
            qraw = [sb(es, f"qraw{i}", [128, 512]) for i in range(2)]; b_qraw = [K.buf(f"qraw{i}") for i in range(2)]
            rs = sb(es, "rs", [128, 512]); b_rs = K.buf("rs")
            rt2 = sb(es, "rt2", [128, 512]); b_rt2 = K.buf("rt2")
            xc_ = [sb(es, f"xc{i}", [128, 512]) for i in range(2)]; b_xc_ = [K.buf(f"xc{i}") for i in range(2)]
            xcb_ = [sb(es, f"xcb{i}", [128, 512], BF) for i in range(2)]; b_xcb_ = [K.buf(f"xcb{i}") for i in range(2)]
            rr_ = [sb(es, f"rr{i}", [128, 512]) for i in range(2)]; b_rr_ = [K.buf(f"rr{i}") for i in range(2)]
            ii_ = [sb(es, f"ii{i}", [128, 512]) for i in range(2)]; b_ii_ = [K.buf(f"ii{i}") for i in range(2)]
            aa_ = [sb(es, f"aa{i}", [128, 512]) for i in range(2)]; b_aa_ = [K.buf(f"aa{i}") for i in range(2)]
            mm__ = [sb(es, f"mm{i}", [128, 512]) for i in range(2)]; b_mm_ = [K.buf(f"mm{i}") for i in range(2)]
            pT = ps(es, "pT", [128, D], BF); b_pT = K.pbuf("pT")
            NZ = 2
            pz = [ps(es, f"pz{i}", [128, 512]) for i in range(NZ)]; b_pz = [K.pbuf(f"pz{i}") for i in range(NZ)]
            pg_ = [[ps(es, f"pg{i}{j}", [128, 512]) for j in range(2)] for i in range(2)]
            b_pg_ = [[K.pbuf(f"pg{i}{j}") for j in range(2)] for i in range(2)]
            pss = ps(es, "pss", [128, 512]); b_pss = K.pbuf("pss")
            for c in range(4):
                K.op(DVE, lambda c=c: V.memset(xl[0][c][:, 0:3], 0.0), writes=[b_xl[0][c]])
            zi = [0]

            def norm_unit(g, tt):
                t = g * 4 + tt
                s = t % 2
                K.dma(SP, lambda: Q.dma_start(out=xt[s][:], in_=x_d[t * 128:(t + 1) * 128, :]), writes=[b_xt[s]])
                K.op(ACT, lambda: A.activation(out=h1[:], in_=xt[s][:], func=AF.Square, accum_out=ss[:, 0:1]),
                     [b_xt[s]], [b_h1, b_ss])
                rms_rstd(ss[:, 0:1], D, rstd[:], b_ss, b_ss, ss[:, 1:2], b_rstd)
                K.op(DVE, lambda: V.scalar_tensor_tensor(out=h1[:], in0=xt[s][:], scalar=rstd[:, 0:1], in1=A1,
                                                         op0=ALU.mult, op1=ALU.mult), [b_xt[s], b_rstd, b_mod[1]], [b_h1])
                K.op(DVE, lambda: V.tensor_tensor(out=hb[:], in0=h1[:], in1=SH1, op=ALU.add), [b_h1, b_mod[0]], [b_hb])

                def tr():
                    for kc in range(8):
                        last = T.transpose(pT[:, kc * 128:(kc + 1) * 128], hb[:, kc * 128:(kc + 1) * 128], ident_b[:])
                    return last
                K.op(PE, tr, [b_hb, b_ident_b], [b_pT])
                K.op(ACT, lambda: A.copy(out=hT[:, :, tt * 128:(tt + 1) * 128],
                                         in_=pT[:].rearrange("p (k n) -> p k n", k=8)), [b_pT], [b_hT])

            def inproj_unit(g, ch):
                tsl = slice(g * 512, (g + 1) * 512)
                gp = g % 2
                z = zi[0] % NZ
                zi[0] += 1
                M = 128 if ch < 11 else 64

                def mmz():
                    for kc in range(8):
                        last = T.matmul(pz[z][0:M, :], lhsT=winb[:, kc, ch * 128:ch * 128 + M], rhs=hT[:, kc, :],
                                        start=(kc == 0), stop=(kc == 7))
                    return last
                K.op(PE, mmz, [b_winb, b_hT], [b_pz[z]])
                if ch < 4:
                    c = ch
                    if g > 0:
                        K.op(DVE, lambda: V.tensor_copy(out=xl[gp][c][:, 0:3], in_=xl[1 - gp][c][:, 512:515]), [b_xl[1 - gp][c]], [b_xl[gp][c]])
                    K.op(ACT, lambda: A.copy(out=xl[gp][c][:, 3:515], in_=pz[z][:]), [b_pz[z]], [b_xl[gp][c]])
                elif ch < 8:
                    c = ch - 4
                    K.op(ACT, lambda: A.activation(out=gy[gp][c][:], in_=pz[z][:], func=AF.Gelu), [b_pz[z]], [b_gy[gp][c]])
                elif ch < 11:
                    j = ch - 8 if ch < 10 else 0
                    gcol = gql[:, j:j + 1] if ch < 10 else gkvl[:, 0:1]
                    b_g = b_gql if ch < 10 else b_gkvl
                    K.op(ACT, lambda: A.activation(out=sq[j][:], in_=pz[z][:], func=AF.Square), [b_pz[z]], [b_sq[j]])
                    K.op(DVE, lambda: V.tensor_scalar(out=qraw[j][:], in0=pz[z][:], scalar1=gcol, scalar2=None,
                                                      op0=ALU.mult), [b_pz[z], b_g], [b_qraw[j]])
                    if ch == 9 or ch == 10:
                        nch = 2 if ch == 9 else 1

                        def mms():
                            for j2 in range(nch):
                                last = T.matmul(pss[:], lhsT=ones_b[:], rhs=sq[j2][:], start=(j2 == 0), stop=(j2 == nch - 1))
                            return last
                        K.op(PE, mms, [b_ones_b] + b_sq[:nch], [b_pss])
                        rms_rstd(pss[:], 128 * nch, rs[:], b_pss, b_rt2, rt2[:], b_rs)
                        for j2 in range(nch):
                            dst = qlatn[:, j2, tsl] if ch == 9 else kvlatn[:, tsl]
                            bd = b_qlatn if ch == 9 else b_kvlatn
                            K.op(DVE, lambda j2=j2, dst=dst: V.tensor_tensor(out=dst, in0=qraw[j2][:], in1=rs[:], op=ALU.mult),
                                 [b_qraw[j2], b_rs], [bd])
                else:
                    K.op(ACT, lambda: A.copy(out=zr[:, tsl], in_=pz[z][0:64, :]), [b_pz[z]], [b_zr])

            def lru_unit(g, c):
                tsl = slice(g * 512, (g + 1) * 512)
                gp = g % 2
                q_ = c % 2
                xc, xcb, rr, ii, aa, mm_, pg = xc_[q_], xcb_[q_], rr_[q_], ii_[q_], aa_[q_], mm__[q_], pg_[q_]
                b_xc, b_xcb, b_rr, b_ii, b_aa, b_mm, b_pg = b_xc_[q_], b_xcb_[q_], b_rr_[q_], b_ii_[q_], b_aa_[q_], b_mm_[q_], b_pg_[q_]
                lv = lambda f: lruv[:, c, f:f + 1]
                K.op(ACT, lambda: A.activation(out=xc[:], in_=xl[gp][c][:, 3:515], func=AF.Identity, scale=lv(3), bias=lv(4)),
                     [b_xl[gp][c], b_lruv], [b_xc])
                for j in range(3):
                    K.op(DVE, lambda j=j: V.scalar_tensor_tensor(out=xc[:], in0=xl[gp][c][:, j:j + 512], scalar=lv(j), in1=xc[:],
                                                                 op0=ALU.mult, op1=ALU.add), [b_xl[gp][c], b_lruv], [b_xc])
                K.op(ACT, lambda: A.copy(out=xcb[:], in_=xc[:]), [b_xc], [b_xcb])
                K.op(PE, lambda: T.matmul(pg[0][:], lhsT=wab[:, c, :], rhs=xcb[:], start=True, stop=True), [b_wab, b_xcb], [b_pg[0]])
                K.op(PE, lambda: T.matmul(pg[1][:], lhsT=wxb[:, c, :], rhs=xcb[:], start=True, stop=True), [b_wxb, b_xcb], [b_pg[1]])
                K.op(ACT, lambda: A.activation(out=rr[:], in_=pg[0][:], func=AF.Sigmoid, bias=lv(5)), [b_pg[0], b_lruv], [b_rr])
                K.op(ACT, lambda: A.activation(out=ii[:], in_=pg[1][:], func=AF.Sigmoid, bias=lv(6)), [b_pg[1], b_lruv], [b_ii])
                K.op(ACT, lambda: A.activation(out=aa[:], in_=rr[:], func=AF.Exp, scale=nsp[:, c, 0:1]), [b_rr, b_nsp], [b_aa])
                K.op(ACT, lambda: A.activation(out=mm_[:], in_=rr[:], func=AF.Exp, scale=nsp[:, c, 1:2]), [b_rr, b_nsp], [b_mm])
                K.op(ACT, lambda: A.activation(out=mm_[:], in_=mm_[:], func=AF.Sqrt, scale=-1.0, bias=1.0), [b_mm], [b_mm])
                K.op(DVE, lambda: V.tensor_tensor(out=ii[:], in0=ii[:], in1=xc[:], op=ALU.mult), [b_xc], [b_ii])
                K.op(DVE, lambda: V.tensor_tensor(out=ii[:], in0=ii[:], in1=mm_[:], op=ALU.mult), [b_mm], [b_ii])
                K.op(DVE, lambda: V.tensor_tensor_scan(out=rr[:], data0=aa[:], data1=ii[:], initial=hstate[:, c:c + 1],
                                                       op0=ALU.mult, op1=ALU.add), [b_aa, b_ii, b_hst], [b_rr])
                K.op(DVE, lambda: V.tensor_copy(out=hstate[:, c:c + 1], in_=rr[:, 511:512]), [b_rr], [b_hst])
                K.op(DVE, lambda: V.tensor_tensor(out=catL[:, c, tsl], in0=rr[:], in1=gy[gp][c][:], op=ALU.mult),
                     [b_rr, b_gy[gp][c]], [b_cat[c]])

            def x_units(g):
                return [lambda tt=tt: norm_unit(g, tt) for tt in range(4)] + [lambda ch=ch: inproj_unit(g, ch) for ch in range(12)]

            for u in x_units(0):
                u()
            if stage == "B":
                K.barrier()
                dbg_dump(hT[:, 0, :], 128, 512, b_hT)
                return finish()
            if stage == "C":
                K.barrier()
                dbg_dump(xl[0][0][:, 3:515], 128, 512, b_xl[0][0])
                dbg_dump(qlatn[:, 0, 0:512], 128, 512, b_qlatn, row0=128)
                return finish()
            for g in range(8):
                nx = x_units(g + 1) if g + 1 < 8 else []
                for c2 in range(2):
                    ra0 = K.record(lambda: lru_unit(g, 2 * c2))
                    ra1 = K.record(lambda: lru_unit(g, 2 * c2 + 1))
                    rb = K.record(lambda: [u() for u in nx[8 * c2:8 * c2 + 8]])
                    K.play(ra0, ra1, rb) if rb else K.play(ra0, ra1)
            if stage == "D":
                for c in range(4):
                    dbg_dump(catL[:, c, 0:1024], 128, 1024, b_cat[c], row0=c * 128)
                return finish()
            K.barrier()

        catA_stack = ExitStack()
        catA = sb(catA_stack, "catA", [128, 4, S], BF)
        with ExitStack() as es:
            wuqb = sb(es, "wuqb", [128, 2, 8, 128], BF); b_wuqb = K.buf("wuqb")
            wukb = sb(es, "wukb", [128, 8, 128], BF); b_wukb = K.buf("wukb")
            wuvb = sb(es, "wuvb", [128, 512], BF); b_wuvb = K.buf("wuvb")
            for h in range(8):
                K.dma(POOL, lambda h=h: G.dma_start(out=wuqb[:, :, h, :], in_=wuq_d[h].rearrange("(kc p) n -> p kc n", p=128)), writes=[b_wuqb])
            K.dma(POOL, lambda: G.dma_start(out=wukb[:], in_=wuk_d.rearrange("h p n -> p h n")), writes=[b_wukb])
            K.dma(POOL, lambda: G.dma_start(out=wuvb[:], in_=wuv_d), writes=[b_wuvb])
            gq = sb(es, "gq", [128, 1]); b_gq = K.buf("gq")
            gk = sb(es, "gk", [128, 1]); b_gk = K.buf("gk")
            rtab = sb(es, "rtab", [64, 3]); b_rtab = K.buf("rtab")
            K.dma(SP, lambda: Q.dma_start(out=gq[:], in_=gq_d), writes=[b_gq])
            K.dma(SP, lambda: Q.dma_start(out=gk[:], in_=gk_d), writes=[b_gk])
            K.dma(SP, lambda: Q.dma_start(out=rtab[:], in_=rtab_d), writes=[b_rtab])
            esel = ident_b
            Tb = sb(es, "Tb", [64, S], BF); b_Tb = K.buf("Tb")
            with ExitStack() as es2:
                CW = 1024
                posi = sb(es2, "posi", [64, CW], I32); b_posi = K.buf("posi")
                tt_ = sb(es2, "tt_", [64, CW]); b_tt = K.buf("tt")
                kf = sb(es2, "kf", [64, CW]); b_kf = K.buf("kf")
                ki = sb(es2, "ki", [64, CW], I32); b_ki = K.buf("ki")
                for cc in range(S // CW):
                    csl = slice(cc * CW, (cc + 1) * CW)
                    K.dma(SP, lambda csl=csl: Q.dma_start(out=posi[:], in_=pos_d[:, csl]), writes=[b_posi])
                    K.op(DVE, lambda: V.tensor_copy(out=tt_[:], in_=posi[:]), [b_posi], [b_tt])
                    K.op(DVE, lambda: V.tensor_scalar(out=tt_[:], in0=tt_[:], scalar1=rtab[:, 0:1], scalar2=rtab[:, 1:2],
                                                      op0=ALU.mult, op1=ALU.add), [b_rtab], [b_tt])
                    K.op(DVE, lambda: V.tensor_scalar(out=kf[:], in0=tt_[:], scalar1=1.0 / TWO_PI, scalar2=None, op0=ALU.mult), [b_tt], [b_kf])
                    K.op(DVE, lambda: V.tensor_copy(out=ki[:], in_=kf[:]), [b_kf], [b_ki])
                    K.op(DVE, lambda: V.tensor_copy(out=kf[:], in_=ki[:]), [b_ki], [b_kf])
                    K.op(DVE, lambda: V.scalar_tensor_tensor(out=tt_[:], in0=kf[:], scalar=-TWO_PI, in1=tt_[:], op0=ALU.mult, op1=ALU.add),
                         [b_kf], [b_tt])
                    K.op(DVE, lambda: V.tensor_scalar(out=kf[:], in0=tt_[:], scalar1=math.pi, scalar2=-TWO_PI, op0=ALU.is_gt, op1=ALU.mult),
                         [b_tt], [b_kf])
                    K.op(DVE, lambda: V.tensor_tensor(out=tt_[:], in0=tt_[:], in1=kf[:], op=ALU.add), [b_kf], [b_tt])
                    K.op(DVE, lambda: V.tensor_scalar(out=tt_[:], in0=tt_[:], scalar1=-math.pi, scalar2=math.pi, op0=ALU.max, op1=ALU.min),
                         [], [b_tt])
                    K.op(ACT, lambda csl=csl: A.activation(out=Tb[:, csl], in_=tt_[:], func=AF.Sin), [b_tt], [b_Tb])
                    K.op(DVE, lambda csl=csl: V.tensor_scalar(out=Tb[:, csl], in0=Tb[:, csl], scalar1=rtab[:, 2:3], scalar2=None, op0=ALU.mult),
                         [b_rtab], [b_Tb])
                K.barrier()
            qT = [sb(es, f"qT{i}", [128, S], BF) for i in range(2)]; b_qT = [K.buf(f"qT{i}") for i in range(2)]
            kT = [sb(es, f"kT{i}", [128, S], BF) for i in range(2)]; b_kT = [K.buf(f"kT{i}") for i in range(2)]
            Va = [sb(es, f"Va{i}", [128, NT, 128], BF) for i in range(2)]; b_Va = [K.buf(f"Va{i}") for i in range(2)]
            for i in range(2):
                K.op(POOL, lambda i=i: G.memset(Va[i][:, :, 64:128], 1.0), writes=[b_Va[i]])
                K.op(DVE, lambda i=i: V.memset(qT[i][0:32, :], 0.0), writes=[b_qT[i]])
                K.op(DVE, lambda i=i: V.memset(kT[i][0:32, :], 0.0), writes=[b_kT[i]])
            sqa = sb(es, "sqa", [128, 512], BF); b_sqa = K.buf("sqa")
            rsa = sb(es, "rsa", [128, 512]); b_rsa = K.buf("rsa")
            qn = sb(es, "qn", [128, 512]); b_qn = K.buf("qn")
            tmp2 = sb(es, "tmp2", [128, 512]); b_tmp2 = K.buf("tmp2")
            PT = [sb(es, f"PT{i}", [128, 2, 512], BF) for i in range(2)]; b_PT = [K.buf(f"PT{i}") for i in range(2)]
            rd = sb(es, "rd", [64, 512]); b_rd = K.buf("rd")
            pq = ps(es, "pq", [128, 512]); b_pq = K.pbuf("pq")
            pssa = ps(es, "pssa", [128, 512]); b_pssa = K.pbuf("pssa")
            pv = pssa[:].rearrange("p (a b) -> p a b", a=8); b_pv = b_pssa
            pS = [ps(es, f"pS{i}", [128, 2, 512]) for i in range(2)]; b_pS = [K.pbuf(f"pS{i}") for i in range(2)]
            pO = [ps(es, f"pO{i}", [128, 512]) for i in range(2)]; b_pO = [K.pbuf(f"pO{i}") for i in range(2)]
            SCALE = math.sqrt(96.0)
            S_REP = 1
            def prep_units(h):
                hb_ = h % 2
                phases = []
                for tg in range(4):
                    def uv_a(tg=tg):
                        def mmv():
                            for j in range(8):
                                t = tg * 8 + j
                                last = T.matmul(pv[:, j, :], lhsT=kvlatn[:, t * 128:(t + 1) * 128], rhs=wuvb[:, h * 64:(h + 1) * 64],
                                                start=True, stop=True)
                            return last
                        K.op(PE, mmv, [b_kvlatn, b_wuvb], [b_pv])

                    def uv_b(tg=tg):
                        K.op(ACT, lambda: A.copy(out=Va[hb_][:, tg * 8:(tg + 1) * 8, 0:64], in_=pv), [b_pv], [b_Va[hb_]])
                    phases += [uv_a, uv_b]
                for g in range(8):
                    for which in range(2):
                        tsl = slice(g * 512, (g + 1) * 512)
                        if which == 0:
                            gcol, b_gc, dstT, b_dst = gq, b_gq, qT[hb_], b_qT[hb_]
                        else:
                            gcol, b_gc, dstT, b_dst = gk, b_gk, kT[hb_], b_kT[hb_]

                        def ua(tsl=tsl, which=which):
                            if which == 0:
                                def mmq():
                                    T.matmul(pq[:], lhsT=wuqb[:, 0, h, :], rhs=qlatn[:, 0, tsl], start=True, stop=False)
                                    return T.matmul(pq[:], lhsT=wuqb[:, 1, h, :], rhs=qlatn[:, 1, tsl], start=False, stop=True)
                                K.op(PE, mmq, [b_wuqb, b_qlatn], [b_pq])
                            else:
                                def mmk():
                                    T.matmul(pq[:], lhsT=wukb[:, h, :], rhs=kvlatn[:, tsl], start=True, stop=False)
                                    return T.matmul(pq[:], lhsT=esel[0:64, :], rhs=zr[:, tsl], start=False, stop=True)
                                K.op(PE, mmk, [b_wukb, b_kvlatn, b_ident_b, b_zr], [b_pq])

                        def ub():
                            K.op(ACT, lambda: A.activation(out=sqa[:], in_=pq[:], func=AF.Square), [b_pq], [b_sqa])
                            K.op(PE, lambda: T.matmul(pssa[:], lhsT=sel_b[:], rhs=sqa[:], start=True, stop=True), [b_sel, b_sqa], [b_pssa])

                        def uc(tsl=tsl, gcol=gcol, b_gc=b_gc, dstT=dstT, b_dst=b_dst):
                            K.op(ACT, lambda: A.activation(out=tmp2[:], in_=pssa[:], func=AF.Ln, bias=96.0 * EPS), [b_pssa], [b_tmp2])
                            K.op(ACT, lambda: A.activation(out=rsa[:], in_=tmp2[:], func=AF.Exp, scale=-0.5), [b_tmp2], [b_rsa])
                            K.op(DVE, lambda: V.scalar_tensor_tensor(out=qn[:], in0=pq[:], scalar=gcol[:, 0:1], in1=rsa[:],
                                                                     op0=ALU.mult, op1=ALU.mult), [b_pq, b_gc, b_rsa], [b_qn])
                            K.op(DVE, lambda: V.tensor_tensor(out=qn[0:64, :], in0=qn[0:64, :], in1=Tb[:, tsl], op=ALU.mult), [b_Tb], [b_qn])
                            K.op(DVE, lambda: V.tensor_copy(out=tmp2[32:64, :], in_=qn[0:32, :]), [b_qn], [b_tmp2])
                            K.op(DVE, lambda: V.tensor_tensor(out=dstT[32:64, tsl], in0=qn[32:64, :], in1=tmp2[32:64, :], op=ALU.add),
                                 [b_qn, b_tmp2], [b_dst])
                            K.op(POOL, lambda: G.tensor_copy(out=dstT[64:128, tsl], in_=qn[64:128, :]), [b_qn], [b_dst])
                        phases += [ua, ub, uc]
                return phases

            for u in prep_units(0):
                u()
            if stage == "E0":
                K.barrier()
                dbg_dump(qT[0][:, 0:1024], 128, 1024, b_qT[0], row0=0)
                dbg_dump(kT[0][:, 0:1024], 128, 1024, b_kT[0], row0=128)
                dbg_dump(Va[0][:, 0:8, :].rearrange("p a b -> p (a b)"), 128, 1024, b_Va[0], row0=256)
                return finish()
            oi = 0
            for h in range(8):
                hb_ = h % 2
                nxt = prep_units(h + 1) if h + 1 < 8 else []
                steps = [(qg, kp) for qg in range(8) for kp in range(2 * qg + 2)]
                every = 1 if nxt else 0

                def geom(qg, kt):
                    n0 = 0 if kt < 4 * qg else 128 * (kt - 4 * qg)
                    return n0, 512 - n0

                def emit_S(i):
                    qg, kp = steps[i]
                    s = i % 2

                    def mmS():
                        for j in range(2):
                            kt = 2 * kp + j
                            n0, w = geom(qg, kt)
                            last = T.matmul(pS[s][:, j, 0:w], lhsT=kT[hb_][:, kt * 128:(kt + 1) * 128],
                                            rhs=qT[hb_][:, qg * 512 + n0:(qg + 1) * 512], start=True, stop=True)
                        return last
                    K.op(PE, mmS, [b_kT[hb_], b_qT[hb_]], [b_pS[s]])
                emit_S(0)
                o = oi % 2
                for i, (qg, kp) in enumerate(steps):
                    s = i % 2
                    nkt = 4 * qg + 4
                    diag = kp >= 2 * qg
                    if kp == 0:
                        o = oi % 2
                        oi += 1
                    if i + 1 < len(steps):
                        emit_S(i + 1)
                    if not diag:
                        K.op(ACT, lambda: A.activation(out=PT[s][:].rearrange("p a b -> p (a b)"), in_=pS[s][:].rearrange("p a b -> p (a b)"),
                                                       func=AF.Exp, scale=SCALE), [b_pS[s]], [b_PT[s]])
                    else:
                        for j in range(2):
                            n0, w = geom(qg, 2 * kp + j)
                            K.op(ACT, lambda j=j, w=w: A.activation(out=PT[s][:, j, 0:w], in_=pS[s][:, j, 0:w], func=AF.Exp, scale=SCALE),
                                 [b_pS[s]], [b_PT[s]])
                        for j in range(2):
                            K.op(DVE, lambda j=j: V.tensor_tensor(out=PT[s][:, j, 0:128], in0=PT[s][:, j, 0:128], in1=tri_b[:], op=ALU.mult),
                                 [b_tri], [b_PT[s]])

                    def mmPV():
                        for j in range(2):
                            kt = 2 * kp + j
                            n0, w = geom(qg, kt)
                            last = T.matmul(pO[o][:, n0:512], lhsT=Va[hb_][:, kt, :], rhs=PT[s][:, j, 0:w],
                                            start=(kt == 0), stop=(kt == nkt - 1))
                        return last
                    K.op(PE, mmPV, [b_Va[hb_], b_PT[s]], [b_pO[o]])
                    if kp == 2 * qg + 1:
                        K.op(DVE, lambda: V.reciprocal(out=rd[:], in_=pO[o][64:128, :]), [b_pO[o]], [b_rd])
                        ch = 4 + h // 2
                        p0 = (h % 2) * 64
                        K.op(DVE, lambda: V.tensor_tensor(out=catA[p0:p0 + 64, ch - 4, qg * 512:(qg + 1) * 512],
                                                          in0=pO[o][0:64, :], in1=rd[:], op=ALU.mult),
                             [b_pO[o], b_rd], [b_cat[ch]])
                    if nxt and every and i % every == every - 1 and (i // every) < len(nxt):
                        nxt[i // every]()
                for j in range((len(steps) // every) if every else 0, len(nxt)):
                    nxt[j]()
            if stage == "E":
                K.barrier()
                for c in range(4):
                    dbg_dump(catA[:, c, 0:1024], 128, 1024, b_cat[4 + c], row0=c * 128)
                return finish()
            K.barrier()

    with ExitStack() as es:
        wob = sb(es, "wob", [128, 8, D], BF); b_wob = K.buf("wob")
        wst2 = [sb(es, f"wst2{i}", [128, D]) for i in range(2)]; b_wst2 = [K.buf(f"wst2{i}") for i in range(2)]
        wout_v = wout_d.rearrange("(kc p) n -> p kc n", p=128)
        for kc in range(8):
            s = kc % 2
            K.dma(SP, lambda kc=kc, s=s: Q.dma_start(out=wst2[s][:], in_=wout_v[:, kc, :]), writes=[b_wst2[s]])
            K.op(DVE, lambda kc=kc, s=s: V.tensor_tensor(out=wob[:, kc, :], in0=wst2[s][:], in1=GATE1, op=ALU.mult),
                 [b_wst2[s], b_mod[2]], [b_wob])
        wrt = sb(es, "wrt", [128, 8, NE]); b_wrt = K.buf("wrt")
        brt = sb(es, "brt", [1, NE]); b_brt = K.buf("brt")
        K.dma(SP, lambda: Q.dma_start(out=wrt[:], in_=wr_d.rearrange("(kc p) n -> p kc n", p=128)), writes=[b_wrt])
        K.dma(SP, lambda: Q.dma_start(out=brt[:], in_=br_d), writes=[b_brt])
        mask_all = sb(es, "mask_all", [128, NT, NE], BF); b_mask = [K.buf(f"mask{t}") for t in range(NT)]
        VAL = sb(es, "VAL", [128, NT, 4], I32); b_VAL = K.buf("VAL")
        K.op(POOL, lambda: G.iota(VAL[:], pattern=[[512, NT], [1, 4]], base=0, channel_multiplier=4), writes=[b_VAL])
        tinit = sb(es, "tinit", [128, 1024], I32); b_tinit = K.buf("tinit")
        K.op(POOL, lambda: G.iota(tinit[:], pattern=[[0, 1024]], base=1 << 30, channel_multiplier=0), writes=[b_tinit])
        K.dma(POOL, lambda: G.dma_start(out=Tab_d.rearrange("(p n) o -> p (n o)", p=128), in_=tinit[:]), [b_tinit], [b_Tab])
        H2r_v = H2r_d.rearrange("(n r) d -> n r d", r=4)
        TC = 8
        xt = [sb(es, f"xtf{i}", [128, D]) for i in range(2)]; b_xt = [K.buf(f"xtf{i}") for i in range(2)]
        x1 = [sb(es, f"x1{i}", [128, D]) for i in range(2)]; b_x1 = [K.buf(f"x1{i}") for i in range(2)]
        junk = sb(es, "junkf", [128, D], BF); b_junk = K.buf("junkf")
        ss = sb(es, "ssf", [128, 2]); b_ss = K.buf("ssf")
        rstd = sb(es, "rstdf", [128, 1]); b_rstd = K.buf("rstdf")
        h2f = [sb(es, f"h2f{i}", [128, D]) for i in range(2)]; b_h2f = [K.buf(f"h2f{i}") for i in range(2)]
        h2b = [sb(es, f"h2b{i}", [128, D], BF) for i in range(2)]; b_h2b = [K.buf(f"h2b{i}") for i in range(2)]
        h2T = sb(es, "h2T", [128, 8, 128]); b_h2T = K.buf("h2T")
        lg_all = sb(es, "lg_all", [128, NT, NE]); b_lg = [K.buf(f"lg{c}") for c in range(NT // TC)]
        top8_all = sb(es, "top8_all", [128, NT, 8]); b_top8 = [K.buf(f"top8{c}") for c in range(NT // TC)]
        d4 = sb(es, "d4", [128, TC, 4]); b_d4 = K.buf("d4")
        den = sb(es, "den", [128, TC]); b_den = K.buf("den")
        oh = sb(es, "oh", [128, 4, TC, NE]); b_oh = K.buf("oh")
        mk = sb(es, "mk", [128, TC, NE]); b_mk = K.buf("mk")
        idxfull = sb(es, "idxfull", [128, TC, NE]); b_idxf = K.buf("idxf")
        junk2 = sb(es, "junk2", [128, TC, NE]); b_junk2 = K.buf("junk2")
        IDXf = sb(es, "IDXf", [128, NT, 4]); b_idx4 = K.buf("idx4")
        cntf = sb(es, "cntf", [1, NE]); b_cntf = K.buf("cntf")
        pm = [ps(es, f"pm{i}", [128, D]) for i in range(2)]; b_pm = [K.pbuf(f"pm{i}") for i in range(2)]
        pT32 = ps(es, "pT32", [128, D]); b_pT32 = K.pbuf("pT32")
        pl = ps(es, "pl", [128, NE]); b_pl = K.pbuf("pl")
        ppos = ps(es, "ppos", [128, TC, NE]); b_ppos = K.pbuf("ppos")
        pcnt = pl[0:1, :]; b_pcnt = b_pl
        AXX = mybir.AxisListType.X

        def load_x(t):
            K.dma(SP, lambda: Q.dma_start(out=xt[t % 2][:], in_=x_d[t * 128:(t + 1) * 128, :]), writes=[b_xt[t % 2]])

        def route_chunk(c):
            t0 = c * TC
            sl = slice(t0, t0 + TC)
            bl, bt = b_lg[c], b_top8[c]
            K.op(DVE, lambda: V.tensor_tensor(out=d4[:], in0=top8_all[:, sl, 0:4], in1=top8_all[:, sl, 0:1].to_broadcast([128, TC, 4]),
                                              op=ALU.subtract), [bt], [b_d4])
            K.op(ACT, lambda: A.activation(out=d4[:], in_=d4[:], func=AF.Exp), [], [b_d4])
            K.op(DVE, lambda: V.tensor_reduce(out=den[:], in_=d4[:], axis=AXX, op=ALU.add), [b_d4], [b_den])
            K.op(DVE, lambda: V.reciprocal(out=den[:], in_=den[:]), [], [b_den])
            K.op(DVE, lambda: V.tensor_tensor(out=G_all[:, sl, :], in0=d4[:], in1=den[:].to_broadcast([128, TC, 4]) if False else
                                              den[:].rearrange("p (t o) -> p t o", o=1).to_broadcast([128, TC, 4]), op=ALU.mult),
                 [b_d4, b_den], [b_G])
            for r in range(4):
                K.op(DVE, lambda r=r: V.tensor_tensor(out=oh[:, r, :, :], in0=lg_all[:, sl, :],
                                                      in1=top8_all[:, sl, r:r + 1].to_broadcast([128, TC, NE]), op=ALU.is_equal),
                     [bl, bt], [b_oh])
            K.op(DVE, lambda: V.tensor_tensor(out=mk[:], in0=oh[:, 0, :, :], in1=oh[:, 1, :, :], op=ALU.add), [b_oh], [b_mk])
            K.op(DVE, lambda: V.tensor_tensor(out=mk[:], in0=mk[:], in1=oh[:, 2, :, :], op=ALU.add), [b_oh], [b_mk])
            K.op(DVE, lambda: V.tensor_tensor(out=mask_all[:, sl, :], in0=mk[:], in1=oh[:, 3, :, :], op=ALU.add), [b_oh, b_mk], b_mask[t0:t0 + TC])

            def mmp():
                for j in range(TC):
                    t = t0 + j
                    last = T.matmul(ppos[:, j, :], lhsT=U_b[:], rhs=mask_all[:, t, :], start=True, stop=(t == 0))
                    for i in range(t):
                        last = T.matmul(ppos[:, j, :], lhsT=ones_b[:], rhs=mask_all[:, i, :], start=False, stop=(i == t - 1))
                return last
            K.op(PE, mmp, [b_U, b_ones_b] + b_mask[:t0 + TC], [b_ppos])
            K.op(DVE, lambda: V.tensor_tensor(out=idxfull[:], in0=ppos[:], in1=ebase[:].rearrange("p (o e) -> p o e", o=1).to_broadcast([128, TC, NE]),
                                              op=ALU.add), [b_ppos, b_ebase], [b_idxf])
            for r in range(4):
                K.op(DVE, lambda r=r: V.tensor_tensor(out=junk2[:], in0=oh[:, r, :, :], in1=idxfull[:], op=ALU.mult), [b_oh, b_idxf], [b_junk2])
                K.op(DVE, lambda r=r: V.tensor_reduce(out=IDXf[:, sl, r], in_=junk2[:], axis=AXX, op=ALU.add), [b_junk2], [b_idx4])
            K.op(DVE, lambda: V.tensor_copy(out=IDX[:, sl, :], in_=IDXf[:, sl, :]), [b_idx4], [b_IDX])
            K.dma(POOL, lambda: [G.indirect_dma_start(out=Tab_d, out_offset=IOA(ap=IDX[:, t, r:r + 1], axis=0),
                                                      in_=VAL[:, t, r:r + 1], in_offset=None)
                                 for t in range(t0, t0 + TC) for r in range(4)],
                  [b_VAL, b_IDX], [b_Tab])

        def stage_a(t):
            s = t % 2
            rows = slice(t * 128, (t + 1) * 128)
            if t + 1 < NT:
                load_x(t + 1)

            def mmo():
                for half in range(2):
                    for kc in range(8):
                        last = T.matmul(pm[s][:, half * 512:(half + 1) * 512], lhsT=(catL[:, kc, rows] if kc < 4 else catA[:, kc - 4, rows]),
                                        rhs=wob[:, kc, half * 512:(half + 1) * 512], start=(kc == 0), stop=(kc == 7))
                return last
            K.op(PE, mmo, b_cat + [b_wob], [b_pm[s]])
            K.op(DVE, lambda: V.tensor_tensor(out=x1[s][:], in0=pm[s][:], in1=xt[s][:], op=ALU.add), [b_pm[s], b_xt[s]], [b_x1[s]])
            K.dma(ACT, lambda: A.dma_start(out=X1_d[rows, :], in_=x1[s][:]), [b_x1[s]], [b_X1])
            if stage == "F1":
                dbg_dump(x1[s][:], 128, D, b_x1[s], row0=t * 128)
                return
            K.op(ACT, lambda: A.activation(out=junk[:], in_=x1[s][:], func=AF.Square, accum_out=ss[:, 0:1]), [b_x1[s]], [b_junk, b_ss])
            rms_rstd(ss[:, 0:1], D, rstd[:], b_ss, b_ss, ss[:, 1:2], b_rstd)
            K.op(DVE, lambda: V.scalar_tensor_tensor(out=h2f[s][:], in0=x1[s][:], scalar=rstd[:, 0:1], in1=A2, op0=ALU.mult, op1=ALU.mult),
                 [b_x1[s], b_rstd, b_mod[4]], [b_h2f[s]])
            K.op(DVE, lambda: V.tensor_tensor(out=h2f[s][:], in0=h2f[s][:], in1=SH2, op=ALU.add), [b_mod[3]], [b_h2f[s]])
            K.op(POOL, lambda: G.tensor_copy(out=h2b[s][:], in_=h2f[s][:]), [b_h2f[s]], [b_h2b[s]])
            K.dma(POOL, lambda: [G.dma_start(out=H2r_v[rows, r, :], in_=h2b[s][:]) for r in range(4)], [b_h2b[s]], [b_H2r])

        def stage_b(t):
            s = t % 2
            c = t // TC

            def tr32():
                for kc in range(8):
                    last = T.transpose(pT32[:, kc * 128:(kc + 1) * 128], h2f[s][:, kc * 128:(kc + 1) * 128], ident_f[:])
                return last
            K.op(PE, tr32, [b_h2f[s], b_ident_f], [b_pT32])
            K.op(ACT, lambda: A.copy(out=h2T[:].rearrange("p k n -> p (k n)"), in_=pT32[:]), [b_pT32], [b_h2T])

            def mmr():
                for kc in range(8):
                    T.matmul(pl[:], lhsT=h2T[:, kc, :], rhs=wrt[:, kc, :], start=(kc == 0), stop=False)
                return T.matmul(pl[:], lhsT=ones_f[0:1, :], rhs=brt[0:1, :], start=False, stop=True)
            K.op(PE, mmr, [b_h2T, b_wrt, b_ones_f, b_brt], [b_pl])
            K.op(DVE, lambda: V.tensor_copy(out=lg_all[:, t, :], in_=pl[:]), [b_pl], [b_lg[c]])
            K.op(DVE, lambda: V.max(out=top8_all[:, t, :], in_=lg_all[:, t, :]), [b_lg[c]], [b_top8[c]])

        load_x(0)
        stage_a(0)
        pending = None
        for t in range(NT):
            if stage == "F1":
                if t + 1 < NT:
                    stage_a(t + 1)
                continue
            streams = []
            if t + 1 < NT:
                streams.append(K.record(lambda: stage_a(t + 1)))
            streams.append(K.record(lambda: stage_b(t)))
            if pending is not None:
                streams.append(pending)
                pending = None
            K.play(*streams)
            if t % TC == TC - 1:
                pending = K.record(lambda: route_chunk(t // TC))
        if pending is not None:
            K.play(pending)
        if stage == "F1":
            return finish()

        def mmc():
            for t in range(NT):
                last = T.matmul(pcnt, lhsT=ones_b[:, 0:1], rhs=mask_all[:, t, :], start=(t == 0), stop=(t == NT - 1))
            return last
        K.op(PE, mmc, [b_ones_b] + b_mask, [b_pcnt])
        K.op(DVE, lambda: V.tensor_copy(out=cntf[:], in_=pcnt), [b_pcnt], [b_cntf])
        K.op(DVE, lambda: V.tensor_copy(out=cnt_i[:], in_=cntf[:]), [b_cntf], [b_cnt])
        if stage == "F":
            K.barrier()
            K.op(DVE, lambda: V.tensor_copy(out=h2f[0][:, 0:4 * NT], in_=IDX[:].rearrange("p a b -> p (a b)")), [b_IDX], [b_h2f[0]])
            dbg_dump(h2f[0][:, 0:4 * NT], 128, 4 * NT, b_h2f[0], row0=0)
            dbg_dump(G_all[:].rearrange("p a b -> p (a b)"), 128, 4 * NT, b_G, row0=128)
            dbg_dump(cntf[:], 1, NE, b_cntf, row0=256)
            return finish()
        K.barrier()

    catA_stack.close()
    es_mix.close()
    cat_stack.close()
    K.barrier()
    mod_stack.close()
    with ExitStack() as es:
        NW = 3
        w1b = [sb(es, f"w1b{i}", [128, 8, 2 * D], BF) for i in range(NW)]
        b_w1b = [[K.buf(f"w1b{i}")] for i in range(NW)]
        w2b = [sb(es, f"w2b{i}", [128, 8, D], BF) for i in range(NW)]
        b_w2b = [[K.buf(f"w2b{i}")] for i in range(NW)]
        b1all = sb(es, "b1all", [96, 2 * D], BF); b_b1b = [K.buf(f"b1b{i}") for i in range(NW)]
        b2all = sb(es, "b2all", [96, D], BF); b_b2b = [K.buf(f"b2b{i}") for i in range(NW)]
        NS = 4
        xe = [[sb(es, f"xe{i}{b}", [128, D], BF) for b in range(2)] for i in range(NS)]
        b_xe = [[K.buf(f"xe{i}{b}") for b in range(2)] for i in range(NS)]
        tb = [[sb(es, f"tb{i}{b}", [128, 1], I32) for b in range(2)] for i in range(NS)]
        b_tb = [[K.buf(f"tb{i}{b}") for b in range(2)] for i in range(NS)]
        for i in range(NS):
            for b in range(2):
                K.op(DVE, lambda i=i, b=b: V.memset(xe[i][b][:], 0.0), writes=[b_xe[i][b]])
        xT2 = [[sb(es, f"xT{i}{b}", [128, 8, 128], BF) for b in range(2)] for i in range(2)]
        b_xT2 = [[K.buf(f"xT{i}{b}") for b in range(2)] for i in range(2)]
        xT = [xT2[i % 2] for i in range(NS)]
        b_xT = [b_xT2[i % 2] for i in range(NS)]
        glu1 = sb(es, "glu", [128, 512]); glu = [glu1, glu1]; b_glu1 = K.buf("glu"); b_glu = [b_glu1, b_glu1]
        sg1 = sb(es, "sg", [128, 512]); sg = [sg1, sg1]; b_sg1 = K.buf("sg"); b_sg = [b_sg1, b_sg1]
        lin1 = sb(es, "lin", [128, 512]); lin = [lin1, lin1]; b_lin1 = K.buf("lin"); b_lin = [b_lin1, b_lin1]
        actb = [sb(es, f"actb{b}", [128, D], BF) for b in range(2)]; b_actb = [K.buf(f"actb{b}") for b in range(2)]
        aT1 = sb(es, "aT", [128, 8, 128], BF); aT = [aT1, aT1]; b_aT1 = K.buf("aT"); b_aT = [b_aT1, b_aT1]
        yo = [sb(es, f"yo{i}", [128, D]) for i in range(2)]; b_yo = [K.buf(f"yo{i}") for i in range(2)]
        pTx = ps(es, "pTx", [128, D], BF); b_pTx = K.pbuf("pTx")
        pgu = [ps(es, f"pgu{i}", [128, D]) for i in range(2)]; b_pgu = [K.pbuf(f"pgu{i}") for i in range(2)]
        pTa = ps(es, "pTa", [128, D], BF); b_pTa = K.pbuf("pTa")
        py = ps(es, "py", [128, D]); b_py = K.pbuf("py")
        NPAIR = nbmax // 2
        GRP = 4

        def load_w(e):
            s = e % NW
            v1 = w1_d[e].rearrange("(kc p) n -> p kc n", p=128)
            v2 = w2_d[e].rearrange("(kc p) n -> p kc n", p=128)
            K.dma(POOL, lambda: [G.dma_start(out=w1b[s][:, kc, :], in_=v1[:, kc, :]) for kc in range(8)], writes=[b_w1b[s][0]])
            K.dma(POOL, lambda: [G.dma_start(out=w2b[s][:, kc, :], in_=v2[:, kc, :]) for kc in range(8)], writes=[b_w2b[s][0]])
            K.dma(POOL, lambda: G.dma_start(out=b1all[32 * s:32 * s + 1, :], in_=b1_d[e:e + 1, :]), writes=[b_b1b[s]])
            K.dma(POOL, lambda: G.dma_start(out=b2all[32 * s:32 * s + 1, :], in_=b2_d[e:e + 1, :]), writes=[b_b2b[s]])

        bcreg = G.alloc_register("bcreg")
        G.reg_mov(bcreg, S * 4 - 1)

        def load_pair(e, pr, s):
            r0 = e * CAP + pr * 256
            for b in range(2):
                K.dma(SP, lambda b=b: Q.dma_start(out=tb[s][b][:], in_=Tab_d[r0 + b * 128:r0 + (b + 1) * 128, :]), [b_Tab], [b_tb[s][b]])
            for b in range(2):
                K.dma(POOL, lambda b=b: G.indirect_dma_start(out=xe[s][b][:], out_offset=None, in_=H2r_d,
                                                             in_offset=IOA(ap=tb[s][b][:, 0:1], axis=0),
                                                             bounds_check=bcreg, oob_is_err=False),
                      [b_H2r, b_tb[s][b]], [b_xe[s][b]])

        ei = [0]

        def slot_of(e, pr):
            return 2 * (e % 2) + (pr % 2)

        def p1(e, pr):
            s = slot_of(e, pr)
            for b in range(2):
                def trx(b=b):
                    for kc in range(8):
                        last = T.transpose(pTx[:, kc * 128:(kc + 1) * 128], xe[s][b][:, kc * 128:(kc + 1) * 128], ident_b[:])
                    return last
                K.op(PE, trx, [b_xe[s][b], b_ident_b], [b_pTx])
                K.op(DVE, lambda b=b: V.tensor_copy(out=xT[s][b][:].rearrange("p k n -> p (k n)"), in_=pTx[:]), [b_pTx], [b_xT[s][b]])

        def pair_body(e, pr, s, ws, after_loads=None):
            if pr + 1 < NPAIR:
                load_pair(e, pr + 1, slot_of(e, pr + 1))
            if after_loads is not None:
                after_loads()
            def stage2(b, h):
                def mm1():
                    for n, col in ((0, h * 512), (1, D + h * 512)):
                        for kc in range(8):
                            T.matmul(pgu[h][:, n * 512:(n + 1) * 512], lhsT=xT[s][b][:, kc, :], rhs=w1b[ws][:, kc, col:col + 512],
                                     start=(kc == 0), stop=False)
                        last = T.matmul(pgu[h][:, n * 512:(n + 1) * 512], lhsT=ones_b[32 * ws:32 * ws + 1, :], rhs=b1all[32 * ws:32 * ws + 1, col:col + 512],
                                        start=False, stop=True)
                    return last
                K.op(PE, mm1, [b_xT[s][b], b_b1b[ws], b_ones_b] + b_w1b[ws], [b_pgu[h]])
                r = ei[0] % 2
                ei[0] += 1
                K.op(DVE, lambda: V.tensor_scalar(out=glu[r][:], in0=pgu[h][:, 0:512], scalar1=7.0, scalar2=None, op0=ALU.min),
                     [b_pgu[h]], [b_glu[r]])
                K.op(ACT, lambda: A.activation(out=sg[r][:], in_=glu[r][:], func=AF.Sigmoid, scale=1.702), [b_glu[r]], [b_sg[r]])
                K.op(DVE, lambda: V.tensor_scalar(out=lin[r][:], in0=pgu[h][:, 512:1024], scalar1=-7.0, scalar2=7.0,
                                                  op0=ALU.max, op1=ALU.min), [b_pgu[h]], [b_lin[r]])
                K.op(DVE, lambda: V.scalar_tensor_tensor(out=lin[r][:], in0=lin[r][:], scalar=1.0, in1=glu[r][:],
                                                         op0=ALU.add, op1=ALU.mult), [b_glu[r]], [b_lin[r]])
                K.op(DVE, lambda: V.tensor_tensor(out=actb[b][:, h * 512:(h + 1) * 512], in0=lin[r][:], in1=sg[r][:], op=ALU.mult),
                     [b_lin[r], b_sg[r]], [b_actb[b]])

            def stage3(b):
                def tra():
                    for kc in range(8):
                        last = T.transpose(pTa[:, kc * 128:(kc + 1) * 128], actb[b][:, kc * 128:(kc + 1) * 128], ident_b[:])
                    return last
                K.op(PE, tra, [b_actb[b], b_ident_b], [b_pTa])
                K.op(ACT, lambda: A.copy(out=aT[b][:].rearrange("p k n -> p (k n)"), in_=pTa[:]), [b_pTa], [b_aT[b]])

            def stage4(b):
                def mm2():
                    for n in range(2):
                        for kc in range(8):
                            T.matmul(py[:, n * 512:(n + 1) * 512], lhsT=aT[b][:, kc, :], rhs=w2b[ws][:, kc, n * 512:(n + 1) * 512],
                                     start=(kc == 0), stop=False)
                        last = T.matmul(py[:, n * 512:(n + 1) * 512], lhsT=ones_b[32 * ws:32 * ws + 1, :], rhs=b2all[32 * ws:32 * ws + 1, n * 512:(n + 1) * 512],
                                        start=False, stop=True)
                    return last
                K.op(PE, mm2, [b_aT[b], b_b2b[ws], b_ones_b] + b_w2b[ws], [b_py])
                K.op(ACT, lambda: A.copy(out=yo[b][:], in_=py[:]), [b_py], [b_yo[b]])
                K.dma(POOL, lambda: G.indirect_dma_start(out=Yb_d, out_offset=IOA(ap=tb[s][b][:, 0:1], axis=0),
                                                         in_=yo[b][:], in_offset=None,
                                                         bounds_check=bcreg, oob_is_err=False),
                      [b_yo[b], b_tb[s][b]], [b_Yb])

            stage2(0, 0)
            stage2(0, 1)
            stage2(1, 0)
            stage3(0)
            stage2(1, 1)
            stage4(0)
            stage3(1)
            stage4(1)
            if pr + 1 < NPAIR:
                p1(e, pr + 1)

        if use_if:
            regs = nc.alloc_registers("cntreg")
        load_w(0)
        load_w(1)
        load_pair(0, 0, slot_of(0, 0))
        p1(0, 0)
        for e in range(NE):
            ws = e % NW
            if e + 1 < NE:
                load_pair(e + 1, 0, slot_of(e + 1, 0))
            if use_if:
                for eng in K.engs:
                    K.wait_buf(eng, b_cnt)
                for reg in regs:
                    nc.reg_load(reg, cnt_i[0:1, e:e + 1])
            pair_body(e, 0, slot_of(e, 0), ws, after_loads=(lambda: load_w(e + 2)) if e + 2 < NE else None)

            def chain(pr):
                if pr >= NPAIR:
                    return
                snap = K.snapshot()
                ctx = nc.If_cmp(regs, 256 * pr, "IS_GT") if use_if else ExitStack()
                with ctx:
                    pair_body(e, pr, slot_of(e, pr), ws)
                    chain(pr + 1)
                if use_if:
                    with nc.Else():
                        K.compensate(snap)
                    K.restore_seen(snap)
            chain(1)
            if e + 1 < NE:
                p1(e + 1, 0)
        K.barrier()

    with ExitStack() as es:
        NR = 3
        x1t = [sb(es, f"x1t{i}", [128, D]) for i in range(NR)]; b_x1t = [K.buf(f"x1t{i}") for i in range(NR)]
        yg = [sb(es, f"yg{i}", [128, 4, D]) for i in range(NR)]; b_yg = [K.buf(f"yg{i}") for i in range(NR)]
        Yb_v = Yb_d.rearrange("(n r) d -> n (r d)", r=4)
        acc = [sb(es, f"acc{i}", [128, D]) for i in range(2)]; b_acc = [K.buf(f"acc{i}") for i in range(2)]

        def loads(t):
            s = t % NR
            rows = slice(t * 128, (t + 1) * 128)
            K.dma(SP, lambda: Q.dma_start(out=yg[s][:].rearrange("p r d -> p (r d)"), in_=Yb_v[rows, :]), [b_Yb], [b_yg[s]])
            K.dma(SP, lambda: Q.dma_start(out=x1t[s][:], in_=X1_d[rows, :]), [b_X1], [b_x1t[s]])
        loads(0)
        loads(1)
        for t in range(NT):
            s = t % NR
            a = t % 2
            rows = slice(t * 128, (t + 1) * 128)
            if t + 2 < NT:
                loads(t + 2)
            K.op(DVE, lambda: V.tensor_scalar(out=acc[a][:], in0=yg[s][:, 0, :], scalar1=G_all[:, t, 0:1], scalar2=None, op0=ALU.mult),
                 [b_yg[s], b_G], [b_acc[a]])
            for r in range(1, 4):
                K.op(DVE, lambda r=r: V.scalar_tensor_tensor(out=acc[a][:], in0=yg[s][:, r, :], scalar=G_all[:, t, r:r + 1],
                                                             in1=acc[a][:], op0=ALU.mult, op1=ALU.add),
                     [b_yg[s], b_G], [b_acc[a]])
            K.op(DVE, lambda: V.tensor_tensor(out=acc[a][:], in0=acc[a][:], in1=gate2k[:], op=ALU.mult), [b_gate2k], [b_acc[a]])
            K.op(DVE, lambda: V.tensor_tensor(out=acc[a][:], in0=acc[a][:], in1=x1t[s][:], op=ALU.add), [b_x1t[s]], [b_acc[a]])
            K.dma(ACT, lambda: A.dma_start(out=out_d[rows, :], in_=acc[a][:]), [b_acc[a]], [b_out])
    return finish()


def prep_inputs(inp):
    f = np.float32
    L = 0
    g = lambda n: np.asarray(inp[n][L])
    w_in = g("w_in")
    kro = w_in[:, 1408:1440]
    perm = np.concatenate([np.arange(16, 32), np.arange(0, 16)])
    w_in_ext = np.ascontiguousarray(np.concatenate([w_in[:, :1408], kro[:, perm], kro], axis=1), dtype=f)
    fm = lambda v: np.ascontiguousarray(np.asarray(v, dtype=f).reshape(-1, 128).T)
    cw = g("conv_w")
    fields = [cw[0], cw[1], cw[2], cw[3], g("conv_b"), g("b_a"), g("b_x"), g("lam")]
    lruv = np.ascontiguousarray(np.stack([fm(v) for v in fields], axis=-1), dtype=f)

    def bd(w):
        o = np.zeros((4, 128, 128), f)
        for c in range(4):
            o[c, 0:64, 0:64] = w[2 * c]
            o[c, 64:128, 64:128] = w[2 * c + 1]
        return o
    w_uq = g("w_uq"); w_ukv = g("w_ukv")
    w_uq_h = np.zeros((8, 256, 128), f)
    w_uk_h = np.zeros((8, 128, 128), f)
    w_uv = np.zeros((128, 512), f)
    for h in range(8):
        nope = w_uq[:, h * 96:h * 96 + 64]; rope = w_uq[:, h * 96 + 64:h * 96 + 96]
        w_uq_h[h] = np.concatenate([rope[:, perm], rope, nope], axis=1)
        w_uk_h[h, :, 64:128] = w_ukv[:, h * 128:h * 128 + 64]
        w_uv[:, h * 64:(h + 1) * 64] = w_ukv[:, h * 128 + 64:h * 128 + 128]

    def grow(gv):
        return np.ascontiguousarray(np.concatenate([gv[64:96][perm], gv[64:96], gv[0:64]]).reshape(128, 1), dtype=f)
    half = 16
    freqs = (10000.0 ** (-np.arange(half, dtype=np.float64) / half)).astype(f)
    fr32 = np.concatenate([freqs, freqs])
    rtab = np.zeros((64, 3), f)
    rtab[0:32, 0] = fr32; rtab[32:64, 0] = fr32
    rtab[0:32, 1] = 0.0; rtab[32:64, 1] = math.pi / 2
    rtab[0:16, 2] = -1.0; rtab[16:32, 2] = 1.0; rtab[32:64, 2] = 1.0
    shared = {
        "w_ada": g("w_ada"), "b_ada": g("b_ada").reshape(1, -1),
        "g_mix_bc": np.ascontiguousarray(np.broadcast_to(g("g_mix")[None, :], (128, D))),
        "g_ffn_bc": np.ascontiguousarray(np.broadcast_to(g("g_ffn")[None, :], (128, D))),
        "w_in_ext": w_in_ext, "lruv": lruv, "wa_bd": bd(g("w_a")), "wx_bd": bd(g("w_x")),
        "g_q_lat": fm(g("g_q_lat")), "g_kv_lat": fm(g("g_kv_lat")),
        "w_uq_h": w_uq_h, "w_uk_h": w_uk_h, "w_uv": w_uv, "gq": grow(g("g_qn")), "gk": grow(g("g_kn")),
        "rtab": rtab, "w_out": g("w_out"), "w_router": g("w_router"), "b_router": g("b_router").reshape(1, -1),
        "w1": g("w1"), "b1": g("b1"), "w2": g("w2"), "b2": g("b2"),
    }
    shared = {k: np.ascontiguousarray(v, dtype=f) for k, v in shared.items()}
    maps = []
    for b in range(8):
        m = dict(shared)
        m["x"] = np.ascontiguousarray(inp["x"][b], dtype=f)
        m["cT"] = fm(np.asarray(inp["c"][b]))
        m["pos"] = np.ascontiguousarray(np.broadcast_to(np.asarray(inp["positions"][b], dtype=np.int32)[None, :], (64, S)))
        maps.append(m)
    return maps


def kernel(**inputs):
    maps = prep_inputs(inputs)
    nc = build_nc("full")
    res = run_bass_kernel_spmd(nc, maps, core_ids=list(range(8)))
    return np.stack([np.asarray(r["out"], dtype=np.float32) for r in res.results], axis=0)
```

```python
import math
from contextlib import ExitStack
import numpy as np
import concourse.bass as bass
import concourse.mybir as mybir
from concourse.bass_utils import run_bass_kernel_spmd

F32 = mybir.dt.float32
BF = mybir.dt.bfloat16
I32 = mybir.dt.int32
AF = mybir.ActivationFunctionType
ALU = mybir.AluOpType
IOA = bass.IndirectOffsetOnAxis

S = 4096
D = 1024
NT = S // 128
NE = 32
CAP = 4096
NBMAX = CAP // 128
EPS = 1e-6
TWO_PI = 2.0 * math.pi


class Dep:
    def __init__(self, nc, name):
        self.sem = nc.alloc_semaphore(name)
        self.n = 0
        self.issuer = None


class Buf:
    def __init__(self, name):
        self.name = name
        self.w = None
        self.r = {}
        self.dsem = None
        self.psum = False


class Eng:
    def __init__(self, k, name, ins, has_dep=True):
        self.name = name
        self.ins = ins
        self.dep = Dep(k.nc, "c_" + name) if has_dep else None
        self.seen = {}


class Kern:
    def __init__(self, nc):
        self.nc = nc
        self.pe = Eng(self, "pe", nc.tensor)
        self.act = Eng(self, "act", nc.scalar)
        self.dve = Eng(self, "dve", nc.vector)
        self.pool = Eng(self, "pool", nc.gpsimd)
        self.sp = Eng(self, "sp", nc.sync, has_dep=False)
        self.engs = [self.pe, self.act, self.dve, self.pool, self.sp]
        self.deps = [e.dep for e in self.engs if e.dep is not None]
        self.nbuf = 0

    def buf(self, name=None):
        self.nbuf += 1
        return Buf(name or f"b{self.nbuf}")

    def pbuf(self, name):
        b = Buf(name)
        b.psum = True
        return b

    def _wait(self, eng, dep, val):
        if dep is eng.dep and eng is self.pe:
            return
        if eng.seen.get(dep, 0) < val:
            eng.ins.wait_ge(dep.sem, val)
            eng.seen[dep] = val

    def _deps_for(self, eng, reads, writes):
        for b in reads:
            if b.w is not None:
                self._wait(eng, *b.w)
        for b in writes:
            if b.w is not None:
                self._wait(eng, *b.w)
            for d, v in b.r.items():
                self._wait(eng, d, v)

    def _commit(self, tok, reads, writes):
        for b in writes:
            b.w = tok
            b.r = {}
        for b in reads:
            if b in writes:
                continue
            d, v = tok
            if b.r.get(d, 0) < v:
                b.r[d] = v

    def record(self, thunk):
        self.rec = []
        thunk()
        r, self.rec = self.rec, None
        return r

    def play(self, *streams):
        idx = [0] * len(streams)
        total = max(len(st) for st in streams) if streams else 0
        for step in range(total):
            for k, st in enumerate(streams):
                upto = (step + 1) * len(st) // total
                while idx[k] < upto:
                    kind, args = st[idx[k]]
                    idx[k] += 1
                    (self.op if kind == "op" else self.dma)(*args)

    def op(self, eng, fn, reads=(), writes=()):
        if getattr(self, "rec", None) is not None:
            self.rec.append(("op", (eng, fn, list(reads), list(writes))))
            return
        reads = list(reads)
        writes = list(writes) + [b for b in reads if b.psum and b not in writes]
        self._deps_for(eng, reads, writes)
        ins = fn()
        d = eng.dep
        d.n += 1
        ins.then_inc(d.sem, 1)
        self._commit((d, d.n), reads, writes)

    def dma(self, eng, fn, reads=(), writes=(), on=None):
        if getattr(self, "rec", None) is not None:
            self.rec.append(("dma", (eng, fn, list(reads), list(writes), on)))
            return
        self._deps_for(eng, reads, writes)
        b = on if on is not None else (writes[0] if writes else reads[0])
        if b.dsem is None:
            b.dsem = Dep(self.nc, "d_" + b.name)
            self.deps.append(b.dsem)
        d = b.dsem
        assert d.issuer in (None, eng), (b.name, d.issuer.name, eng.name)
        d.issuer = eng
        ins = fn()
        for i1 in (ins if isinstance(ins, (list, tuple)) else [ins]):
            d.n += 16
            i1.then_inc(d.sem, 16)
        self._commit((d, d.n), reads, writes)

    def wait_buf(self, eng, b):
        self._deps_for(eng, [b], [])

    def barrier(self):
        for e in self.engs:
            for d in self.deps:
                if d.n > 0:
                    self._wait(e, d, d.n)

    def snapshot(self):
        return ({d: d.n for d in self.deps}, {e: dict(e.seen) for e in self.engs})

    def compensate(self, snap):
        before, _ = snap
        for d in self.deps:
            b4 = before.get(d, 0)
            delta = d.n - b4
            if delta <= 0:
                continue
            eng = d.issuer
            if eng is None:
                eng = [e for e in self.engs if e.dep is d][0]
            eng.ins.wait_ge(d.sem, b4)
            eng.ins.sem_inc(d.sem, delta)

    def restore_seen(self, snap):
        _, seen = snap
        for e in self.engs:
            e.seen = dict(seen[e])


def build_nc(stage="full", use_if=True, nbmax=NBMAX):
    nc = bass.Bass("TRN2", target_bir_lowering=False)
    K = Kern(nc)
    PE, ACT, DVE, POOL, SP = K.pe, K.act, K.dve, K.pool, K.sp
    T, A, V, G, Q = nc.tensor, nc.scalar, nc.vector, nc.gpsimd, nc.sync

    def din(name, shape, dt=F32):
        return nc.dram_tensor(name, list(shape), dt, kind="ExternalInput").ap()

    x_d = din("x", [S, D])
    cT_d = din("cT", [128, 8])
    pos_d = din("pos", [64, S], I32)
    wada_d = din("w_ada", [D, 6 * D])
    bada_d = din("b_ada", [1, 6 * D])
    gmix_d = din("g_mix_bc", [128, D])
    gffn_d = din("g_ffn_bc", [128, D])
    win_d = din("w_in_ext", [D, 1472])
    lruv_d = din("lruv", [128, 4, 8])
    wabd_d = din("wa_bd", [4, 128, 128])
    wxbd_d = din("wx_bd", [4, 128, 128])
    gql_d = din("g_q_lat", [128, 2])
    gkvl_d = din("g_kv_lat", [128, 1])
    wuq_d = din("w_uq_h", [8, 256, 128])
    wuk_d = din("w_uk_h", [8, 128, 128])
    wuv_d = din("w_uv", [128, 512])
    gq_d = din("gq", [128, 1])
    gk_d = din("gk", [128, 1])
    rtab_d = din("rtab", [64, 3])
    wout_d = din("w_out", [D, D])
    wr_d = din("w_router", [D, NE])
    br_d = din("b_router", [1, NE])
    w1_d = din("w1", [NE, D, 2 * D])
    b1_d = din("b1", [NE, 2 * D])
    w2_d = din("w2", [NE, D, D])
    b2_d = din("b2", [NE, D])
    out_d = nc.dram_tensor("out", [S, D], F32, kind="ExternalOutput").ap()
    dbg_d = None
    if stage != "full":
        dbg_d = nc.dram_tensor("dbg", [S, D], F32, kind="ExternalOutput").ap()
    X1_d = nc.dram_tensor("X1s", [S, D], F32, kind="Internal").ap()
    H2r_d = nc.dram_tensor("H2r", [S * 4, D], BF, kind="Internal").ap()
    Yb_d = nc.dram_tensor("Ybs", [S * 4, D], F32, kind="Internal").ap()
    Tab_d = nc.dram_tensor("Tabs", [NE * CAP, 1], I32, kind="Internal").ap()
    b_out, b_dbg, b_X1, b_H2r, b_Yb, b_Tab = K.buf("out"), K.buf("dbg"), K.buf("X1"), K.buf("H2r"), K.buf("Yb"), K.buf("Tab")

    top = ExitStack()

    def sb(es, name, shape, dt=F32):
        return es.enter_context(nc.sbuf_tensor("s_" + name, list(shape), dt))

    def ps(es, name, shape, dt=F32):
        return es.enter_context(nc.psum_tensor("p_" + name, list(shape), dt))

    ident_b = sb(top, "ident_b", [128, 128], BF); b_ident_b = K.buf("identb")
    ident_f = sb(top, "ident_f", [128, 128], F32); b_ident_f = K.buf("identf")
    ones_b = sb(top, "ones_b", [128, 128], BF); b_ones_b = K.buf("onesb")
    ones_f = sb(top, "ones_f", [128, 128], F32); b_ones_f = K.buf("onesf")
    sel_b = sb(top, "sel_b", [128, 128], BF); b_sel = K.buf("sel")
    U_b = sb(top, "U_b", [128, 128], BF); b_U = K.buf("U")
    tri_b = sb(top, "tri_b", [128, 128], BF); b_tri = K.buf("tri")
    iof = sb(top, "iof", [128, 128], F32); b_iof = K.buf("iof")
    pidx = sb(top, "pidx", [128, 1], F32); b_pidx = K.buf("pidx")
    ebase = sb(top, "ebase", [128, NE], F32); b_ebase = K.buf("ebase")
    gate2k = sb(top, "gate2k", [128, D], F32); b_gate2k = K.buf("gate2k")
    G_all = sb(top, "G_all", [128, NT, 4], F32); b_G = K.buf("G_all")
    IDX = sb(top, "IDX", [128, NT, 4], I32); b_IDX = K.buf("IDX")
    cnt_i = sb(top, "cnt_i", [1, NE], I32); b_cnt = K.buf("cnt")
    mod_stack = ExitStack()
    mod = sb(mod_stack, "mod", [128, 5 * D], F32)
    b_mod = [K.buf(f"mod{i}") for i in range(6)]
    cat_stack = ExitStack()
    catL = sb(cat_stack, "catL", [128, 4, S], BF); b_cat = [K.buf(f"cat{i}") for i in range(8)]

    K.op(POOL, lambda: G.iota(iof[:], pattern=[[1, 128]], base=0, channel_multiplier=-1,
                              allow_small_or_imprecise_dtypes=True), writes=[b_iof])
    K.op(POOL, lambda: G.iota(pidx[:], pattern=[[0, 1]], base=0, channel_multiplier=1,
                              allow_small_or_imprecise_dtypes=True), writes=[b_pidx])
    K.op(POOL, lambda: G.iota(ebase[:], pattern=[[CAP, NE]], base=0, channel_multiplier=0,
                              allow_small_or_imprecise_dtypes=True), writes=[b_ebase])
    K.op(DVE, lambda: V.tensor_single_scalar(out=ident_b[:], in_=iof[:], scalar=0.0, op=ALU.is_equal), [b_iof], [b_ident_b])
    K.op(DVE, lambda: V.tensor_single_scalar(out=ident_f[:], in_=iof[:], scalar=0.0, op=ALU.is_equal), [b_iof], [b_ident_f])
    K.op(DVE, lambda: V.tensor_single_scalar(out=U_b[:], in_=iof[:], scalar=0.0, op=ALU.is_gt), [b_iof], [b_U])
    K.op(DVE, lambda: V.tensor_single_scalar(out=tri_b[:], in_=iof[:], scalar=0.0, op=ALU.is_ge), [b_iof], [b_tri])
    K.op(DVE, lambda: V.memset(ones_b[:], 1.0), writes=[b_ones_b])
    K.op(DVE, lambda: V.memset(ones_f[:], 1.0), writes=[b_ones_f])
    K.op(DVE, lambda: V.tensor_scalar(out=sel_b[:], in0=ones_f[:], scalar1=pidx[:, 0:1], scalar2=32.0,
                                      op0=ALU.mult, op1=ALU.is_ge), [b_ones_f, b_pidx], [b_sel])

    def dbg_dump(src_ap, rows, cols, b_src, row0=0, col0=0):
        K.dma(POOL, lambda: G.dma_start(out=dbg_d[row0:row0 + rows, col0:col0 + cols], in_=src_ap), [b_src], [b_dbg])

    def finish():
        K.barrier()
        for d in K.deps:
            if d.n > 0:
                Q.wait_ge(d.sem, d.n)
        pass
        return nc

    with ExitStack() as es:
        cT = sb(es, "cT", [128, 8]); b_cT = K.buf("cT")
        sc = sb(es, "sc", [128, 8]); b_sc = K.buf("sc")
        scb = sb(es, "scb", [128, 8, 128]); b_scb = K.buf("scb")
        bada = sb(es, "bada", [1, 6 * D]); b_bada = K.buf("bada")
        wst = [sb(es, f"wst{i}", [128, 8, 512]) for i in range(2)]; b_wst = [K.buf(f"wst{i}") for i in range(2)]
        pA = [ps(es, f"pA{i}", [128, 512]) for i in range(2)]; b_pA = [K.pbuf(f"pA{i}") for i in range(2)]
        gm = sb(es, "gm", [128, D]); b_gm = K.buf("gm")
        gf = sb(es, "gf", [128, D]); b_gf = K.buf("gf")
        K.dma(SP, lambda: Q.dma_start(out=cT[:], in_=cT_d), writes=[b_cT])
        K.dma(SP, lambda: Q.dma_start(out=bada[:], in_=bada_d), writes=[b_bada])
        K.dma(SP, lambda: Q.dma_start(out=gm[:], in_=gmix_d), writes=[b_gm])
        K.dma(SP, lambda: Q.dma_start(out=gf[:], in_=gffn_d), writes=[b_gf])
        K.op(ACT, lambda: A.activation(out=sc[:], in_=cT[:], func=AF.Silu), [b_cT], [b_sc])
        for kc in range(8):
            K.op(DVE, lambda kc=kc: V.tensor_scalar(out=scb[:, kc, :], in0=ones_f[:], scalar1=sc[:, kc:kc + 1],
                                                   scalar2=None, op0=ALU.mult), [b_sc, b_ones_f], [b_scb])
        wada_v = wada_d.rearrange("(kc p) n -> p kc n", p=128)
        for n in range(12):
            s = n % 2
            K.dma(SP, lambda n=n, s=s: Q.dma_start(out=wst[s][:], in_=wada_v[:, :, n * 512:(n + 1) * 512]), writes=[b_wst[s]])

            def mm(n=n, s=s):
                for kc in range(8):
                    T.matmul(pA[s][:], lhsT=scb[:, kc, :], rhs=wst[s][:, kc, :], start=(kc == 0), stop=False)
                return T.matmul(pA[s][:], lhsT=ones_f[0:1, :], rhs=bada[0:1, n * 512:(n + 1) * 512], start=False, stop=True)
            K.op(PE, mm, [b_scb, b_wst[s], b_ones_f, b_bada], [b_pA[s]])
            if n < 10:
                K.op(ACT, lambda n=n, s=s: A.copy(out=mod[:, n * 512:(n + 1) * 512], in_=pA[s][:]), [b_pA[s]], [b_mod[n // 2]])
            else:
                K.op(ACT, lambda n=n, s=s: A.copy(out=gate2k[:, (n - 10) * 512:(n - 9) * 512], in_=pA[s][:]), [b_pA[s]], [b_gate2k])
        K.op(DVE, lambda: V.scalar_tensor_tensor(out=mod[:, D:2 * D], in0=mod[:, D:2 * D], scalar=1.0, in1=gm[:],
                                                 op0=ALU.add, op1=ALU.mult), [b_gm], [b_mod[1]])
        K.op(DVE, lambda: V.scalar_tensor_tensor(out=mod[:, 4 * D:5 * D], in0=mod[:, 4 * D:5 * D], scalar=1.0, in1=gf[:],
                                                 op0=ALU.add, op1=ALU.mult), [b_gf], [b_mod[4]])
        if stage == "A":
            for i in range(5):
                dbg_dump(mod[0:1, i * D:(i + 1) * D], 1, D, b_mod[i], row0=i)
            dbg_dump(gate2k[0:1, :], 1, D, b_gate2k, row0=5)
            return finish()
        K.barrier()
    SH1, A1, GATE1, SH2, A2 = [mod[:, i * D:(i + 1) * D] for i in range(5)]

    def rms_rstd(ss_ap, n, out_ap, b_in, b_tmp, tmp_ap, b_out_):
        K.op(ACT, lambda: A.activation(out=tmp_ap, in_=ss_ap, func=AF.Ln, scale=1.0 / n, bias=EPS), [b_in], [b_tmp])
        K.op(ACT, lambda: A.activation(out=out_ap, in_=tmp_ap, func=AF.Exp, scale=-0.5), [b_tmp], [b_out_])

    es_mix = ExitStack()
    if True:
        qlatn = sb(es_mix, "qlatn", [128, 2, S], BF); b_qlatn = K.buf("qlatn")
        kvlatn = sb(es_mix, "kvlatn", [128, S], BF); b_kvlatn = K.buf("kvlatn")
        zr = sb(es_mix, "zr", [64, S], BF); b_zr = K.buf("zr")
        with ExitStack() as es:
            winb = sb(es, "winb", [128, 8, 1472], BF); b_winb = K.buf("winb")
            win_v = win_d.rearrange("(kc p) n -> p kc n", p=128)
            for kc in range(8):
                K.dma(POOL, lambda kc=kc: G.dma_start(out=winb[:, kc, :], in_=win_v[:, kc, :]), writes=[b_winb])
            lruv = sb(es, "lruv", [128, 4, 8]); b_lruv = K.buf("lruv")
            K.dma(SP, lambda: Q.dma_start(out=lruv[:], in_=lruv_d), writes=[b_lruv])
            nsp = sb(es, "nsp", [128, 4, 2]); b_nsp = K.buf("nsp")
            spt = sb(es, "spt", [128, 4]); b_spt = K.buf("spt")
            K.op(ACT, lambda: A.activation(out=spt[:], in_=lruv[:, :, 7], func=AF.Exp, scale=-1.0), [b_lruv], [b_spt])
            K.op(ACT, lambda: A.activation(out=spt[:], in_=spt[:], func=AF.Ln, bias=1.0), [b_spt], [b_spt])
            K.op(DVE, lambda: V.tensor_scalar(out=nsp[:, :, 0], in0=spt[:], scalar1=-8.0, scalar2=None, op0=ALU.mult), [b_spt], [b_nsp])
            K.op(DVE, lambda: V.tensor_scalar(out=nsp[:, :, 1], in0=spt[:], scalar1=-16.0, scalar2=None, op0=ALU.mult), [b_spt], [b_nsp])
            wab = sb(es, "wab", [128, 4, 128], BF); b_wab = K.buf("wab")
            wxb = sb(es, "wxb", [128, 4, 128], BF); b_wxb = K.buf("wxb")
            K.dma(POOL, lambda: G.dma_start(out=wab[:], in_=wabd_d.rearrange("c p n -> p c n")), writes=[b_wab])
            K.dma(POOL, lambda: G.dma_start(out=wxb[:], in_=wxbd_d.rearrange("c p n -> p c n")), writes=[b_wxb])
            gql = sb(es, "gql", [128, 2]); b_gql = K.buf("gql")
            gkvl = sb(es, "gkvl", [128, 1]); b_gkvl = K.buf("gkvl")
            K.dma(SP, lambda: Q.dma_start(out=gql[:], in_=gql_d), writes=[b_gql])
            K.dma(SP, lambda: Q.dma_start(out=gkvl[:], in_=gkvl_d), writes=[b_gkvl])
            hstate = sb(es, "hstate", [128, 4]); b_hst = K.buf("hstate")
            K.op(DVE, lambda: V.memset(hstate[:], 0.0), writes=[b_hst])
            xt = [sb(es, f"xt{i}", [128, D]) for i in range(2)]; b_xt = [K.buf(f"xt{i}") for i in range(2)]
            ss = sb(es, "ss", [128, 2]); b_ss = K.buf("ss")
            rstd = sb(es, "rstd", [128, 1]); b_rstd = K.buf("rstd")
            h1 = sb(es, "h1", [128, D]); b_h1 = K.buf("h1")
            hb = sb(es, "hb", [128, D], BF); b_hb = K.buf("hb")
            hT = sb(es, "hT", [128, 8, 512], BF); b_hT = K.buf("hT")
            xl = [[sb(es, f"xl{i}{c}", [128, 3 + 512]) for c in range(4)] for i in range(2)]
            b_xl = [[K.buf(f"xl{i}{c}") for c in range(4)] for i in range(2)]
            gy = [[sb(es, f"gy{i}{c}", [128, 512]) for c in range(4)] for i in range(2)]
            b_gy = [[K.buf(f"gy{i}{c}") for c in range(4)] for i in range(2)]
            sq = [sb(es, f"sq{i}", [128, 512], BF) for i in range(2)]; b_sq = [K.buf(f"sq{i}") for i in range(2)]
            qraw = [sb(es, f"qraw{i}", [128, 512]) for i in range(2)]; b_qraw = [K.buf(f"qraw{i}") for i in range(2)]
            rs = sb(es, "rs", [128, 512]); b_rs = K.buf("rs")
            rt2 = sb(es, "rt2", [128, 512]); b_rt2 = K.buf("rt2")
            xc_ = [sb(es, f"xc{i}", [128, 512]) for i in range(2)]; b_xc_ = [K.buf(f"xc{i}") for i in range(2)]
            xcb_ = [sb(es, f"xcb{i}", [128, 512], BF) for i in range(2)]; b_xcb_ = [K.buf(f"xcb{i}") for i in range(2)]
            rr_ = [sb(es, f"rr{i}", [128, 512]) for i in range(2)]; b_rr_ = [K.buf(f"rr{i}") for i in range(2)]
            ii_ = [sb(es, f"ii{i}", [128, 512]) for i in range(2)]; b_ii_ = [K.buf(f"ii{i}") for i in range(2)]
            aa_ = [sb(es, f"aa{i}", [128, 512]) for i in range(2)]; b_aa_ = [K.buf(f"aa{i}") for i in range(2)]
            mm__ = [sb(es, f"mm{i}", [128, 512]) for i in range(2)]; b_mm_ = [K.buf(f"mm{i}") for i in range(2)]
            pT = ps(es, "pT", [128, D], BF); b_pT = K.pbuf("pT")
            NZ = 2
            pz = [ps(es, f"pz{i}", [128, 512]) for i in range(NZ)]; b_pz = [K.pbuf(f"pz{i}") for i in range(NZ)]
            pg_ = [[ps(es, f"pg{i}{j}", [128, 512]) for j in range(2)] for i in range(2)]
            b_pg_ = [[K.pbuf(f"pg{i}{j}") for j in range(2)] for i in range(2)]
            pss = ps(es, "pss", [128, 512]); b_pss = K.pbuf("pss")
            for c in range(4):
                K.op(DVE, lambda c=c: V.memset(xl[0][c][:, 0:3], 0.0), writes=[b_xl[0][c]])
            zi = [0]

            def norm_unit(g, tt):
                t = g * 4 + tt
                s = t % 2
                K.dma(SP, lambda: Q.dma_start(out=xt[s][:], in_=x_d[t * 128:(t + 1) * 128, :]), writes=[b_xt[s]])
                K.op(ACT, lambda: A.activation(out=h1[:], in_=xt[s][:], func=AF.Square, accum_out=ss[:, 0:1]),
                     [b_xt[s]], [b_h1, b_ss])
                rms_rstd(ss[:, 0:1], D, rstd[:], b_ss, b_ss, ss[:, 1:2], b_rstd)
                K.op(DVE, lambda: V.scalar_tensor_tensor(out=h1[:], in0=xt[s][:], scalar=rstd[:, 0:1], in1=A1,
                                                         op0=ALU.mult, op1=ALU.mult), [b_xt[s], b_rstd, b_mod[1]], [b_h1])
                K.op(DVE, lambda: V.tensor_tensor(out=hb[:], in0=h1[:], in1=SH1, op=ALU.add), [b_h1, b_mod[0]], [b_hb])

                def tr():
                    for kc in range(8):
                        last = T.transpose(pT[:, kc * 128:(kc + 1) * 128], hb[:, kc * 128:(kc + 1) * 128], ident_b[:])
                    return last
                K.op(PE, tr, [b_hb, b_ident_b], [b_pT])
                K.op(ACT, lambda: A.copy(out=hT[:, :, tt * 128:(tt + 1) * 128],
                                         in_=pT[:].rearrange("p (k n) -> p k n", k=8)), [b_pT], [b_hT])

            def inproj_unit(g, ch):
                tsl = slice(g * 512, (g + 1) * 512)
                gp = g % 2
                z = zi[0] % NZ
                zi[0] += 1
                M = 128 if ch < 11 else 64

                def mmz():
                    for kc in range(8):
                        last = T.matmul(pz[z][0:M, :], lhsT=winb[:, kc, ch * 128:ch * 128 + M], rhs=hT[:, kc, :],
                                        start=(kc == 0), stop=(kc == 7))
                    return last
                K.op(PE, mmz, [b_winb, b_hT], [b_pz[z]])
                if ch < 4:
                    c = ch
                    if g > 0:
                        K.op(DVE, lambda: V.tensor_copy(out=xl[gp][c][:, 0:3], in_=xl[1 - gp][c][:, 512:515]), [b_xl[1 - gp][c]], [b_xl[gp][c]])
                    K.op(ACT, lambda: A.copy(out=xl[gp][c][:, 3:515], in_=pz[z][:]), [b_pz[z]], [b_xl[gp][c]])
                elif ch < 8:
                    c = ch - 4
                    K.op(ACT, lambda: A.activation(out=gy[gp][c][:], in_=pz[z][:], func=AF.Gelu), [b_pz[z]], [b_gy[gp][c]])
                elif ch < 11:
                    j = ch - 8 if ch < 10 else 0
                    gcol = gql[:, j:j + 1] if ch < 10 else gkvl[:, 0:1]
                    b_g = b_gql if ch < 10 else b_gkvl
                    K.op(ACT, lambda: A.activation(out=sq[j][:], in_=pz[z][:], func=AF.Square), [b_pz[z]], [b_sq[j]])
                    K.op(DVE, lambda: V.tensor_scalar(out=qraw[j][:], in0=pz[z][:], scalar1=gcol, scalar2=None,
                                                      op0=ALU.mult), [b_pz[z], b_g], [b_qraw[j]])
                    if ch == 9 or ch == 10:
                        nch = 2 if ch == 9 else 1

                        def mms():
                            for j2 in range(nch):
                                last = T.matmul(pss[:], lhsT=ones_b[:], rhs=sq[j2][:], start=(j2 == 0), stop=(j2 == nch - 1))
                            return last
                        K.op(PE, mms, [b_ones_b] + b_sq[:nch], [b_pss])
                        rms_rstd(pss[:], 128 * nch, rs[:], b_pss, b_rt2, rt2[:], b_rs)
                        for j2 in range(nch):
                            dst = qlatn[:, j2, tsl] if ch == 9 else kvlatn[:, tsl]
                            bd = b_qlatn if ch == 9 else b_kvlatn
                            K.op(DVE, lambda j2=j2, dst=dst: V.tensor_tensor(out=dst, in0=qraw[j2][:], in1=rs[:], op=ALU.mult),
                                 [b_qraw[j2], b_rs], [bd])
                else:
                    K.op(ACT, lambda: A.copy(out=zr[:, tsl], in_=pz[z][0:64, :]), [b_pz[z]], [b_zr])

            def lru_unit(g, c):
                tsl = slice(g * 512, (g + 1) * 512)
                gp = g % 2
                q_ = c % 2
                xc, xcb, rr, ii, aa, mm_, pg = xc_[q_], xcb_[q_], rr_[q_], ii_[q_], aa_[q_], mm__[q_], pg_[q_]
                b_xc, b_xcb, b_rr, b_ii, b_aa, b_mm, b_pg = b_xc_[q_], b_xcb_[q_], b_rr_[q_], b_ii_[q_], b_aa_[q_], b_mm_[q_], b_pg_[q_]
                lv = lambda f: lruv[:, c, f:f + 1]
                K.op(ACT, lambda: A.activation(out=xc[:], in_=xl[gp][c][:, 3:515], func=AF.Identity, scale=lv(3), bias=lv(4)),
                     [b_xl[gp][c], b_lruv], [b_xc])
                for j in range(3):
                    K.op(DVE, lambda j=j: V.scalar_tensor_tensor(out=xc[:], in0=xl[gp][c][:, j:j + 512], scalar=lv(j), in1=xc[:],
                                                                 op0=ALU.mult, op1=ALU.add), [b_xl[gp][c], b_lruv], [b_xc])
                K.op(ACT, lambda: A.copy(out=xcb[:], in_=xc[:]), [b_xc], [b_xcb])
                K.op(PE, lambda: T.matmul(pg[0][:], lhsT=wab[:, c, :], rhs=xcb[:], start=True, stop=True), [b_wab, b_xcb], [b_pg[0]])
                K.op(PE, lambda: T.matmul(pg[1][:], lhsT=wxb[:, c, :], rhs=xcb[:], start=True, stop=True), [b_wxb, b_xcb], [b_pg[1]])
                K.op(ACT, lambda: A.activation(out=rr[:], in_=pg[0][:], func=AF.Sigmoid, bias=lv(5)), [b_pg[0], b_lruv], [b_rr])
                K.op(ACT, lambda: A.activation(out=ii[:], in_=pg[1][:], func=AF.Sigmoid, bias=lv(6)), [b_pg[1], b_lruv], [b_ii])
                K.op(ACT, lambda: A.activation(out=aa[:], in_=rr[:], func=AF.Exp, scale=nsp[:, c, 0:1]), [b_rr, b_nsp], [b_aa])
                K.op(ACT, lambda: A.activation(out=mm_[:], in_=rr[:], func=AF.Exp, scale=nsp[:, c, 1:2]), [b_rr, b_nsp], [b_mm])
                K.op(ACT, lambda: A.activation(out=mm_[:], in_=mm_[:], func=AF.Sqrt, scale=-1.0, bias=1.0), [b_mm], [b_mm])
                K.op(DVE, lambda: V.tensor_tensor(out=ii[:], in0=ii[:], in1=xc[:], op=ALU.mult), [b_xc], [b_ii])
                K.op(DVE, lambda: V.tensor_tensor(out=ii[:], in0=ii[:], in1=mm_[:], op=ALU.mult), [b_mm], [b_ii])
                K.op(DVE, lambda: V.tensor_tensor_scan(out=rr[:], data0=aa[:], data1=ii[:], initial=hstate[:, c:c + 1],
                                                       op0=ALU.mult, op1=ALU.add), [b_aa, b_ii, b_hst], [b_rr])
                K.op(DVE, lambda: V.tensor_copy(out=hstate[:, c:c + 1], in_=rr[:, 511:512]), [b_rr], [b_hst])
                K.op(DVE, lambda: V.tensor_tensor(out=catL[:, c, tsl], in0=rr[:], in1=gy[gp][c][:], op=ALU.mult),
                     [b_rr, b_gy[gp][c]], [b_cat[c]])

            def x_units(g):
                return [lambda tt=tt: norm_unit(g, tt) for tt in range(4)] + [lambda ch=ch: inproj_unit(g, ch) for ch in range(12)]

            for u in x_units(0):
                u()
            if stage == "B":
                K.barrier()
                dbg_dump(hT[:, 0, :], 128, 512, b_hT)
                return finish()
            if stage == "C":
                K.barrier()
                dbg_dump(xl[0][0][:, 3:515], 128, 512, b_xl[0][0])
                dbg_dump(qlatn[:, 0, 0:512], 128, 512, b_qlatn, row0=128)
                return finish()
            for g in range(8):
                nx = x_units(g + 1) if g + 1 < 8 else []
                for c2 in range(2):
                    ra0 = K.record(lambda: lru_unit(g, 2 * c2))
                    ra1 = K.record(lambda: lru_unit(g, 2 * c2 + 1))
                    rb = K.record(lambda: [u() for u in nx[8 * c2:8 * c2 + 8]])
                    K.play(ra0, ra1, rb) if rb else K.play(ra0, ra1)
            if stage == "D":
                for c in range(4):
                    dbg_dump(catL[:, c, 0:1024], 128, 1024, b_cat[c], row0=c * 128)
                return finish()
            K.barrier()

        catA_stack = ExitStack()
        catA = sb(catA_stack, "catA", [128, 4, S], BF)
        with ExitStack() as es:
            wuqb = sb(es, "wuqb", [128, 2, 8, 128], BF); b_wuqb = K.buf("wuqb")
            wukb = sb(es, "wukb", [128, 8, 128], BF); b_wukb = K.buf("wukb")
            wuvb = sb(es, "wuvb", [128, 512], BF); b_wuvb = K.buf("wuvb")
            for h in range(8):
                K.dma(POOL, lambda h=h: G.dma_start(out=wuqb[:, :, h, :], in_=wuq_d[h].rearrange("(kc p) n -> p kc n", p=128)), writes=[b_wuqb])
            K.dma(POOL, lambda: G.dma_start(out=wukb[:], in_=wuk_d.rearrange("h p n -> p h n")), writes=[b_wukb])
            K.dma(POOL, lambda: G.dma_start(out=wuvb[:], in_=wuv_d), writes=[b_wuvb])
            gq = sb(es, "gq", [128, 1]); b_gq = K.buf("gq")
            gk = sb(es, "gk", [128, 1]); b_gk = K.buf("gk")
            rtab = sb(es, "rtab", [64, 3]); b_rtab = K.buf("rtab")
            K.dma(SP, lambda: Q.dma_start(out=gq[:], in_=gq_d), writes=[b_gq])
            K.dma(SP, lambda: Q.dma_start(out=gk[:], in_=gk_d), writes=[b_gk])
            K.dma(SP, lambda: Q.dma_start(out=rtab[:], in_=rtab_d), writes=[b_rtab])
            esel = ident_b
            Tb = sb(es, "Tb", [64, S], BF); b_Tb = K.buf("Tb")
            with ExitStack() as es2:
                CW = 1024
                posi = sb(es2, "posi", [64, CW], I32); b_posi = K.buf("posi")
                tt_ = sb(es2, "tt_", [64, CW]); b_tt = K.buf("tt")
                kf = sb(es2, "kf", [64, CW]); b_kf = K.buf("kf")
                ki = sb(es2, "ki", [64, CW], I32); b_ki = K.buf("ki")
                for cc in range(S // CW):
                    csl = slice(cc * CW, (cc + 1) * CW)
                    K.dma(SP, lambda csl=csl: Q.dma_start(out=posi[:], in_=pos_d[:, csl]), writes=[b_posi])
                    K.op(DVE, lambda: V.tensor_copy(out=tt_[:], in_=posi[:]), [b_posi], [b_tt])
                    K.op(DVE, lambda: V.tensor_scalar(out=tt_[:], in0=tt_[:], scalar1=rtab[:, 0:1], scalar2=rtab[:, 1:2],
                                                      op0=ALU.mult, op1=ALU.add), [b_rtab], [b_tt])
                    K.op(DVE, lambda: V.tensor_scalar(out=kf[:], in0=tt_[:], scalar1=1.0 / TWO_PI, scalar2=None, op0=ALU.mult), [b_tt], [b_kf])
                    K.op(DVE, lambda: V.tensor_copy(out=ki[:], in_=kf[:]), [b_kf], [b_ki])
                    K.op(DVE, lambda: V.tensor_copy(out=kf[:], in_=ki[:]), [b_ki], [b_kf])
                    K.op(DVE, lambda: V.scalar_tensor_tensor(out=tt_[:], in0=kf[:], scalar=-TWO_PI, in1=tt_[:], op0=ALU.mult, op1=ALU.add),
                         [b_kf], [b_tt])
                    K.op(DVE, lambda: V.tensor_scalar(out=kf[:], in0=tt_[:], scalar1=math.pi, scalar2=-TWO_PI, op0=ALU.is_gt, op1=ALU.mult),
                         [b_tt], [b_kf])
                    K.op(DVE, lambda: V.tensor_tensor(out=tt_[:], in0=tt_[:], in1=kf[:], op=ALU.add), [b_kf], [b_tt])
                    K.op(DVE, lambda: V.tensor_scalar(out=tt_[:], in0=tt_[:], scalar1=-math.pi, scalar2=math.pi, op0=ALU.max, op1=ALU.min),
                         [], [b_tt])
                    K.op(ACT, lambda csl=csl: A.activation(out=Tb[:, csl], in_=tt_[:], func=AF.Sin), [b_tt], [b_Tb])
                    K.op(DVE, lambda csl=csl: V.tensor_scalar(out=Tb[:, csl], in0=Tb[:, csl], scalar1=rtab[:, 2:3], scalar2=None, op0=ALU.mult),
                         [b_rtab], [b_Tb])
                K.barrier()
            qT = [sb(es, f"qT{i}", [128, S], BF) for i in range(2)]; b_qT = [K.buf(f"qT{i}") for i in range(2)]
            kT = [sb(es, f"kT{i}", [128, S], BF) for i in range(2)]; b_kT = [K.buf(f"kT{i}") for i in range(2)]
            Va = [sb(es, f"Va{i}", [128, NT, 128], BF) for i in range(2)]; b_Va = [K.buf(f"Va{i}") for i in range(2)]
            for i in range(2):
                K.op(POOL, lambda i=i: G.memset(Va[i][:, :, 64:128], 1.0), writes=[b_Va[i]])
                K.op(DVE, lambda i=i: V.memset(qT[i][0:32, :], 0.0), writes=[b_qT[i]])
                K.op(DVE, lambda i=i: V.memset(kT[i][0:32, :], 0.0), writes=[b_kT[i]])
            sqa = sb(es, "sqa", [128, 512], BF); b_sqa = K.buf("sqa")
            rsa = sb(es, "rsa", [128, 512]); b_rsa = K.buf("rsa")
            qn = sb(es, "qn", [128, 512]); b_qn = K.buf("qn")
            tmp2 = sb(es, "tmp2", [128, 512]); b_tmp2 = K.buf("tmp2")
            PT = [sb(es, f"PT{i}", [128, 2, 512], BF) for i in range(2)]; b_PT = [K.buf(f"PT{i}") for i in range(2)]
            rd = sb(es, "rd", [64, 512]); b_rd = K.buf("rd")
            pq = ps(es, "pq", [128, 512]); b_pq = K.pbuf("pq")
            pssa = ps(es, "pssa", [128, 512]); b_pssa = K.pbuf("pssa")
            pv = pssa[:].rearrange("p (a b) -> p a b", a=8); b_pv = b_pssa
            pS = [ps(es, f"pS{i}", [128, 2, 512]) for i in range(2)]; b_pS = [K.pbuf(f"pS{i}") for i in range(2)]
            pO = [ps(es, f"pO{i}", [128, 512]) for i in range(2)]; b_pO = [K.pbuf(f"pO{i}") for i in range(2)]
            SCALE = math.sqrt(96.0)
            S_REP = 1
            def prep_units(h):
                hb_ = h % 2
                phases = []
                for tg in range(4):
                    def uv_a(tg=tg):
                        def mmv():
                            for j in range(8):
                                t = tg * 8 + j
                                last = T.matmul(pv[:, j, :], lhsT=kvlatn[:, t * 128:(t + 1) * 128], rhs=wuvb[:, h * 64:(h + 1) * 64],
                                                start=True, stop=True)
                            return last
                        K.op(PE, mmv, [b_kvlatn, b_wuvb], [b_pv])

                    def uv_b(tg=tg):
                        K.op(ACT, lambda: A.copy(out=Va[hb_][:, tg * 8:(tg + 1) * 8, 0:64], in_=pv), [b_pv], [b_Va[hb_]])
                    phases += [uv_a, uv_b]
                for g in range(8):
                    for which in range(2):
                        tsl = slice(g * 512, (g + 1) * 512)
                        if which == 0:
                            gcol, b_gc, dstT, b_dst = gq, b_gq, qT[hb_], b_qT[hb_]
                        else:
                            gcol, b_gc, dstT, b_dst = gk, b_gk, kT[hb_], b_kT[hb_]

                        def ua(tsl=tsl, which=which):
                            if which == 0:
                                def mmq():
                                    T.matmul(pq[:], lhsT=wuqb[:, 0, h, :], rhs=qlatn[:, 0, tsl], start=True, stop=False)
                                    return T.matmul(pq[:], lhsT=wuqb[:, 1, h, :], rhs=qlatn[:, 1, tsl], start=False, stop=True)
                                K.op(PE, mmq, [b_wuqb, b_qlatn], [b_pq])
                            else:
                                def mmk():
                                    T.matmul(pq[:], lhsT=wukb[:, h, :], rhs=kvlatn[:, tsl], start=True, stop=False)
                                    return T.matmul(pq[:], lhsT=esel[0:64, :], rhs=zr[:, tsl], start=False, stop=True)
                                K.op(PE, mmk, [b_wukb, b_kvlatn, b_ident_b, b_zr], [b_pq])

                        def ub():
                            K.op(ACT, lambda: A.activation(out=sqa[:], in_=pq[:], func=AF.Square), [b_pq], [b_sqa])
                            K.op(PE, lambda: T.matmul(pssa[:], lhsT=sel_b[:], rhs=sqa[:], start=True, stop=True), [b_sel, b_sqa], [b_pssa])

                        def uc(tsl=tsl, gcol=gcol, b_gc=b_gc, dstT=dstT, b_dst=b_dst):
                            K.op(ACT, lambda: A.activation(out=tmp2[:], in_=pssa[:], func=AF.Ln, bias=96.0 * EPS), [b_pssa], [b_tmp2])
                            K.op(ACT, lambda: A.activation(out=rsa[:], in_=tmp2[:], func=AF.Exp, scale=-0.5), [b_tmp2], [b_rsa])
                            K.op(DVE, lambda: V.scalar_tensor_tensor(out=qn[:], in0=pq[:], scalar=gcol[:, 0:1], in1=rsa[:],
                                                                     op0=ALU.mult, op1=ALU.mult), [b_pq, b_gc, b_rsa], [b_qn])
                            K.op(DVE, lambda: V.tensor_tensor(out=qn[0:64, :], in0=qn[0:64, :], in1=Tb[:, tsl], op=ALU.mult), [b_Tb], [b_qn])
                            K.op(DVE, lambda: V.tensor_copy(out=tmp2[32:64, :], in_=qn[0:32, :]), [b_qn], [b_tmp2])
                            K.op(DVE, lambda: V.tensor_tensor(out=dstT[32:64, tsl], in0=qn[32:64, :], in1=tmp2[32:64, :], op=ALU.add),
                                 [b_qn, b_tmp2], [b_dst])
                            K.op(POOL, lambda: G.tensor_copy(out=dstT[64:128, tsl], in_=qn[64:128, :]), [b_qn], [b_dst])
                        phases += [ua, ub, uc]
                return phases

            for u in prep_units(0):
                u()
            if stage == "E0":
                K.barrier()
                dbg_dump(qT[0][:, 0:1024], 128, 1024, b_qT[0], row0=0)
                dbg_dump(kT[0][:, 0:1024], 128, 1024, b_kT[0], row0=128)
                dbg_dump(Va[0][:, 0:8, :].rearrange("p a b -> p (a b)"), 128, 1024, b_Va[0], row0=256)
                return finish()
            oi = 0
            for h in range(8):
                hb_ = h % 2
                nxt = prep_units(h + 1) if h + 1 < 8 else []
                steps = [(qg, kp) for qg in range(8) for kp in range(2 * qg + 2)]
                every = 1 if nxt else 0

                def geom(qg, kt):
                    n0 = 0 if kt < 4 * qg else 128 * (kt - 4 * qg)
                    return n0, 512 - n0

                def emit_S(i):
                    qg, kp = steps[i]
                    s = i % 2

                    def mmS():
                        for j in range(2):
                            kt = 2 * kp + j
                            n0, w = geom(qg, kt)
                            last = T.matmul(pS[s][:, j, 0:w], lhsT=kT[hb_][:, kt * 128:(kt + 1) * 128],
                                            rhs=qT[hb_][:, qg * 512 + n0:(qg + 1) * 512], start=True, stop=True)
                        return last
                    K.op(PE, mmS, [b_kT[hb_], b_qT[hb_]], [b_pS[s]])
                emit_S(0)
                o = oi % 2
                for i, (qg, kp) in enumerate(steps):
                    s = i % 2
                    nkt = 4 * qg + 4
                    diag = kp >= 2 * qg
                    if kp == 0:
                        o = oi % 2
                        oi += 1
                    if i + 1 < len(steps):
                        emit_S(i + 1)
                    if not diag:
                        K.op(ACT, lambda: A.activation(out=PT[s][:].rearrange("p a b -> p (a b)"), in_=pS[s][:].rearrange("p a b -> p (a b)"),
                                                       func=AF.Exp, scale=SCALE), [b_pS[s]], [b_PT[s]])
                    else:
                        for j in range(2):
                            n0, w = geom(qg, 2 * kp + j)
                            K.op(ACT, lambda j=j, w=w: A.activation(out=PT[s][:, j, 0:w], in_=pS[s][:, j, 0:w], func=AF.Exp, scale=SCALE),
                                 [b_pS[s]], [b_PT[s]])
                        for j in range(2):
                            K.op(DVE, lambda j=j: V.tensor_tensor(out=PT[s][:, j, 0:128], in0=PT[s][:, j, 0:128], in1=tri_b[:], op=ALU.mult),
                                 [b_tri], [b_PT[s]])

                    def mmPV():
                        for j in range(2):
                            kt = 2 * kp + j
                            n0, w = geom(qg, kt)
                            last = T.matmul(pO[o][:, n0:512], lhsT=Va[hb_][:, kt, :], rhs=PT[s][:, j, 0:w],
                                            start=(kt == 0), stop=(kt == nkt - 1))
                        return last
                    K.op(PE, mmPV, [b_Va[hb_], b_PT[s]], [b_pO[o]])
                    if kp == 2 * qg + 1:
                        K.op(DVE, lambda: V.reciprocal(out=rd[:], in_=pO[o][64:128, :]), [b_pO[o]], [b_rd])
                        ch = 4 + h // 2
                        p0 = (h % 2) * 64
                        K.op(DVE, lambda: V.tensor_tensor(out=catA[p0:p0 + 64, ch - 4, qg * 512:(qg + 1) * 512],
                                                          in0=pO[o][0:64, :], in1=rd[:], op=ALU.mult),
                             [b_pO[o], b_rd], [b_cat[ch]])
                    if nxt and every and i % every == every - 1 and (i // every) < len(nxt):
                        nxt[i // every]()
                for j in range((len(steps) // every) if every else 0, len(nxt)):
                    nxt[j]()
            if stage == "E":
                K.barrier()
                for c in range(4):
                    dbg_dump(catA[:, c, 0:1024], 128, 1024, b_cat[4 + c], row0=c * 128)
                return finish()
            K.barrier()

    with ExitStack() as es:
        wob = sb(es, "wob", [128, 8, D], BF); b_wob = K.buf("wob")
        wst2 = [sb(es, f"wst2{i}", [128, D]) for i in range(2)]; b_wst2 = [K.buf(f"wst2{i}") for i in range(2)]
        wout_v = wout_d.rearrange("(kc p) n -> p kc n", p=128)
        for kc in range(8):
            s = kc % 2
            K.dma(SP, lambda kc=kc, s=s: Q.dma_start(out=wst2[s][:], in_=wout_v[:, kc, :]), writes=[b_wst2[s]])
            K.op(DVE, lambda kc=kc, s=s: V.tensor_tensor(out=wob[:, kc, :], in0=wst2[s][:], in1=GATE1, op=ALU.mult),
                 [b_wst2[s], b_mod[2]], [b_wob])
        wrt = sb(es, "wrt", [128, 8, NE]); b_wrt = K.buf("wrt")
        brt = sb(es, "brt", [1, NE]); b_brt = K.buf("brt")
        K.dma(SP, lambda: Q.dma_start(out=wrt[:], in_=wr_d.rearrange("(kc p) n -> p kc n", p=128)), writes=[b_wrt])
        K.dma(SP, lambda: Q.dma_start(out=brt[:], in_=br_d), writes=[b_brt])
        mask_all = sb(es, "mask_all", [128, NT, NE], BF); b_mask = [K.buf(f"mask{t}") for t in range(NT)]
        VAL = sb(es, "VAL", [128, NT, 4], I32); b_VAL = K.buf("VAL")
        K.op(POOL, lambda: G.iota(VAL[:], pattern=[[512, NT], [1, 4]], base=0, channel_multiplier=4), writes=[b_VAL])
        tinit = sb(es, "tinit", [128, 1024], I32); b_tinit = K.buf("tinit")
        K.op(POOL, lambda: G.iota(tinit[:], pattern=[[0, 1024]], base=1 << 30, channel_multiplier=0), writes=[b_tinit])
        K.dma(POOL, lambda: G.dma_start(out=Tab_d.rearrange("(p n) o -> p (n o)", p=128), in_=tinit[:]), [b_tinit], [b_Tab])
        H2r_v = H2r_d.rearrange("(n r) d -> n r d", r=4)
        TC = 8
        xt = [sb(es, f"xtf{i}", [128, D]) for i in range(2)]; b_xt = [K.buf(f"xtf{i}") for i in range(2)]
        x1 = [sb(es, f"x1{i}", [128, D]) for i in range(2)]; b_x1 = [K.buf(f"x1{i}") for i in range(2)]
        junk = sb(es, "junkf", [128, D], BF); b_junk = K.buf("junkf")
        ss = sb(es, "ssf", [128, 2]); b_ss = K.buf("ssf")
        rstd = sb(es, "rstdf", [128, 1]); b_rstd = K.buf("rstdf")
        h2f = [sb(es, f"h2f{i}", [128, D]) for i in range(2)]; b_h2f = [K.buf(f"h2f{i}") for i in range(2)]
        h2b = [sb(es, f"h2b{i}", [128, D], BF) for i in range(2)]; b_h2b = [K.buf(f"h2b{i}") for i in range(2)]
        h2T = sb(es, "h2T", [128, 8, 128]); b_h2T = K.buf("h2T")
        lg_all = sb(es, "lg_all", [128, NT, NE]); b_lg = [K.buf(f"lg{c}") for c in range(NT // TC)]
        top8_all = sb(es, "top8_all", [128, NT, 8]); b_top8 = [K.buf(f"top8{c}") for c in range(NT // TC)]
        d4 = sb(es, "d4", [128, TC, 4]); b_d4 = K.buf("d4")
        den = sb(es, "den", [128, TC]); b_den = K.buf("den")
        oh = sb(es, "oh", [128, 4, TC, NE]); b_oh = K.buf("oh")
        mk = sb(es, "mk", [128, TC, NE]); b_mk = K.buf("mk")
        idxfull = sb(es, "idxfull", [128, TC, NE]); b_idxf = K.buf("idxf")
        junk2 = sb(es, "junk2", [128, TC, NE]); b_junk2 = K.buf("junk2")
        IDXf = sb(es, "IDXf", [128, NT, 4]); b_idx4 = K.buf("idx4")
        cntf = sb(es, "cntf", [1, NE]); b_cntf = K.buf("cntf")
        pm = [ps(es, f"pm{i}", [128, D]) for i in range(2)]; b_pm = [K.pbuf(f"pm{i}") for i in range(2)]
        pT32 = ps(es, "pT32", [128, D]); b_pT32 = K.pbuf("pT32")
        pl = ps(es, "pl", [128, NE]); b_pl = K.pbuf("pl")
        ppos = ps(es, "ppos", [128, TC, NE]); b_ppos = K.pbuf("ppos")
        pcnt = pl[0:1, :]; b_pcnt = b_pl
        AXX = mybir.AxisListType.X

        def load_x(t):
            K.dma(SP, lambda: Q.dma_start(out=xt[t % 2][:], in_=x_d[t * 128:(t + 1) * 128, :]), writes=[b_xt[t % 2]])

        def route_chunk(c):
            t0 = c * TC
            sl = slice(t0, t0 + TC)
            bl, bt = b_lg[c], b_top8[c]
            K.op(DVE, lambda: V.tensor_tensor(out=d4[:], in0=top8_all[:, sl, 0:4], in1=top8_all[:, sl, 0:1].to_broadcast([128, TC, 4]),
                                              op=ALU.subtract), [bt], [b_d4])
            K.op(ACT, lambda: A.activation(out=d4[:], in_=d4[:], func=AF.Exp), [], [b_d4])
            K.op(DVE, lambda: V.tensor_reduce(out=den[:], in_=d4[:], axis=AXX, op=ALU.add), [b_d4], [b_den])
            K.op(DVE, lambda: V.reciprocal(out=den[:], in_=den[:]), [], [b_den])
            K.op(DVE, lambda: V.tensor_tensor(out=G_all[:, sl, :], in0=d4[:], in1=den[:].to_broadcast([128, TC, 4]) if False else
                                              den[:].rearrange("p (t o) -> p t o", o=1).to_broadcast([128, TC, 4]), op=ALU.mult),
                 [b_d4, b_den], [b_G])
            for r in range(4):
                K.op(DVE, lambda r=r: V.tensor_tensor(out=oh[:, r, :, :], in0=lg_all[:, sl, :],
                                                      in1=top8_all[:, sl, r:r + 1].to_broadcast([128, TC, NE]), op=ALU.is_equal),
                     [bl, bt], [b_oh])
            K.op(DVE, lambda: V.tensor_tensor(out=mk[:], in0=oh[:, 0, :, :], in1=oh[:, 1, :, :], op=ALU.add), [b_oh], [b_mk])
            K.op(DVE, lambda: V.tensor_tensor(out=mk[:], in0=mk[:], in1=oh[:, 2, :, :], op=ALU.add), [b_oh], [b_mk])
            K.op(DVE, lambda: V.tensor_tensor(out=mask_all[:, sl, :], in0=mk[:], in1=oh[:, 3, :, :], op=ALU.add), [b_oh, b_mk], b_mask[t0:t0 + TC])

            def mmp():
                for j in range(TC):
                    t = t0 + j
                    last = T.matmul(ppos[:, j, :], lhsT=U_b[:], rhs=mask_all[:, t, :], start=True, stop=(t == 0))
                    for i in range(t):
                        last = T.matmul(ppos[:, j, :], lhsT=ones_b[:], rhs=mask_all[:, i, :], start=False, stop=(i == t - 1))
                return last
            K.op(PE, mmp, [b_U, b_ones_b] + b_mask[:t0 + TC], [b_ppos])
            K.op(DVE, lambda: V.tensor_tensor(out=idxfull[:], in0=ppos[:], in1=ebase[:].rearrange("p (o e) -> p o e", o=1).to_broadcast([128, TC, NE]),
                                              op=ALU.add), [b_ppos, b_ebase], [b_idxf])
            for r in range(4):
                K.op(DVE, lambda r=r: V.tensor_tensor(out=junk2[:], in0=oh[:, r, :, :], in1=idxfull[:], op=ALU.mult), [b_oh, b_idxf], [b_junk2])
                K.op(DVE, lambda r=r: V.tensor_reduce(out=IDXf[:, sl, r], in_=junk2[:], axis=AXX, op=ALU.add), [b_junk2], [b_idx4])
            K.op(DVE, lambda: V.tensor_copy(out=IDX[:, sl, :], in_=IDXf[:, sl, :]), [b_idx4], [b_IDX])
            K.dma(POOL, lambda: [G.indirect_dma_start(out=Tab_d, out_offset=IOA(ap=IDX[:, t, r:r + 1], axis=0),
                                                      in_=VAL[:, t, r:r + 1], in_offset=None)
                                 for t in range(t0, t0 + TC) for r in range(4)],
                  [b_VAL, b_IDX], [b_Tab])

        def stage_a(t):
            s = t % 2
            rows = slice(t * 128, (t + 1) * 128)
            if t + 1 < NT:
                load_x(t + 1)

            def mmo():
                for half in range(2):
                    for kc in range(8):
                        last = T.matmul(pm[s][:, half * 512:(half + 1) * 512], lhsT=(catL[:, kc, rows] if kc < 4 else catA[:, kc - 4, rows]),
                                        rhs=wob[:, kc, half * 512:(half + 1) * 512], start=(kc == 0), stop=(kc == 7))
                return last
            K.op(PE, mmo, b_cat + [b_wob], [b_pm[s]])
            K.op(DVE, lambda: V.tensor_tensor(out=x1[s][:], in0=pm[s][:], in1=xt[s][:], op=ALU.add), [b_pm[s], b_xt[s]], [b_x1[s]])
            K.dma(ACT, lambda: A.dma_start(out=X1_d[rows, :], in_=x1[s][:]), [b_x1[s]], [b_X1])
            if stage == "F1":
                dbg_dump(x1[s][:], 128, D, b_x1[s], row0=t * 128)
                return
            K.op(ACT, lambda: A.activation(out=junk[:], in_=x1[s][:], func=AF.Square, accum_out=ss[:, 0:1]), [b_x1[s]], [b_junk, b_ss])
            rms_rstd(ss[:, 0:1], D, rstd[:], b_ss, b_ss, ss[:, 1:2], b_rstd)
            K.op(DVE, lambda: V.scalar_tensor_tensor(out=h2f[s][:], in0=x1[s][:], scalar=rstd[:, 0:1], in1=A2, op0=ALU.mult, op1=ALU.mult),
                 [b_x1[s], b_rstd, b_mod[4]], [b_h2f[s]])
            K.op(DVE, lambda: V.tensor_tensor(out=h2f[s][:], in0=h2f[s][:], in1=SH2, op=ALU.add), [b_mod[3]], [b_h2f[s]])
            K.op(POOL, lambda: G.tensor_copy(out=h2b[s][:], in_=h2f[s][:]), [b_h2f[s]], [b_h2b[s]])
            K.dma(POOL, lambda: [G.dma_start(out=H2r_v[rows, r, :], in_=h2b[s][:]) for r in range(4)], [b_h2b[s]], [b_H2r])

        def stage_b(t):
            s = t % 2
            c = t // TC

            def tr32():
                for kc in range(8):
                    last = T.transpose(pT32[:, kc * 128:(kc + 1) * 128], h2f[s][:, kc * 128:(kc + 1) * 128], ident_f[:])
                return last
            K.op(PE, tr32, [b_h2f[s], b_ident_f], [b_pT32])
            K.op(ACT, lambda: A.copy(out=h2T[:].rearrange("p k n -> p (k n)"), in_=pT32[:]), [b_pT32], [b_h2T])

            def mmr():
                for kc in range(8):
                    T.matmul(pl[:], lhsT=h2T[:, kc, :], rhs=wrt[:, kc, :], start=(kc == 0), stop=False)
                return T.matmul(pl[:], lhsT=ones_f[0:1, :], rhs=brt[0:1, :], start=False, stop=True)
            K.op(PE, mmr, [b_h2T, b_wrt, b_ones_f, b_brt], [b_pl])
            K.op(DVE, lambda: V.tensor_copy(out=lg_all[:, t, :], in_=pl[:]), [b_pl], [b_lg[c]])
            K.op(DVE, lambda: V.max(out=top8_all[:, t, :], in_=lg_all[:, t, :]), [b_lg[c]], [b_top8[c]])

        load_x(0)
        stage_a(0)
        pending = None
        for t in range(NT):
            if stage == "F1":
                if t + 1 < NT:
                    stage_a(t + 1)
                continue
            streams = []
            if t + 1 < NT:
                streams.append(K.record(lambda: stage_a(t + 1)))
            streams.append(K.record(lambda: stage_b(t)))
            if pending is not None:
                streams.append(pending)
                pending = None
            K.play(*streams)
            if t % TC == TC - 1:
                pending = K.record(lambda: route_chunk(t // TC))
        if pending is not None:
            K.play(pending)
        if stage == "F1":
            return finish()

        def mmc():
            for t in range(NT):
                last = T.matmul(pcnt, lhsT=ones_b[:, 0:1], rhs=mask_all[:, t, :], start=(t == 0), stop=(t == NT - 1))
            return last
        K.op(PE, mmc, [b_ones_b] + b_mask, [b_pcnt])
        K.op(DVE, lambda: V.tensor_copy(out=cntf[:], in_=pcnt), [b_pcnt], [b_cntf])
        K.op(DVE, lambda: V.tensor_copy(out=cnt_i[:], in_=cntf[:]), [b_cntf], [b_cnt])
        if stage == "F":
            K.barrier()
            K.op(DVE, lambda: V.tensor_copy(out=h2f[0][:, 0:4 * NT], in_=IDX[:].rearrange("p a b -> p (a b)")), [b_IDX], [b_h2f[0]])
            dbg_dump(h2f[0][:, 0:4 * NT], 128, 4 * NT, b_h2f[0], row0=0)
            dbg_dump(G_all[:].rearrange("p a b -> p (a b)"), 128, 4 * NT, b_G, row0=128)
            dbg_dump(cntf[:], 1, NE, b_cntf, row0=256)
            return finish()
        K.barrier()

    catA_stack.close()
    es_mix.close()
    cat_stack.close()
    K.barrier()
    mod_stack.close()
    with ExitStack() as es:
        NW = 3
        w1b = [sb(es, f"w1b{i}", [128, 8, 2 * D], BF) for i in range(NW)]
        b_w1b = [[K.buf(f"w1b{i}")] for i in range(NW)]
        w2b = [sb(es, f"w2b{i}", [128, 8, D], BF) for i in range(NW)]
        b_w2b = [[K.buf(f"w2b{i}")] for i in range(NW)]
        b1all = sb(es, "b1all", [96, 2 * D], BF); b_b1b = [K.buf(f"b1b{i}") for i in range(NW)]
        b2all = sb(es, "b2all", [96, D], BF); b_b2b = [K.buf(f"b2b{i}") for i in range(NW)]
        NS = 4
        xe = [[sb(es, f"xe{i}{b}", [128, D], BF) for b in range(2)] for i in range(NS)]
        b_xe = [[K.buf(f"xe{i}{b}") for b in range(2)] for i in range(NS)]
        tb = [[sb(es, f"tb{i}{b}", [128, 1], I32) for b in range(2)] for i in range(NS)]
        b_tb = [[K.buf(f"tb{i}{b}") for b in range(2)] for i in range(NS)]
        for i in range(NS):
            for b in range(2):
                K.op(DVE, lambda i=i, b=b: V.memset(xe[i][b][:], 0.0), writes=[b_xe[i][b]])
        xT2 = [[sb(es, f"xT{i}{b}", [128, 8, 128], BF) for b in range(2)] for i in range(2)]
        b_xT2 = [[K.buf(f"xT{i}{b}") for b in range(2)] for i in range(2)]
        xT = [xT2[i % 2] for i in range(NS)]
        b_xT = [b_xT2[i % 2] for i in range(NS)]
        glu1 = sb(es, "glu", [128, 512]); glu = [glu1, glu1]; b_glu1 = K.buf("glu"); b_glu = [b_glu1, b_glu1]
        sg1 = sb(es, "sg", [128, 512]); sg = [sg1, sg1]; b_sg1 = K.buf("sg"); b_sg = [b_sg1, b_sg1]
        lin1 = sb(es, "lin", [128, 512]); lin = [lin1, lin1]; b_lin1 = K.buf("lin"); b_lin = [b_lin1, b_lin1]
        actb = [sb(es, f"actb{b}", [128, D], BF) for b in range(2)]; b_actb = [K.buf(f"actb{b}") for b in range(2)]
        aT1 = sb(es, "aT", [128, 8, 128], BF); aT = [aT1, aT1]; b_aT1 = K.buf("aT"); b_aT = [b_aT1, b_aT1]
        yo = [sb(es, f"yo{i}", [128, D]) for i in range(2)]; b_yo = [K.buf(f"yo{i}") for i in range(2)]
        pTx = ps(es, "pTx", [128, D], BF); b_pTx = K.pbuf("pTx")
        pgu = [ps(es, f"pgu{i}", [128, D]) for i in range(2)]; b_pgu = [K.pbuf(f"pgu{i}") for i in range(2)]
        pTa = ps(es, "pTa", [128, D], BF); b_pTa = K.pbuf("pTa")
        py = ps(es, "py", [128, D]); b_py = K.pbuf("py")
        NPAIR = nbmax // 2
        GRP = 4

        def load_w(e):
            s = e % NW
            v1 = w1_d[e].rearrange("(kc p) n -> p kc n", p=128)
            v2 = w2_d[e].rearrange("(kc p) n -> p kc n", p=128)
            K.dma(POOL, lambda: [G.dma_start(out=w1b[s][:, kc, :], in_=v1[:, kc, :]) for kc in range(8)], writes=[b_w1b[s][0]])
            K.dma(POOL, lambda: [G.dma_start(out=w2b[s][:, kc, :], in_=v2[:, kc, :]) for kc in range(8)], writes=[b_w2b[s][0]])
            K.dma(POOL, lambda: G.dma_start(out=b1all[32 * s:32 * s + 1, :], in_=b1_d[e:e + 1, :]), writes=[b_b1b[s]])
            K.dma(POOL, lambda: G.dma_start(out=b2all[32 * s:32 * s + 1, :], in_=b2_d[e:e + 1, :]), writes=[b_b2b[s]])

        bcreg = G.alloc_register("bcreg")
        G.reg_mov(bcreg, S * 4 - 1)

        def load_pair(e, pr, s):
            r0 = e * CAP + pr * 256
            for b in range(2):
                K.dma(SP, lambda b=b: Q.dma_start(out=tb[s][b][:], in_=Tab_d[r0 + b * 128:r0 + (b + 1) * 128, :]), [b_Tab], [b_tb[s][b]])
            for b in range(2):
                K.dma(POOL, lambda b=b: G.indirect_dma_start(out=xe[s][b][:], out_offset=None, in_=H2r_d,
                                                             in_offset=IOA(ap=tb[s][b][:, 0:1], axis=0),
                                                             bounds_check=bcreg, oob_is_err=False),
                      [b_H2r, b_tb[s][b]], [b_xe[s][b]])

        ei = [0]

        def slot_of(e, pr):
            return 2 * (e % 2) + (pr % 2)

        def p1(e, pr):
            s = slot_of(e, pr)
            for b in range(2):
                def trx(b=b):
                    for kc in range(8):
                        last = T.transpose(pTx[:, kc * 128:(kc + 1) * 128], xe[s][b][:, kc * 128:(kc + 1) * 128], ident_b[:])
                    return last
                K.op(PE, trx, [b_xe[s][b], b_ident_b], [b_pTx])
                K.op(DVE, lambda b=b: V.tensor_copy(out=xT[s][b][:].rearrange("p k n -> p (k n)"), in_=pTx[:]), [b_pTx], [b_xT[s][b]])

        def pair_body(e, pr, s, ws, after_loads=None):
            if pr + 1 < NPAIR:
                load_pair(e, pr + 1, slot_of(e, pr + 1))
            if after_loads is not None:
                after_loads()
            def stage2(b, h):
                def mm1():
                    for n, col in ((0, h * 512), (1, D + h * 512)):
                        for kc in range(8):
                            T.matmul(pgu[h][:, n * 512:(n + 1) * 512], lhsT=xT[s][b][:, kc, :], rhs=w1b[ws][:, kc, col:col + 512],
                                     start=(kc == 0), stop=False)
                        last = T.matmul(pgu[h][:, n * 512:(n + 1) * 512], lhsT=ones_b[32 * ws:32 * ws + 1, :], rhs=b1all[32 * ws:32 * ws + 1, col:col + 512],
                                        start=False, stop=True)
                    return last
                K.op(PE, mm1, [b_xT[s][b], b_b1b[ws], b_ones_b] + b_w1b[ws], [b_pgu[h]])
                r = ei[0] % 2
                ei[0] += 1
                K.op(DVE, lambda: V.tensor_scalar(out=glu[r][:], in0=pgu[h][:, 0:512], scalar1=7.0, scalar2=None, op0=ALU.min),
                     [b_pgu[h]], [b_glu[r]])
                K.op(ACT, lambda: A.activation(out=sg[r][:], in_=glu[r][:], func=AF.Sigmoid, scale=1.702), [b_glu[r]], [b_sg[r]])
                K.op(DVE, lambda: V.tensor_scalar(out=lin[r][:], in0=pgu[h][:, 512:1024], scalar1=-7.0, scalar2=7.0,
                                                  op0=ALU.max, op1=ALU.min), [b_pgu[h]], [b_lin[r]])
                K.op(DVE, lambda: V.scalar_tensor_tensor(out=lin[r][:], in0=lin[r][:], scalar=1.0, in1=glu[r][:],
                                                         op0=ALU.add, op1=ALU.mult), [b_glu[r]], [b_lin[r]])
                K.op(DVE, lambda: V.tensor_tensor(out=actb[b][:, h * 512:(h + 1) * 512], in0=lin[r][:], in1=sg[r][:], op=ALU.mult),
                     [b_lin[r], b_sg[r]], [b_actb[b]])

            def stage3(b):
                def tra():
                    for kc in range(8):
                        last = T.transpose(pTa[:, kc * 128:(kc + 1) * 128], actb[b][:, kc * 128:(kc + 1) * 128], ident_b[:])
                    return last
                K.op(PE, tra, [b_actb[b], b_ident_b], [b_pTa])
                K.op(DVE, lambda: V.tensor_copy(out=aT[b][:].rearrange("p k n -> p (k n)"), in_=pTa[:]), [b_pTa], [b_aT[b]])

            def stage4(b):
                def mm2():
                    for n in range(2):
                        for kc in range(8):
                            T.matmul(py[:, n * 512:(n + 1) * 512], lhsT=aT[b][:, kc, :], rhs=w2b[ws][:, kc, n * 512:(n + 1) * 512],
                                     start=(kc == 0), stop=False)
                        last = T.matmul(py[:, n * 512:(n + 1) * 512], lhsT=ones_b[32 * ws:32 * ws + 1, :], rhs=b2all[32 * ws:32 * ws + 1, n * 512:(n + 1) * 512],
                                        start=False, stop=True)
                    return last
                K.op(PE, mm2, [b_aT[b], b_b2b[ws], b_ones_b] + b_w2b[ws], [b_py])
                K.op(ACT, lambda: A.copy(out=yo[b][:], in_=py[:]), [b_py], [b_yo[b]])
                K.dma(POOL, lambda: G.indirect_dma_start(out=Yb_d, out_offset=IOA(ap=tb[s][b][:, 0:1], axis=0),
                                                         in_=yo[b][:], in_offset=None,
                                                         bounds_check=bcreg, oob_is_err=False),
                      [b_yo[b], b_tb[s][b]], [b_Yb])

            stage2(0, 0)
            stage2(0, 1)
            stage2(1, 0)
            stage3(0)
            stage2(1, 1)
            stage4(0)
            stage3(1)
            stage4(1)
            if pr + 1 < NPAIR:
                p1(e, pr + 1)

        if use_if:
            regs = nc.alloc_registers("cntreg")
        load_w(0)
        load_w(1)
        load_pair(0, 0, slot_of(0, 0))
        p1(0, 0)
        for e in range(NE):
            ws = e % NW
            if e + 1 < NE:
                load_pair(e + 1, 0, slot_of(e + 1, 0))
            if use_if:
                for eng in K.engs:
                    K.wait_buf(eng, b_cnt)
                for reg in regs:
                    nc.reg_load(reg, cnt_i[0:1, e:e + 1])
            pair_body(e, 0, slot_of(e, 0), ws, after_loads=(lambda: load_w(e + 2)) if e + 2 < NE else None)

            def chain(pr):
                if pr >= NPAIR:
                    return
                snap = K.snapshot()
                ctx = nc.If_cmp(regs, 256 * pr, "IS_GT") if use_if else ExitStack()
                with ctx:
                    pair_body(e, pr, slot_of(e, pr), ws)
                    chain(pr + 1)
                if use_if:
                    with nc.Else():
                        K.compensate(snap)
                    K.restore_seen(snap)
            chain(1)
            if e + 1 < NE:
                p1(e + 1, 0)
        K.barrier()

    with ExitStack() as es:
        NR = 3
        x1t = [sb(es, f"x1t{i}", [128, D]) for i in range(NR)]; b_x1t = [K.buf(f"x1t{i}") for i in range(NR)]
        yg = [sb(es, f"yg{i}", [128, 4, D]) for i in range(NR)]; b_yg = [K.buf(f"yg{i}") for i in range(NR)]
        Yb_v = Yb_d.rearrange("(n r) d -> n (r d)", r=4)
        acc = [sb(es, f"acc{i}", [128, D]) for i in range(2)]; b_acc = [K.buf(f"acc{i}") for i in range(2)]

        def loads(t):
            s = t % NR
            rows = slice(t * 128, (t + 1) * 128)
            K.dma(SP, lambda: Q.dma_start(out=yg[s][:].rearrange("p r d -> p (r d)"), in_=Yb_v[rows, :]), [b_Yb], [b_yg[s]])
            K.dma(SP, lambda: Q.dma_start(out=x1t[s][:], in_=X1_d[rows, :]), [b_X1], [b_x1t[s]])
        loads(0)
        loads(1)
        for t in range(NT):
            s = t % NR
            a = t % 2
            rows = slice(t * 128, (t + 1) * 128)
            if t + 2 < NT:
                loads(t + 2)
            K.op(DVE, lambda: V.tensor_scalar(out=acc[a][:], in0=yg[s][:, 0, :], scalar1=G_all[:, t, 0:1], scalar2=None, op0=ALU.mult),
                 [b_yg[s], b_G], [b_acc[a]])
            for r in range(1, 4):
                K.op(DVE, lambda r=r: V.scalar_tensor_tensor(out=acc[a][:], in0=yg[s][:, r, :], scalar=G_all[:, t, r:r + 1],
                                                             in1=acc[a][:], op0=ALU.mult, op1=ALU.add),
                     [b_yg[s], b_G], [b_acc[a]])
            K.op(DVE, lambda: V.tensor_tensor(out=acc[a][:], in0=acc[a][:], in1=gate2k[:], op=ALU.mult), [b_gate2k], [b_acc[a]])
            K.op(DVE, lambda: V.tensor_tensor(out=acc[a][:], in0=acc[a][:], in1=x1t[s][:], op=ALU.add), [b_x1t[s]], [b_acc[a]])
            K.dma(ACT, lambda: A.dma_start(out=out_d[rows, :], in_=acc[a][:]), [b_acc[a]], [b_out])
    return finish()


def prep_inputs(inp):
    f = np.float32
    L = 0
    g = lambda n: np.asarray(inp[n][L])
    w_in = g("w_in")
    kro = w_in[:, 1408:1440]
    perm = np.concatenate([np.arange(16, 32), np.arange(0, 16)])
    w_in_ext = np.ascontiguousarray(np.concatenate([w_in[:, :1408], kro[:, perm], kro], axis=1), dtype=f)
    fm = lambda v: np.ascontiguousarray(np.asarray(v, dtype=f).reshape(-1, 128).T)
    cw = g("conv_w")
    fields = [cw[0], cw[1], cw[2], cw[3], g("conv_b"), g("b_a"), g("b_x"), g("lam")]
    lruv = np.ascontiguousarray(np.stack([fm(v) for v in fields], axis=-1), dtype=f)

    def bd(w):
        o = np.zeros((4, 128, 128), f)
        for c in range(4):
            o[c, 0:64, 0:64] = w[2 * c]
            o[c, 64:128, 64:128] = w[2 * c + 1]
        return o
    w_uq = g("w_uq"); w_ukv = g("w_ukv")
    w_uq_h = np.zeros((8, 256, 128), f)
    w_uk_h = np.zeros((8, 128, 128), f)
    w_uv = np.zeros((128, 512), f)
    for h in range(8):
        nope = w_uq[:, h * 96:h * 96 + 64]; rope = w_uq[:, h * 96 + 64:h * 96 + 96]
        w_uq_h[h] = np.concatenate([rope[:, perm], rope, nope], axis=1)
        w_uk_h[h, :, 64:128] = w_ukv[:, h * 128:h * 128 + 64]
        w_uv[:, h * 64:(h + 1) * 64] = w_ukv[:, h * 128 + 64:h * 128 + 128]

    def grow(gv):
        return np.ascontiguousarray(np.concatenate([gv[64:96][perm], gv[64:96], gv[0:64]]).reshape(128, 1), dtype=f)
    half = 16
    freqs = (10000.0 ** (-np.arange(half, dtype=np.float64) / half)).astype(f)
    fr32 = np.concatenate([freqs, freqs])
    rtab = np.zeros((64, 3), f)
    rtab[0:32, 0] = fr32; rtab[32:64, 0] = fr32
    rtab[0:32, 1] = 0.0; rtab[32:64, 1] = math.pi / 2
    rtab[0:16, 2] = -1.0; rtab[16:32, 2] = 1.0; rtab[32:64, 2] = 1.0
    shared = {
        "w_ada": g("w_ada"), "b_ada": g("b_ada").reshape(1, -1),
        "g_mix_bc": np.ascontiguousarray(np.broadcast_to(g("g_mix")[None, :], (128, D))),
        "g_ffn_bc": np.ascontiguousarray(np.broadcast_to(g("g_ffn")[None, :], (128, D))),
        "w_in_ext": w_in_ext, "lruv": lruv, "wa_bd": bd(g("w_a")), "wx_bd": bd(g("w_x")),
        "g_q_lat": fm(g("g_q_lat")), "g_kv_lat": fm(g("g_kv_lat")),
        "w_uq_h": w_uq_h, "w_uk_h": w_uk_h, "w_uv": w_uv, "gq": grow(g("g_qn")), "gk": grow(g("g_kn")),
        "rtab": rtab, "w_out": g("w_out"), "w_router": g("w_router"), "b_router": g("b_router").reshape(1, -1),
        "w1": g("w1"), "b1": g("b1"), "w2": g("w2"), "b2": g("b2"),
    }
    shared = {k: np.ascontiguousarray(v, dtype=f) for k, v in shared.items()}
    maps = []
    for b in range(8):
        m = dict(shared)
        m["x"] = np.ascontiguousarray(inp["x"][b], dtype=f)
        m["cT"] = fm(np.asarray(inp["c"][b]))
        m["pos"] = np.ascontiguousarray(np.broadcast_to(np.asarray(inp["positions"][b], dtype=np.int32)[None, :], (64, S)))
        maps.append(m)
    return maps


def kernel(**inputs):
    maps = prep_inputs(inputs)
    nc = build_nc("full")
    res = run_bass_kernel_spmd(nc, maps, core_ids=list(range(8)))
    return np.stack([np.asarray(r["out"], dtype=np.float32) for r in res.results], axis=0)
```
